# Optimizing a Trainium2 kernel written in Bass

```python
import jax, jax.numpy as jnp
from jax import lax
import numpy as np

D_MODEL = 1024
BATCH = 8
SEQ = 4096
DEPTH = 1

CONV_CH = 512
CONV_WIDTH = 31
N_HEADS = 8
N_KV_HEADS = 2
HEAD_DIM = 64
WINDOW = 128
ATTN_BLOCK = 128
N_GROUPS = 4
EXPERTS_PER_GROUP = 8
N_EXPERTS = N_GROUPS * EXPERTS_PER_GROUP
EXPERT_TOP_K = 2
D_FF_EXPERT = 512
MOE_BLOCK = 128
NORM_EPS = 1e-6

Q_DIM = N_HEADS * HEAD_DIM
KV_DIM = N_KV_HEADS * HEAD_DIM
IN_COLS = 2 * CONV_CH + Q_DIM + 2 * KV_DIM + 2 * D_MODEL

kernel_name = "hybrid_conv_swa_sink_alibi_hiermoe"


def rms_norm(x, g):
    xf = x.astype(jnp.float32)
    y = xf * lax.rsqrt(jnp.mean(xf * xf, axis=-1, keepdims=True) + NORM_EPS)
    return (y * g.astype(jnp.float32)).astype(x.dtype)


def layer_norm(x, g, b):
    xf = x.astype(jnp.float32)
    mu = jnp.mean(xf, axis=-1, keepdims=True)
    var = jnp.mean(jnp.square(xf - mu), axis=-1, keepdims=True)
    y = (xf - mu) * lax.rsqrt(var + NORM_EPS)
    return (y * g.astype(jnp.float32) + b.astype(jnp.float32)).astype(x.dtype)


def alibi_slopes():
    return jnp.asarray(np.array([2.0 ** (-8.0 * (h + 1) / N_HEADS) for h in range(N_HEADS)], np.float32))


def conformer_conv(u, w_dw, b_dw, ln_g, ln_b):
    a, gate = jnp.split(u, 2, axis=-1)
    v = a * jax.nn.sigmoid(gate)
    y = lax.conv_general_dilated(
        v, w_dw, window_strides=(1,), padding=[(CONV_WIDTH - 1, 0)],
        dimension_numbers=("NWC", "WIO", "NWC"), feature_group_count=CONV_CH)
    y = y + b_dw
    return jax.nn.silu(layer_norm(y, ln_g, ln_b))


def sliding_window_attention(q, k, v, sinks):
    b, s = q.shape[0], q.shape[1]
    nb = s // ATTN_BLOCK
    grp = N_HEADS // N_KV_HEADS
    qb = q.reshape(b, nb, ATTN_BLOCK, N_KV_HEADS, grp, HEAD_DIM)
    kb = k.reshape(b, nb, ATTN_BLOCK, N_KV_HEADS, HEAD_DIM)
    vb = v.reshape(b, nb, ATTN_BLOCK, N_KV_HEADS, HEAD_DIM)

    def with_prev(t):
        prev = jnp.pad(t[:, :-1], ((0, 0), (1, 0), (0, 0), (0, 0), (0, 0)))
        return jnp.concatenate([prev, t], axis=2)

    kk, vv = with_prev(kb), with_prev(vb)
    scores = jnp.einsum("bnqhgd,bnkhd->bnhgqk", qb, kk,
                        preferred_element_type=jnp.float32) * (HEAD_DIM ** -0.5)
    qi = jnp.arange(ATTN_BLOCK)[:, None]
    kj = jnp.arange(2 * ATTN_BLOCK)[None, :]
    rel = ATTN_BLOCK + qi - kj
    in_window = (rel >= 0) & (rel < WINDOW)
    key_exists = (jnp.arange(nb)[:, None, None] * ATTN_BLOCK + kj[None] - ATTN_BLOCK) >= 0
    mask = in_window[None] & key_exists
    slopes = alibi_slopes().reshape(N_KV_HEADS, grp)
    scores = scores - slopes[:, :, None, None] * rel.astype(jnp.float32)
    scores = jnp.where(mask[None, :, None, None], scores, -jnp.inf)
    sink = sinks.astype(jnp.float32).reshape(N_KV_HEADS, grp)[None, None, :, :, None, None]
    m = jnp.maximum(jnp.max(scores, axis=-1, keepdims=True), sink)
    p = jnp.exp(scores - m)
    p = p / (jnp.sum(p, axis=-1, keepdims=True) + jnp.exp(sink - m))
    out = jnp.einsum("bnhgqk,bnkhd->bnqhgd", p.astype(v.dtype), vv)
    return out.reshape(b, s, Q_DIM)


def hierarchical_moe(xn, w_group, b_group, w_expert, b_expert, w_gate, w_up, w_down):
    b, s, d = xn.shape
    t = b * s
    xt = xn.reshape(t, d)
    g_logits = (xt @ w_group).astype(jnp.float32) + b_group.astype(jnp.float32)
    g_prob = jax.nn.softmax(g_logits, axis=-1)
    g_sel = jnp.argmax(g_logits, axis=-1).astype(jnp.int32)
    p_group = jnp.take_along_axis(g_prob, g_sel[:, None], axis=-1)[:, 0]
    e_logits = ((xt @ w_expert).astype(jnp.float32) + b_expert.astype(jnp.float32)).reshape(t, N_GROUPS, EXPERTS_PER_GROUP)
    e_in = jnp.take_along_axis(e_logits, g_sel[:, None, None], axis=1)[:, 0]
    top_val, top_idx = lax.top_k(e_in, EXPERT_TOP_K)
    weights = p_group[:, None] * jax.nn.softmax(top_val, axis=-1)
    expert_id = (g_sel[:, None] * EXPERTS_PER_GROUP + top_idx.astype(jnp.int32)).reshape(-1)
    token_id = jnp.repeat(jnp.arange(t, dtype=jnp.int32), EXPERT_TOP_K)
    w_flat = weights.reshape(-1)
    n_assign = t * EXPERT_TOP_K
    n_blocks = -(-(n_assign + N_EXPERTS * (MOE_BLOCK - 1)) // MOE_BLOCK)
    order = jnp.argsort(expert_id, stable=True)
    sorted_e = expert_id[order]
    counts = jnp.zeros((N_EXPERTS,), jnp.int32).at[expert_id].add(1)
    padded = ((counts + MOE_BLOCK - 1) // MOE_BLOCK) * MOE_BLOCK
    padded_ends = jnp.cumsum(padded)
    padded_starts = padded_ends - padded
    starts = jnp.cumsum(counts) - counts
    rank = jnp.arange(n_assign, dtype=jnp.int32) - starts[sorted_e]
    dest = padded_starts[sorted_e] + rank
    slot_token = jnp.zeros((n_blocks * MOE_BLOCK,), jnp.int32).at[dest].set(token_id[order])
    slot_w = jnp.zeros((n_blocks * MOE_BLOCK,), jnp.float32).at[dest].set(w_flat[order])
    block_start = jnp.arange(n_blocks, dtype=jnp.int32) * MOE_BLOCK
    block_expert = jnp.clip(jnp.searchsorted(padded_ends, block_start, side="right"), 0, N_EXPERTS - 1).astype(jnp.int32)

    def run_block(args):
        tok, wt, e = args
        xb = xt[tok]
        hmid = jax.nn.silu(xb @ w_gate[e]) * (xb @ w_up[e])
        return (hmid @ w_down[e]) * wt[:, None].astype(xb.dtype)

    ys = lax.map(run_block, (slot_token.reshape(n_blocks, MOE_BLOCK),
                             slot_w.reshape(n_blocks, MOE_BLOCK), block_expert))
    out = jnp.zeros((t, d), xn.dtype).at[slot_token].add(ys.reshape(-1, d))
    return out.reshape(b, s, d)


def setup_inputs(seed: int = 0) -> dict:
    key = jax.random.key(seed)
    ks = jax.random.split(key, 24)
    f32 = jnp.float32

    def nrm(k, shape, scale):
        return jax.random.normal(k, shape, f32) * scale

    L = DEPTH
    return {
        "x": jax.random.normal(ks[0], (BATCH, SEQ, D_MODEL), f32),
        "g_mix": 1.0 + nrm(ks[1], (L, D_MODEL), 0.02),
        "w_in": nrm(ks[2], (L, D_MODEL, IN_COLS), D_MODEL ** -0.5),
        "w_dw": nrm(ks[3], (L, CONV_WIDTH, 1, CONV_CH), CONV_WIDTH ** -0.5),
        "b_dw": nrm(ks[4], (L, CONV_CH), 0.02),
        "ln_conv_g": 1.0 + nrm(ks[5], (L, CONV_CH), 0.02),
        "ln_conv_b": nrm(ks[6], (L, CONV_CH), 0.02),
        "sinks": nrm(ks[7], (L, N_HEADS), 0.5),
        "w_conv_out": nrm(ks[8], (L, CONV_CH, D_MODEL), CONV_CH ** -0.5),
        "w_attn_out": nrm(ks[9], (L, Q_DIM, D_MODEL), Q_DIM ** -0.5),
        "w_out": nrm(ks[10], (L, D_MODEL, D_MODEL), D_MODEL ** -0.5),
        "g_ffn": 1.0 + nrm(ks[11], (L, D_MODEL), 0.02),
        "w_group": nrm(ks[12], (L, D_MODEL, N_GROUPS), D_MODEL ** -0.5),
        "b_group": nrm(ks[13], (L, N_GROUPS), 0.01),
        "w_expert": nrm(ks[14], (L, D_MODEL, N_EXPERTS), D_MODEL ** -0.5),
        "b_expert": nrm(ks[15], (L, N_EXPERTS), 0.01),
        "w_gate": nrm(ks[16], (L, N_EXPERTS, D_MODEL, D_FF_EXPERT), D_MODEL ** -0.5),
        "w_up": nrm(ks[17], (L, N_EXPERTS, D_MODEL, D_FF_EXPERT), D_MODEL ** -0.5),
        "w_down": nrm(ks[18], (L, N_EXPERTS, D_FF_EXPERT, D_MODEL), D_FF_EXPERT ** -0.5),
        "g_final": 1.0 + nrm(ks[19], (D_MODEL,), 0.02),
    }


def reference(x, g_mix, w_in, w_dw, b_dw, ln_conv_g, ln_conv_b, sinks, w_conv_out, w_attn_out,
              w_out, g_ffn, w_group, b_group, w_expert, b_expert, w_gate, w_up, w_down, g_final):
    b, s, _ = x.shape
    cuts = list(np.cumsum([2 * CONV_CH, Q_DIM, KV_DIM, KV_DIM, D_MODEL]))
    h = x
    for l in range(DEPTH):
        xn = rms_norm(h, g_mix[l])
        proj = xn @ w_in[l]
        u_conv, q, k, v, gate_conv, gate_attn = jnp.split(proj, cuts, axis=-1)
        conv_o = conformer_conv(u_conv, w_dw[l], b_dw[l], ln_conv_g[l], ln_conv_b[l]) @ w_conv_out[l]
        attn = sliding_window_attention(q.reshape(b, s, N_HEADS, HEAD_DIM),
                                        k.reshape(b, s, N_KV_HEADS, HEAD_DIM),
                                        v.reshape(b, s, N_KV_HEADS, HEAD_DIM), sinks[l])
        attn_o = attn @ w_attn_out[l]
        merged = jax.nn.sigmoid(gate_conv) * conv_o + jax.nn.sigmoid(gate_attn) * attn_o
        h = h + merged @ w_out[l]
        h = h + hierarchical_moe(rms_norm(h, g_ffn[l]), w_group[l], b_group[l], w_expert[l],
                                 b_expert[l], w_gate[l], w_up[l], w_down[l])
    return rms_norm(h, g_final)
```

```python
import numpy as np
from contextlib import ExitStack
import concourse.bass as bass
import concourse.mybir as mybir
from concourse.bass_utils import run_bass_kernel_spmd

F32 = mybir.dt.float32
BF16 = mybir.dt.bfloat16
I32 = mybir.dt.int32
AF = mybir.ActivationFunctionType
ALU = mybir.AluOpType
AX = mybir.AxisListType

PE, ACT, DVE, POOL, SP = "pe", "act", "dve", "pool", "sp"

D_MODEL = 1024
SEQ = 4096
NTILE = SEQ // 128
NT = 256
TPS = NT // 128
NST = SEQ // NT
CONV_CH = 512
CW = 31
N_EXP = 32
CAP = 448
BLKS = [(0, 128), (128, 128), (256, 128), (384, 64)]
DFF = 512
EPS = 1e-6
BIG = 1.0e30
SEM_EPOCH = 20000


class Prog:
    def __init__(self, nc, n_dma_sems=8):
        self.nc = nc
        self.ops = []
        self.n_dma_sems = n_dma_sems
        self.last_w = {}
        self.readers = {}
        self.last_op = {}
        self.recent_dma = {}

    def add(self, eng, emit, reads=(), writes=(), dma=False):
        ops = self.ops
        i = len(ops)
        op = dict(eng=eng, emit=emit, dma=dma, deps=set(), sig=False)
        deps = set()
        for r in reads:
            w = self.last_w.get(r)
            if w is not None:
                deps.add((w, False))
        for wr in writes:
            w = self.last_w.get(wr)
            if w is not None:
                deps.add((w, False))
            for rd in self.readers.get(wr, ()):
                deps.add((rd, True))
        for r in reads:
            self.readers.setdefault(r, []).append(i)
        for wr in writes:
            self.last_w[wr] = i
            self.readers[wr] = []
        final = set()
        for d, war in deps:
            if d == i:
                continue
            p = ops[d]
            if not p["dma"] and not dma and p["eng"] == eng:
                if eng == PE:
                    continue
                if war:
                    continue
            final.add(d)
        op["deps"] = final
        for d in final:
            ops[d]["sig"] = True
        ops.append(op)
        self.last_op[eng] = i
        if dma:
            self.recent_dma.setdefault(eng, []).append(i)
            self.recent_dma[eng] = self.recent_dma[eng][-self.n_dma_sems:]
        return i

    def barrier(self):
        deps = set(self.last_op.values())
        for lst in self.recent_dma.values():
            deps.update(lst)
        for e in (PE, ACT, DVE, POOL, SP):
            i = len(self.ops)
            op = dict(eng=e, emit=None, dma=False, deps=set(deps), sig=False)
            self.ops.append(op)
            self.last_op[e] = i
        for d in deps:
            self.ops[d]["sig"] = True
        self.last_w = {}
        self.readers = {}

    def emit(self, stack):
        nc = self.nc
        ops = self.ops
        engs = [PE, ACT, DVE, POOL, SP]
        nsig = {e: sum(1 for o in ops if o["eng"] == e and o["sig"] and not o["dma"]) for e in engs}
        csem = {e: [stack.enter_context(nc.semaphore("c_%s%d" % (e, k)))
                    for k in range(nsig[e] // SEM_EPOCH + 1)] for e in engs}
        dsem = {e: [stack.enter_context(nc.semaphore("d_%s%d" % (e, k)))
                    for k in range(self.n_dma_sems)] for e in (ACT, POOL, SP)}
        ccount = {e: 0 for e in engs}
        dcount = {e: 0 for e in dsem}
        dval = {e: [0] * self.n_dma_sems for e in dsem}
        for op in ops:
            e = op["eng"]
            if op["dma"]:
                k = dcount[e] % self.n_dma_sems
                dcount[e] += 1
                op["prev_slot"] = (dsem[e][k], dval[e][k]) if dval[e][k] else None
                dval[e][k] += 16
                op["signal"] = (dsem[e][k], dval[e][k])
            elif op["sig"]:
                ep, v = divmod(ccount[e], SEM_EPOCH)
                ccount[e] += 1
                op["signal"] = (csem[e][ep], v + 1)
            else:
                op["signal"] = None
        per_eng = {e: [op for op in ops if op["eng"] == e] for e in engs}
        self.stats = {e: len(per_eng[e]) for e in engs}
        nwaits = {e: 0 for e in engs}

        def run(e, engine):
            waited = {}

            def wait(sem, val):
                key = id(sem)
                if waited.get(key, 0) >= val:
                    return
                waited[key] = val
                engine.wait_ge(sem, val)
                nwaits[e] += 1

            for op in per_eng[e]:
                need = {}
                for d in op["deps"]:
                    s, v = ops[d]["signal"]
                    k = id(s)
                    if k not in need or need[k][1] < v:
                        need[k] = (s, v)
                if op["dma"] and op["prev_slot"] is not None:
                    s, v = op["prev_slot"]
                    k = id(s)
                    if k not in need or need[k][1] < v:
                        need[k] = (s, v)
                for s, v in need.values():
                    wait(s, v)
                if op["emit"] is None:
                    if op["signal"] is not None:
                        engine.nop().then_inc(op["signal"][0], 1)
                    continue
                ins = op["emit"](engine)
                if op["signal"] is not None:
                    s, v = op["signal"]
                    ins.then_inc(s, 16 if op["dma"] else 1)
            if e in dsem:
                for k, s in enumerate(dsem[e]):
                    if dval[e][k]:
                        wait(s, dval[e][k])

        with nc.Block() as block:
            @block.tensor
            def _(eng):
                run(PE, eng)

            @block.scalar
            def _(eng):
                run(ACT, eng)

            @block.vector
            def _(eng):
                run(DVE, eng)

            @block.gpsimd
            def _(eng):
                run(POOL, eng)

            @block.sync
            def _(eng):
                run(SP, eng)
        self.nwaits = nwaits


def build_nc(debug=False):
    nc = bass.Bass("TRN2", target_bir_lowering=False)
    scr_kind = "ExternalOutput" if debug else "Internal"
    dt_in = lambda n, s: nc.dram_tensor(n, s, F32, kind="ExternalInput")
    x_d = dt_in("x", [SEQ, D_MODEL]).ap()
    g_mix_d = dt_in("g_mix", [1, D_MODEL])
    w_in_d = dt_in("w_in", [1, D_MODEL, 3840]).ap()[0]
    w_dw_d = dt_in("w_dw", [1, CW, 1, CONV_CH]).ap()
    b_dw_d = dt_in("b_dw", [1, CONV_CH]).ap()
    lng_d = dt_in("ln_conv_g", [1, CONV_CH]).ap()
    lnb_d = dt_in("ln_conv_b", [1, CONV_CH]).ap()
    sinks_d = dt_in("sinks", [1, 8]).ap()
    wc_d = dt_in("w_conv_out", [1, CONV_CH, D_MODEL]).ap()[0]
    wa_d = dt_in("w_attn_out", [1, 512, D_MODEL]).ap()[0]
    wo_d = dt_in("w_out", [1, D_MODEL, D_MODEL]).ap()[0]
    g_ffn_d = dt_in("g_ffn", [1, D_MODEL]).ap()
    wgrp_d = dt_in("w_group", [1, D_MODEL, 4]).ap()[0]
    bgrp_d = dt_in("b_group", [1, 4]).ap()
    wexp_d = dt_in("w_expert", [1, D_MODEL, N_EXP]).ap()[0]
    bexp_d = dt_in("b_expert", [1, N_EXP]).ap()
    wgate_d = dt_in("w_gate", [1, N_EXP, D_MODEL, DFF]).ap()[0]
    wup_d = dt_in("w_up", [1, N_EXP, D_MODEL, DFF]).ap()[0]
    wdown_d = dt_in("w_down", [1, N_EXP, DFF, D_MODEL]).ap()[0]
    g_fin_d = dt_in("g_final", [D_MODEL]).ap()
    out_d = nc.dram_tensor("out", [SEQ, D_MODEL], F32, kind="ExternalOutput").ap()
    hbuf_d = nc.dram_tensor("hbuf", [SEQ, D_MODEL], F32, kind=scr_kind).ap()
    xs_d = nc.dram_tensor("xs_scr", [N_EXP * CAP, D_MODEL], BF16, kind=scr_kind).ap()
    ys_d = nc.dram_tensor("ys_scr", [N_EXP * CAP, D_MODEL], BF16, kind=scr_kind).ap()
    wbf_d = nc.dram_tensor("wbf_scr", [N_EXP, 3, 128, 4096], BF16, kind="Internal").ap()

    with ExitStack() as st:
        def sb(name, shape, dtype):
            return st.enter_context(nc.sbuf_tensor(name, shape, dtype))

        P = Prog(nc)
        pbank = [st.enter_context(nc.psum_tensor("pb%d" % k, [128, 512], F32)) for k in range(8)]
        bank_ctr = [0]

        def nb():
            k = bank_ctr[0] % 8
            bank_ctr[0] += 1
            return k, pbank[k], ("pb", k)

        def MM(out, lhsT, rhs, start, stop, r, w):
            P.add(PE, lambda e: e.matmul(out, lhsT=lhsT, rhs=rhs, start=start, stop=stop), r, w)

        def TR32(out, in_, ident, r, w):
            P.add(PE, lambda e: e.transpose(out, in_, ident), r, w)

        def ACTV(out, in_, func, r, w, bias=None, scale=None, accum=None):
            kw = {}
            if bias is not None:
                kw["bias"] = bias
            if scale is not None:
                kw["scale"] = scale
            if accum is not None:
                kw["accum_out"] = accum
            P.add(ACT, lambda e: e.activation(out=out, in_=in_, func=func, **kw), r, w)

        def TT(eng, out, in0, in1, op, r, w):
            P.add(eng, lambda e: e.tensor_tensor(out=out, in0=in0, in1=in1, op=op), r, w)

        def TS(eng, out, in0, s1, s2, op0, op1, r, w, accum=None):
            if op1 is None:
                P.add(eng, lambda e: e.tensor_scalar(out=out, in0=in0, scalar1=s1, scalar2=None, op0=op0), r, w)
            elif accum is None:
                P.add(eng, lambda e: e.tensor_scalar(out=out, in0=in0, scalar1=s1, scalar2=s2, op0=op0, op1=op1), r, w)
            else:
                P.add(eng, lambda e: e.tensor_scalar(out=out, in0=in0, scalar1=s1, scalar2=s2, op0=op0, op1=op1,
                                                     accum_out=accum), r, w)

        def RSTD(dst, src, scale, rs, ws):
            P.add(ACT, lambda e: e.activation(out=dst, in_=src, func=AF.Sqrt, bias=epsc[:, 0:1], scale=scale), rs + ["epsc"], ws)
            P.add(DVE, lambda e: e.reciprocal(out=dst, in_=dst), ws, ws)

        def STT(eng, out, in0, scalar, in1, op0, op1, r, w):
            P.add(eng, lambda e: e.scalar_tensor_tensor(out=out, in0=in0, scalar=scalar, in1=in1, op0=op0, op1=op1), r, w)

        def CP(eng, out, in_, r, w):
            if eng == ACT:
                P.add(ACT, lambda e: e.activation(out=out, in_=in_, func=AF.Copy), r, w)
            else:
                P.add(eng, lambda e: e.tensor_copy(out=out, in_=in_), r, w)

        def RED(eng, out, in_, op, r, w):
            P.add(eng, lambda e: e.tensor_reduce(out=out, in_=in_, axis=AX.X, op=op), r, w)

        def DMA(eng, out, in_, r, w, **kw):
            P.add(eng, lambda e: e.dma_start(out=out, in_=in_, **kw), r, w, dma=True)

        def MEMSET(eng, ap, val, w):
            P.add(eng, lambda e: e.memset(ap, val), (), w)

        def TAP(name, ap, reads):
            if not debug:
                return
            shape = list(ap.shape)
            d = nc.dram_tensor("dbg_" + name, shape, F32, kind="ExternalOutput").ap()
            DMA(POOL, d, ap, reads, [("dbg", name)])

        identb = sb("identb", [128, 128], BF16)
        identf = sb("identf", [128, 128], F32)
        onesf = sb("onesf", [128, 128], F32)
        onesb = sb("onesb", [128, 128], BF16)
        ustrict = sb("ustrict", [128, 128], BF16)
        gffn_bc = sb("gffn_bc", [128, D_MODEL], F32)
        gmixT = sb("gmixT", [128, 8], F32)
        bdw = sb("bdw", [128, 4], F32)
        lng = sb("lng", [128, 4], F32)
        lnb = sb("lnb", [128, 4], F32)
        wdw_raw = sb("wdw_raw", [CW, CONV_CH], F32)
        wdwT = sb("wdwT", [128, 4, CW], F32)
        esink = sb("esink", [128, 8], F32)
        relt = sb("relt", [128, 2, 128], F32)
        amask = sb("amask", [128, 2, 128], F32)
        EM = sb("EM", [128, 4, 4, 128], BF16)
        wr = sb("wr", [128, 8, 36], F32)
        rbias = sb("rbias", [128, 36], F32)
        ebase = sb("ebase", [128, N_EXP], F32)
        cum = sb("cum", [128, N_EXP], F32)
        cumb2 = sb("cumb2", [128, 2, N_EXP], BF16)
        destall = sb("destall", [128, NTILE, 2], I32)
        wall = sb("wall", [128, NTILE, 2], F32)
        ss = sb("ss", [128, 8], F32)
        rstd = sb("rstd", [128, 8], F32)
        epsc = sb("epsc", [128, 1], F32)
        ztile = sb("ztile", [128, D_MODEL], BF16)
        ht2 = sb("ht2", [128, D_MODEL], F32)
        mtmp2 = sb("mtmp2", [128, 4, NT], F32)
        t1buf = sb("t1buf", [128, 2, NT], F32)

        ARENA_W = 44900
        arena = sb("arena", [128, ARENA_W], F32)
        off = [0]

        def carve(words, dtype, pattern=None, **kw):
            a = arena[:, off[0]:off[0] + words]
            off[0] += words
            assert off[0] <= ARENA_W, off[0]
            if dtype != F32:
                a = a.bitcast(dtype)
            if pattern:
                a = a.rearrange(pattern, **kw)
            return a

        wb_in = carve(15360, BF16, "p (k n) -> p k n", k=8)
        wcb = carve(2048, BF16, "p (k n) -> p k n", k=4)
        wab = carve(2048, BF16, "p (k n) -> p k n", k=4)
        wob = carve(4096, BF16, "p (k n) -> p k n", k=8)
        D64 = carve(3968, BF16, "p (c j m) -> p c j m", c=4, j=CW)
        xt = carve(4096, F32, "p (s n) -> p s n", s=4)
        xsb = carve(1024, BF16, "p (s n) -> p s n", s=2)
        xnT = carve(1024, BF16, "p (k n) -> p k n", k=8)
        vTs = carve(576, BF16, "p (c n) -> p c n", c=4)
        sig = carve(256, F32)
        qTs = carve(512, BF16, "p (c n) -> p c n", c=4)
        kTr = carve(192, BF16)
        vtok_raw = carve(200, BF16)
        vtok = vtok_raw[:, 0:390].rearrange("p (s k d) -> p s k d", s=3, k=2)
        RA = carve(1024, F32)
        RB = carve(1024, F32)
        emf = arena[:, off[0] - 2048:off[0]].rearrange("p (h k q) -> p h k q", h=8, k=2)
        RC = carve(1024, F32)
        RD = carve(1024, F32)
        pexp = carve(512, BF16, "p (b n) -> p b n", b=2)
        attn_tok = carve(256, BF16)
        mtmp = carve(1024, F32, "p (a n) -> p a n", a=4)
        mT = carve(1024, BF16, "p (k n) -> p k n", k=8)
        xn2b2 = [carve(512, BF16) for _ in range(2)]
        attnT = carve(512, BF16, "p (c n) -> p c n", c=4)
        rsm = carve(1024, F32)
        A_END = off[0]

        y32 = RA.rearrange("p (c n) -> p c n", c=4)
        xn2_ = [RA, RB]
        ybf = RB[:, 0:512].bitcast(BF16).rearrange("p (c n) -> p c n", c=4)
        ysq = RB[:, 512:1024].bitcast(BF16).rearrange("p (c n) -> p c n", c=4)
        cT = RB[:, 512:1024].bitcast(BF16).rearrange("p (c n) -> p c n", c=4)
        ln_mean = RC[:, 0:256]
        ln_rstd = RC[:, 256:512]
        ln_tmp = RC[:, 512:768]
        ln_t1 = RC[:, 768:1024]
        ht = RC
        PT = RD.bitcast(BF16).rearrange("p (g c n) -> p g c n", g=4, c=4)
        xn2T_ = [RD.rearrange("p (k n) -> p k n", k=8), mtmp.rearrange("p a n -> p (a n)").rearrange("p (k n) -> p k n", k=8)]

        MEMSET(DVE, onesf[:], 1.0, ["onesf"])
        MEMSET(DVE, onesb[:], 1.0, ["onesb"])
        P.add(POOL, lambda e: e.affine_select(out=identb[:], in_=onesf[:], pattern=[[-1, 128]], compare_op=ALU.is_equal,
                                              fill=0.0, base=0, channel_multiplier=1), ["onesf"], ["identb"])
        P.add(POOL, lambda e: e.affine_select(out=identf[:], in_=onesf[:], pattern=[[-1, 128]], compare_op=ALU.is_equal,
                                              fill=0.0, base=0, channel_multiplier=1), ["onesf"], ["identf"])
        P.add(POOL, lambda e: e.affine_select(out=ustrict[:], in_=onesf[:], pattern=[[1, 128]], compare_op=ALU.is_gt,
                                              fill=0.0, base=0, channel_multiplier=-1), ["onesf"], ["ustrict"])
        P.add(POOL, lambda e: e.iota(ebase[:], [[CAP, N_EXP]], base=0, channel_multiplier=0,
                                     allow_small_or_imprecise_dtypes=True), (), ["ebase"])
        for kb in range(2):
            P.add(POOL, lambda e, kb=kb: e.iota(relt[:, kb, :], [[1, 128]], base=128 * (1 - kb), channel_multiplier=-1,
                                                allow_small_or_imprecise_dtypes=True), (), ["relt"])
        P.add(POOL, lambda e: e.affine_select(out=amask[:, 0, :], in_=onesf[:], pattern=[[-1, 128]], compare_op=ALU.is_gt,
                                              fill=0.0, base=0, channel_multiplier=1), ["onesf"], ["amask"])
        P.add(POOL, lambda e: e.affine_select(out=amask[:, 1, :], in_=onesf[:], pattern=[[1, 128]], compare_op=ALU.is_ge,
                                              fill=0.0, base=0, channel_multiplier=-1), ["onesf"], ["amask"])
        win_v = w_in_d.rearrange("(k p) n -> p k n", p=128)

        def win_load(d0, s0, n):
            for kh in range(2):
                DMA(POOL, wb_in[:, 4 * kh:4 * kh + 4, d0:d0 + n], win_v[:, 4 * kh:4 * kh + 4, s0:s0 + n], [],
                    [("wb_in", d0, kh)])

        def win_res(col):
            if 1024 <= col < 1536:
                c_ = (col - 1024) // 128
                return [("wb_q", c_, 0), ("wb_q", c_, 1)]
            for d0, n in ((0, 512), (512, 512), (1536, 128), (3712, 128), (1664, 1024), (2688, 1024)):
                if d0 <= col < d0 + n:
                    return [("wb_in", d0, 0), ("wb_in", d0, 1)]
            raise ValueError(col)

        DMA(SP, gmixT[:], g_mix_d.ap()[0].rearrange("(c p) -> p c", p=128), [], ["gmixT"], allow_slow_non_contiguous=True)
        win_load(0, 0, 512)
        win_load(512, 512, 512)
        for c in range(4):
            for two in range(2):
                src0 = 1024 + 64 * (4 * two + c)
                DMA(POOL, wb_in[:, :, 1024 + 128 * c + 64 * two:1024 + 128 * c + 64 * two + 64],
                    win_v[:, :, src0:src0 + 64], [], [("wb_q", c, two)])
        win_load(1536, 1536, 128)
        win_load(3712, 1664, 128)
        win_load(1664, 1792, 1024)
        win_load(2688, 2816, 1024)
        DMA(POOL, wcb, wc_d.rearrange("(k p) n -> p k n", p=128), [], ["wcb"])
        DMA(POOL, wab, wa_d.rearrange("(k p) n -> p k n", p=128), [], ["wab"])
        for kh in range(2):
            DMA(POOL, wob[:, 4 * kh:4 * kh + 4, :], wo_d.rearrange("(k p) n -> p k n", p=128)[:, 4 * kh:4 * kh + 4, :],
                [], [("wob", kh)])
        MEMSET(DVE, cum[:], 0.0, ["cum"])
        MEMSET(DVE, epsc[:], EPS, ["epsc"])
        MEMSET(DVE, vtok_raw, 1.0, ["vtok0", "vtok1", "vtok2"])
        MEMSET(DVE, vTs, 0.0, ["vTs"])
        DMA(SP, gffn_bc[:], g_ffn_d[0].partition_broadcast(128), [], ["gffn_bc"])
        DMA(SP, bdw[:], b_dw_d[0].rearrange("(c p) -> p c", p=128), [], ["bdw"], allow_slow_non_contiguous=True)
        DMA(SP, lng[:], lng_d[0].rearrange("(c p) -> p c", p=128), [], ["lng"], allow_slow_non_contiguous=True)
        DMA(SP, lnb[:], lnb_d[0].rearrange("(c p) -> p c", p=128), [], ["lnb"], allow_slow_non_contiguous=True)
        DMA(SP, wdw_raw[:], w_dw_d[0].rearrange("j o c -> j (o c)"), [], ["wdw_raw"])
        DMA(SP, esink[:], sinks_d[0].partition_broadcast(128), [], ["esink"])
        DMA(SP, rbias[:, 0:4], bgrp_d[0].partition_broadcast(128), [], ["rbias"])
        DMA(SP, rbias[:, 4:36], bexp_d[0].partition_broadcast(128), [], ["rbias2"])
        DMA(SP, wr[:, :, 0:4], wgrp_d.rearrange("(k p) n -> p k n", p=128), [], ["wr"])
        DMA(SP, wr[:, :, 4:36], wexp_d.rearrange("(k p) n -> p k n", p=128), [], ["wr2"])
        ACTV(esink[:], esink[:], AF.Exp, ["esink"], ["esink"])
        k_, bk, br = nb()
        for c in range(4):
            TR32(bk[:, c * 32:c * 32 + CW], wdw_raw[:, c * 128:(c + 1) * 128], identf[0:CW, 0:CW],
                 ["wdw_raw", "identf"], [br])
        CP(DVE, wdwT[:], bk[:, 0:128].rearrange("p (c j) -> p c j", c=4)[:, :, 0:CW], [br], ["wdwT"])
        for c in range(4):
            for hf in range(2):
                sl = slice(64 * hf, 64 * hf + 64)
                TT(DVE, D64[sl, c, :, :], identb[sl, 64 * hf:64 * hf + 64].unsqueeze(1).broadcast_to([64, CW, 64]),
                   wdwT[sl, c, :].unsqueeze(2).broadcast_to([64, CW, 64]), ALU.mult, ["identb", "wdwT"],
                   [("D64", c, 0), ("D64", c, 1)])
        TS(DVE, relt[:], relt[:], 0.0, 128.0, ALU.max, ALU.min, ["relt"], ["relt"])
        def emf_res(h):
            if h < 4:
                return [("RA", h)]
            hh = h - 4
            return [("RB%d" % (hh // 2), 2 * (hh % 2)), ("RB%d" % (hh // 2), 2 * (hh % 2) + 1)]

        for h in range(8):
            ACTV(emf[:, h, :, :], relt[:], AF.Exp, ["relt"], emf_res(h), scale=-(2.0 ** (-(h + 1))))
        for kv in range(2):
            for kb in range(2):
                TT(DVE, EM[:, 2 * kv + kb, :, :], emf[:, 4 * kv:4 * kv + 4, kb, :],
                   amask[:, kb, :].unsqueeze(1).broadcast_to([128, 4, 128]), ALU.mult,
                   sum([emf_res(h_) for h_ in range(4 * kv, 4 * kv + 4)], []) + ["amask"], ["EM"])

        def prep(s):
            for i in range(TPS):
                g = s * TPS + i
                slot = g % 4
                DMA(SP, xt[:, slot, :], x_d[g * 128:(g + 1) * 128, :], [], [("xt", slot)])
                ACTV(xsb[:, i, :], xt[:, slot, :], AF.Square, [("xt", slot)], [("xsb", i), ("ss", i)],
                     accum=ss[:, i:i + 1])
                RSTD(rstd[:, i:i + 1], ss[:, i:i + 1], 1.0 / D_MODEL, [("ss", i)], [("rstd", i)])
                TS(DVE, xsb[:, i, :], xt[:, slot, :], rstd[:, i:i + 1], None, ALU.mult, None,
                   [("xt", slot), ("rstd", i)], [("xsb", i)])

        evac_flip = [0]

        def transposes(s):
            for i in range(TPS):
                for hf in range(2):
                    k_, bk, br = nb()
                    for q in range(4):
                        kk = 4 * hf + q
                        MM(bk[:, q * 128:(q + 1) * 128], xsb[:, i, kk * 128:(kk + 1) * 128], identb[:], True, True,
                           [("xsb", i), "identb"], [br])
                    for q in range(4):
                        kk = 4 * hf + q
                        evac_flip[0] ^= 1
                        if evac_flip[0]:
                            ACTV(xnT[:, kk, i * 128:(i + 1) * 128], bk[:, q * 128:(q + 1) * 128], AF.Copy,
                                 [br, "gmixT"], ["xnT"], scale=gmixT[:, kk:kk + 1])
                        else:
                            TS(DVE, xnT[:, kk, i * 128:(i + 1) * 128], bk[:, q * 128:(q + 1) * 128],
                               gmixT[:, kk:kk + 1], None, ALU.mult, None, [br, "gmixT"], ["xnT"])

        def proj_chunk(j):
            k_, bk, br = nb()
            wres = win_res(j * 128)
            for k in range(8):
                MM(bk[:, 0:NT], wb_in[:, k, j * 128:(j + 1) * 128], xnT[:, k, :], k == 0, k == 7, wres + ["xnT"], [br])
            return bk, br

        def inproj_glu(s):
            if s > 0:
                CP(POOL, vTs[:, :, 0:30], vTs[:, :, NT:NT + 30], ["vTs"], ["vTs"])
                CP(POOL, kTr[:, 0:128], kTr[:, NT:NT + 128], ["kTr"], ["kTr"])
                CP(POOL, vtok[:, 0, :, :], vtok[:, TPS, :, :], ["vtok%d" % TPS], ["vtok0"])
            for j in range(4):
                ba, bar = proj_chunk(j)
                bg, bgr = proj_chunk(4 + j)
                ACTV(sig, bg[:, 0:NT], AF.Sigmoid, [bgr], ["sig"])
                TT(DVE, vTs[:, j, 30:30 + NT], ba[:, 0:NT], sig, ALU.mult, [bar, "sig"], ["vTs"])

        def inproj_qkv(s):
            for c in range(4):
                bq, bqr = proj_chunk(8 + c)
                CP(ACT, qTs[:, c, :], bq[:, 0:NT], [bqr], ["qTs"])
            bk_, bkr = proj_chunk(12)
            CP(DVE, kTr[:, 128:128 + NT], bk_[:, 0:NT], [bkr], ["kTr"])
            wres = win_res(3712)
            for i in range(TPS):
                k_, bv, bvr = nb()
                for k in range(8):
                    MM(bv[:, 0:128], xnT[:, k, i * 128:(i + 1) * 128], wb_in[:, k, 3712:3840], k == 0, k == 7,
                       ["xnT"] + wres, [bvr])
                CP(ACT, vtok[:, 1 + i, :, 0:64], bv[:, 0:128].rearrange("p (k d) -> p k d", k=2), [bvr],
                   ["vtok%d" % (1 + i)])

        def conv_chunk(s, c):
            banks = []
            for hf in range(2):
                k_, bk, br = nb()
                banks.append((bk, br))
            for j in range(CW):
                for hf in range(2):
                    bk, br = banks[hf]
                    MM(bk[64 * hf:64 * hf + 64, 0:NT], D64[64 * hf:64 * hf + 64, c, j, :],
                       vTs[64 * hf:64 * hf + 64, c, j:j + NT], j == 0, j == CW - 1, [("D64", c, 0), ("D64", c, 1), "vTs"], [br])
            for hf in range(2):
                bk, br = banks[hf]
                sl = slice(64 * hf, 64 * hf + 64)
                ACTV(y32[sl, c, :], bk[sl, 0:NT], AF.Identity, [br, "bdw"], [("RA", c)], bias=bdw[sl, c:c + 1])
                ACTV(ysq[sl, c, :], bk[sl, 0:NT], AF.Square, [br, "bdw"], [("RB1", c)], bias=bdw[sl, c:c + 1])
            CP(DVE, ybf[:, c, :], y32[:, c, :], [("RA", c)], [("RB0", c)])

        def ln(s):
            k1, b1, b1r = nb()
            k2, b2, b2r = nb()
            for c in range(4):
                MM(b1[:, 0:NT], onesb[:], ybf[:, c, :], c == 0, c == 3, ["onesb", ("RB0", c)], [b1r])
            for c in range(4):
                MM(b2[:, 0:NT], onesb[:], ysq[:, c, :], c == 0, c == 3, ["onesb", ("RB1", c)], [b2r])
            TS(DVE, ln_mean, b1[:, 0:NT], 1.0 / CONV_CH, None, ALU.mult, None, [b1r], ["RC0"])
            TT(DVE, ln_tmp, ln_mean, ln_mean, ALU.mult, ["RC0"], ["RC2"])
            STT(DVE, ln_rstd, b2[:, 0:NT], 1.0 / CONV_CH, ln_tmp, ALU.mult, ALU.subtract, [b2r, "RC2"], ["RC1"])
            RSTD(ln_rstd, ln_rstd, 1.0, ["RC1"], ["RC1"])
            for c in range(4):
                TT(DVE, ln_t1, y32[:, c, :], ln_mean, ALU.subtract, [("RA", c), "RC0"], ["RC3"])
                TT(DVE, ln_t1, ln_t1, ln_rstd, ALU.mult, ["RC3", "RC1"], ["RC3"])
                ACTV(cT[:, c, :], ln_t1, AF.Silu, ["RC3", "lng", "lnb", ("RB1", c)], [("RB1", c)],
                     scale=lng[:, c:c + 1], bias=lnb[:, c:c + 1])

        def attn_qk(s, i):
            n = s * TPS + i
            qb = i * 128
            kbs = [1] if n == 0 else [0, 1]
            for kv in range(2):
                sl = slice(64 * kv, 64 * kv + 64)
                for kb in kbs:
                    k_, bk, br = nb()
                    kc = qb + 128 * kb
                    MM(bk[:, :].rearrange("p (c q) -> p c q", c=4), kTr[sl, kc:kc + 128], qTs[sl, :, qb:qb + 128],
                       True, True, ["kTr", "qTs"], [br])
                    pe_ = ("pexp", kb)
                    ACTV(pexp[:, kb, :], bk[:, :], AF.Exp, [br], [pe_], scale=0.125)
                    TT(DVE, PT[:, 2 * kv + kb, :, :], pexp[:, kb, :].rearrange("p (c q) -> p c q", c=4),
                       EM[:, 2 * kv + kb, :, :], ALU.mult, [pe_, "EM"], [("RD", 2 * kv + kb)])

        def attn_pv(s, i):
            n = s * TPS + i
            kbs = [1] if n == 0 else [0, 1]
            for kv in range(2):
                k_, bo, bor = nb()
                ov = bo[:, 0:260].rearrange("p (c d) -> p c d", c=4)
                for c in range(4):
                    for kb in kbs:
                        MM(ov[:, c, :], PT[:, 2 * kv + kb, c, :], vtok[:, i + kb, kv, :], kb == kbs[0], kb == 1,
                           [("RD", 2 * kv + kb), "vtok%d" % (i + kb)], [bor])
                den = rsm[:, 4 * kv:4 * kv + 4]
                dr = ("den", kv)
                TT(DVE, den, ov[:, :, 64], esink[:, 4 * kv:4 * kv + 4], ALU.add, [bor, "esink"], [dr])
                P.add(DVE, lambda e, den=den: e.reciprocal(out=den, in_=den), [dr], [dr])
                TT(DVE, attn_tok[:, 256 * kv:256 * kv + 256].rearrange("p (c d) -> p c d", c=4), ov[:, :, 0:64],
                   den.unsqueeze(2).broadcast_to([128, 4, 64]), ALU.mult, [bor, dr], [("attn_tok", kv)])

        def attn_T(s, i):
            qb = i * 128
            k_, bt, btr = nb()
            for c in range(4):
                MM(bt[:, c * 128:(c + 1) * 128], attn_tok[:, c * 128:(c + 1) * 128], identb[:], True, True,
                   [("attn_tok", c // 2), "identb"], [btr])
            CP(ACT, attnT[:, :, qb:qb + 128], bt[:, :].rearrange("p (c q) -> p c q", c=4), [btr], ["attnT"])

        def merge(s):
            sg_c = [mtmp[:, 0, :], mtmp[:, 2, :]]
            sg_a = [mtmp[:, 1, :], mtmp[:, 3, :]]
            sg_cr = ["sgc", "t1"]
            sg_ar = ["sga", "t2"]
            t2s = [mtmp2[:, jj, :] for jj in range(4)]
            t2r = [("t2s", jj) for jj in range(4)]
            for i in range(2):
                v_ = xn2b2[i].bitcast(F32).rearrange("p (a n) -> p a n", a=2)
                t2s += [v_[:, 0, :], v_[:, 1, :]]
                t2r += [("xn2b", i), ("xn2b", i)]
            for j in range(8):
                k_, bb, bbr = nb()
                for k in range(4):
                    MM(bb[:, 0:NT], wab[:, k, j * 128:(j + 1) * 128], attnT[:, k, :], k == 0, k == 3, ["wab", "attnT"], [bbr])
                bd, bdr = proj_chunk(21 + j)
                ACTV(sg_a[j % 2], bd[:, 0:NT], AF.Sigmoid, [bdr], [sg_ar[j % 2]])
                TT(DVE, t2s[j], bb[:, 0:NT], sg_a[j % 2], ALU.mult, [bbr, sg_ar[j % 2]], [t2r[j]])
            for j in range(8):
                k_, ba, bar = nb()
                for k in range(4):
                    MM(ba[:, 0:NT], wcb[:, k, j * 128:(j + 1) * 128], cT[:, k, :], k == 0, k == 3,
                       ["wcb", ("RB1", k)], [bar])
                bc, bcr = proj_chunk(13 + j)
                ACTV(sg_c[j % 2], bc[:, 0:NT], AF.Sigmoid, [bcr], [sg_cr[j % 2]])
                TT(DVE, t1buf[:, j % 2, :], ba[:, 0:NT], sg_c[j % 2], ALU.mult, [bar, sg_cr[j % 2]], [("t1b", j % 2)])
                TT(POOL, mT[:, j, :], t1buf[:, j % 2, :], t2s[j], ALU.add, [("t1b", j % 2), t2r[j]], [("mT", j)])

        RAr = [("RA", c) for c in range(4)]
        RBr = [("RB0", c) for c in range(4)] + [("RB1", c) for c in range(4)]
        RCr = ["RC0", "RC1", "RC2", "RC3"]
        RDr = [("RD", q) for q in range(4)]
        MTr = ["sgc", "sga", "t1", "t2"]
        xn2_res = [RAr, RBr]
        hts = [ht, ht2[:]]
        htr = [RCr, ["ht2"]]
        xn2T_res = [RDr, MTr]

        def tail_wout(s):
            mres = [("mT", j) for j in range(8)]
            for i in range(TPS):
                g = s * TPS + i
                slot = g % 4
                for hf in range(2):
                    k_, bk, br = nb()
                    for k in range(8):
                        MM(bk[:, :], mT[:, k, i * 128:(i + 1) * 128], wob[:, k, hf * 512:(hf + 1) * 512], k == 0, k == 7,
                           mres + [("wob", 0), ("wob", 1)], [br])
                    TT(DVE, hts[i][:, hf * 512:(hf + 1) * 512], bk[:, :], xt[:, slot, hf * 512:(hf + 1) * 512], ALU.add,
                       [br, ("xt", slot)], htr[i])
                DMA(SP, hbuf_d[g * 128:(g + 1) * 128, :], hts[i], htr[i], [("hbuf", g)])

        def tail_wout_b(s):
            for i in range(TPS):
                xb = xn2b2[i]
                ACTV(xb, hts[i], AF.Square, htr[i], [("xn2b", i), ("ss2", i)], accum=ss[:, 2 + i:3 + i])
                RSTD(rstd[:, 2 + i:3 + i], ss[:, 2 + i:3 + i], 1.0 / D_MODEL, [("ss2", i)], [("rstd2", i)])
                STT(DVE, xn2_[i], hts[i], rstd[:, 2 + i:3 + i], gffn_bc[:], ALU.mult, ALU.mult,
                    htr[i] + [("rstd2", i), "gffn_bc"], xn2_res[i])
                CP(ACT, xb, xn2_[i], xn2_res[i], [("xn2b", i)])

        def tail_rtr_T(s):
            for i in range(TPS):
                for hf in range(2):
                    k_, bk, br = nb()
                    for q in range(4):
                        kk = 4 * hf + q
                        TR32(bk[:, q * 128:(q + 1) * 128], xn2_[i][:, kk * 128:(kk + 1) * 128], identf[:],
                             xn2_res[i] + ["identf"], [br])
                    if hf == 0:
                        CP(ACT, xn2T_[i][:, 0:4, :], bk[:, :].rearrange("p (k n) -> p k n", k=4), [br], xn2T_res[i][0:2])
                    else:
                        CP(DVE, xn2T_[i][:, 4:8, :], bk[:, :].rearrange("p (k n) -> p k n", k=4), [br], xn2T_res[i][2:4])

        def rfields(i):
            o = 8 + 500 * i
            f = {}
            names = [("lg", 36), ("gmax", 1), ("ngmax", 1), ("gexp", 4), ("gsum", 1), ("pgrp", 1), ("gone", 4), ("pen", 4),
                     ("em", 32), ("m1", 1), ("one1", 32), ("em2", 32), ("m2", 1), ("one2", 32), ("dlt", 1), ("w1", 1),
                     ("ind", 32), ("indb", 16), ("pos", 32), ("tmp", 32), ("d12", 2)]
            for nm, w_ in names:
                f[nm] = rsm[:, o:o + w_]
                o += w_
            assert o <= 8 + 500 * (i + 1)
            return f

        tail_state = {}

        def tail_logits(s):
            for i in range(TPS):
                k_, bl, blr = nb()
                for k in range(8):
                    MM(bl[:, 0:36], xn2T_[i][:, k, :], wr[:, k, :], k == 0, k == 7, xn2T_res[i] + ["wr", "wr2"], [blr])
                TT(DVE, rfields(i)["lg"], bl[:, 0:36], rbias[:], ALU.add, [blr, "rbias", "rbias2"], [("lg", i)])

        def tail_route(s, i):
            if True:
                g = s * TPS + i
                F = rfields(i)
                R = lambda nm: (nm, i)
                lg, gmax, ngmax, gexp, gsum, pgrp = F["lg"], F["gmax"], F["ngmax"], F["gexp"], F["gsum"], F["pgrp"]
                gone, pen, em, m1, one1, em2, m2, one2 = F["gone"], F["pen"], F["em"], F["m1"], F["one1"], F["em2"], F["m2"], F["one2"]
                dlt, w1, ind = F["dlt"], F["w1"], F["ind"]
                indb = F["indb"].bitcast(BF16)
                RED(DVE, gmax, lg[:, 0:4], ALU.max, [R("lg")], [R("gmax")])
                TS(DVE, ngmax, gmax, -1.0, None, ALU.mult, None, [R("gmax")], [R("ngmax")])
                ACTV(gexp, lg[:, 0:4], AF.Exp, [R("lg"), R("ngmax")], [R("gexp"), R("gsum")], bias=ngmax, accum=gsum)
                P.add(DVE, lambda e, pgrp=pgrp, gsum=gsum: e.reciprocal(out=pgrp, in_=gsum), [R("gsum")], [R("pgrp")])
                TS(DVE, gone, lg[:, 0:4], gmax, None, ALU.is_equal, None, [R("lg"), R("gmax")], [R("gone")])
                TS(DVE, pen, gone, -1.0, BIG, ALU.add, ALU.mult, [R("gone")], [R("pen")])
                TT(DVE, em.rearrange("p (g j) -> p g j", g=4), lg[:, 4:36].rearrange("p (g j) -> p g j", g=4),
                   pen.unsqueeze(2).broadcast_to([128, 4, 8]), ALU.add, [R("lg"), R("pen")], [R("em")])
                RED(DVE, m1, em, ALU.max, [R("em")], [R("m1")])
                TS(DVE, one1, em, m1, None, ALU.is_equal, None, [R("em"), R("m1")], [R("one1")])
                STT(DVE, em2, one1, -BIG, em, ALU.mult, ALU.add, [R("one1"), R("em")], [R("em2")])
                RED(DVE, m2, em2, ALU.max, [R("em2")], [R("m2")])
                TS(DVE, one2, em2, m2, None, ALU.is_equal, None, [R("em2"), R("m2")], [R("one2")])
                TT(DVE, dlt, m2, m1, ALU.subtract, [R("m1"), R("m2")], [R("dlt")])
                ACTV(dlt, dlt, AF.Exp, [R("dlt")], [R("dlt")])
                TS(DVE, dlt, dlt, 1.0, None, ALU.add, None, [R("dlt")], [R("dlt")])
                P.add(DVE, lambda e, w1=w1, dlt=dlt: e.reciprocal(out=w1, in_=dlt), [R("dlt")], [R("w1")])
                TT(DVE, wall[:, g, 0:1], w1, pgrp, ALU.mult, [R("w1"), R("pgrp")], [("wall", g)])
                TT(DVE, wall[:, g, 1:2], pgrp, wall[:, g, 0:1], ALU.subtract, [R("pgrp"), ("wall", g)], [("wall", g)])
                TT(DVE, ind, one1, one2, ALU.add, [R("one1"), R("one2")], [R("ind")])
                CP(DVE, indb, ind, [R("ind")], [R("indb")])

        def tail_pos(s):
            for i in range(TPS):
                g = s * TPS + i
                F = rfields(i)
                R = lambda nm: (nm, i)
                ind, pos, tmp, one1, one2, d12 = F["ind"], F["pos"], F["tmp"], F["one1"], F["one2"], F["d12"]
                indb = F["indb"].bitcast(BF16)
                cb_ = cumb2[:, i, :]
                CP(DVE, cb_, cum[:], ["cum"], [("cumb", i)])
                k_, bp, bpr = nb()
                MM(bp[:, 0:32], ustrict[:], indb, True, False, ["ustrict", R("indb")], [bpr])
                MM(bp[:, 0:32], onesb[:], cb_, False, True, ["onesb", ("cumb", i)], [bpr])
                TT(DVE, cum[:], cum[:], ind, ALU.add, ["cum", R("ind")], ["cum"])
                TS(DVE, pos, bp[:, 0:32], float(CAP - 1), None, ALU.min, None, [bpr], [R("pos")])
                TT(DVE, pos, pos, ebase[:], ALU.add, [R("pos"), "ebase"], [R("pos")])
                TT(DVE, tmp, pos, one1, ALU.mult, [R("pos"), R("one1")], [R("tmp")])
                RED(DVE, d12[:, 0:1], tmp, ALU.add, [R("tmp")], [R("d1f")])
                TT(DVE, tmp, pos, one2, ALU.mult, [R("pos"), R("one2")], [R("tmp")])
                RED(DVE, d12[:, 1:2], tmp, ALU.add, [R("tmp")], [R("d2f")])
                CP(DVE, destall[:, g, :], d12, [R("d1f"), R("d2f")], [("dest", g)])
                zres = [("xs_z", r0) for r0 in range(0, N_EXP * CAP, 1024)]
                for k in range(2):
                    P.add(POOL, lambda e, g=g, k=k, i=i: e.indirect_dma_start(
                        out=xs_d, out_offset=bass.IndirectOffsetOnAxis(ap=destall[:, g, k:k + 1], axis=0),
                        in_=xn2b2[i], in_offset=None), [("xn2b", i), ("dest", g)] + zres, [("xs_scr", g, k)], dma=True)

        def precast(e):
            srcs = (wgate_d[e].rearrange("(k p) n -> p k n", p=128), wup_d[e].rearrange("(k p) n -> p k n", p=128),
                    wdown_d[e].rearrange("(k p) n -> p k n", p=128))
            for m_, src in enumerate(srcs):
                kk = src.shape[1]
                dst = wbf_d[e, m_].rearrange("p (k n) -> p k n", k=kk)
                DMA(POOL, dst, src, [], [("wbf", e, m_)])

        def zero_fill():
            MEMSET(DVE, ztile[:], 0.0, ["ztile"])
            for r0 in range(0, N_EXP * CAP, 1024):
                DMA(POOL, xs_d[r0:r0 + 1024, :].rearrange("(b p) n -> p b n", p=128),
                    ztile[:].unsqueeze(1).broadcast_to([128, 8, D_MODEL]), ["ztile"], [("xs_z", r0)])

        prep(0)
        for s in range(NST):
            transposes(s)
            if s > 0:
                tail_wout(s - 1)
            inproj_glu(s)
            if s > 0:
                tail_wout_b(s - 1)
            inproj_qkv(s)
            if s == 0:
                zero_fill()
            if s + 1 < NST:
                prep(s + 1)
            if s > 0:
                tail_rtr_T(s - 1)
            conv_chunk(s, 0)
            if s > 0:
                tail_logits(s - 1)
                tail_route(s - 1, 0)
            attn_qk(s, 0)
            conv_chunk(s, 1)
            if s > 0:
                tail_route(s - 1, 1)
            attn_pv(s, 0)
            attn_qk(s, 1)
            conv_chunk(s, 2)
            attn_T(s, 0)
            attn_pv(s, 1)
            if s > 0:
                tail_pos(s - 1)
            conv_chunk(s, 3)
            attn_T(s, 1)
            ln(s)
            merge(s)
            for e_ in range(s * (N_EXP // NST), (s + 1) * (N_EXP // NST)):
                precast(e_)
        tail_wout(NST - 1)
        tail_wout_b(NST - 1)
        tail_rtr_T(NST - 1)
        tail_logits(NST - 1)
        tail_route(NST - 1, 0)
        tail_route(NST - 1, 1)
        tail_pos(NST - 1)

        P.barrier()
        off[0] = 0
        NWB = 4
        wexp = []
        for b in range(NWB):
            wg = carve(2048, BF16, "p (k n) -> p k n", k=8)
            wu = carve(2048, BF16, "p (k n) -> p k n", k=8)
            wd = carve(2048, BF16, "p (k n) -> p k n", k=4)
            wexp.append((wg, wu, wd))
        NXS = 3
        xsr = [carve(2048, BF16, "p (b n) -> p b n", b=4) for _ in range(NXS)]
        xsT = [carve(4 * CAP, BF16, "p (k n) -> p k n", k=8) for _ in range(2)]
        sgt = [carve(CAP, F32) for _ in range(2)]
        hT = [carve(2 * CAP, BF16, "p (f n) -> p f n", f=4) for _ in range(2)]
        NYB = 4
        ysb = [carve(512, BF16) for _ in range(NYB)]
        B_END = off[0]

        def load_weights(e):
            b = e % NWB
            wg, wu, wd = wexp[b]
            DMA(SP, wg.rearrange("p k n -> p (k n)"), wbf_d[e, 0], [], [("wg", b, 0), ("wg", b, 1)])
            DMA(SP, wu.rearrange("p k n -> p (k n)"), wbf_d[e, 1], [], [("wu", b, 0), ("wu", b, 1)])
            DMA(SP, wd.rearrange("p k n -> p (k n)"), wbf_d[e, 2], [], [("wd", b, 0), ("wd", b, 1)])

        def load_xs(e):
            b = e % NXS
            DMA(SP, xsr[b][:, 0:3, :], xs_d[e * CAP:e * CAP + 384, :].rearrange("(b p) n -> p b n", p=128), [], [("xsr", b)])
            DMA(SP, xsr[b][0:64, 3, :], xs_d[e * CAP + 384:(e + 1) * CAP, :], [], [("xsrt", b)])

        def exp_transposes(e):
            b = e % NXS
            xT = xsT[e % 2]
            for blk, (s0, sz) in enumerate(BLKS):
                for hf in range(2):
                    k_, bk, br = nb()
                    for q in range(4):
                        kk = 4 * hf + q
                        MM(bk[:, q * sz:(q + 1) * sz], xsr[b][0:sz, blk, kk * 128:(kk + 1) * 128], identb[0:sz, 0:sz], True, True,
                           [("xsr", b), ("xsrt", b), "identb"], [br])
                    dst = xT[:, 4 * hf:4 * hf + 4, s0:s0 + sz]
                    src = bk[:, 0:4 * sz].rearrange("p (k n) -> p k n", k=4)
                    if hf == 0:
                        CP(ACT, dst, src, [br], [("xsT", e % 2)])
                    else:
                        CP(DVE, dst, src, [br], [("xsT", e % 2)])

        def exp_gateup(e):
            b = e % NWB
            wg, wu, wd = wexp[b]
            xT = xsT[e % 2]
            h_ = hT[e % 2]
            for f in range(4):
                k_, bg, bgr = nb()
                k_, bu, bur = nb()
                for k in range(8):
                    MM(bg[:, 0:CAP], wg[:, k, f * 128:(f + 1) * 128], xT[:, k, :], k == 0, k == 7,
                       [("wg", b, 0), ("wg", b, 1), ("xsT", e % 2)], [bgr])
                for k in range(8):
                    MM(bu[:, 0:CAP], wu[:, k, f * 128:(f + 1) * 128], xT[:, k, :], k == 0, k == 7,
                       [("wu", b, 0), ("wu", b, 1), ("xsT", e % 2)], [bur])
                sg_ = sgt[f % 2]
                ACTV(sg_, bg[:, 0:CAP], AF.Silu, [bgr], [("sgt", f % 2)])
                TT(DVE, h_[:, f, :], bu[:, 0:CAP], sg_, ALU.mult, [bur, ("sgt", f % 2)], [("hT", e % 2, f)])

        ycount = [0]

        def exp_down(e):
            b = e % NWB
            wg, wu, wd = wexp[b]
            h_ = hT[e % 2]
            for blk, (s0, sz) in enumerate(BLKS):
                yb = ysb[ycount[0] % NYB]
                yr = ("ysb", ycount[0] % NYB)
                ycount[0] += 1
                for hf in range(2):
                    k_, bk, br = nb()
                    for f in range(4):
                        MM(bk[0:sz, :], h_[:, f, s0:s0 + sz], wd[:, f, hf * 512:(hf + 1) * 512], f == 0, f == 3,
                           [("hT", e % 2, f_) for f_ in range(4)] + [("wd", b, 0), ("wd", b, 1)], [br])
                    if hf == 0:
                        CP(ACT, yb[0:sz, 0:512], bk[0:sz, :], [br], [yr])
                    else:
                        CP(DVE, yb[0:sz, 512:1024], bk[0:sz, :], [br], [yr])
                r0 = e * CAP + s0
                DMA(SP, ys_d[r0:r0 + sz, :], yb[0:sz, :], [yr], [("ys_scr", e, blk)])

        for e in range(3):
            load_weights(e)
            load_xs(e) if e < 3 else None
        exp_transposes(0)
        for e in range(N_EXP):
            if e + 3 < N_EXP:
                load_weights(e + 3)
            exp_gateup(e)
            if e + 1 < N_EXP:
                exp_transposes(e + 1)
            if e + 3 < N_EXP:
                load_xs(e + 3)
            exp_down(e)

        P.barrier()
        off[0] = 0
        gfin_bc = carve(1024, F32)
        NCB = 4
        cb = []
        for b in range(NCB):
            cb.append(dict(h=carve(1024, F32), y1=carve(512, BF16), y2=carve(512, BF16), o=carve(1024, F32)))
        DMA(SP, gfin_bc, g_fin_d.partition_broadcast(128), [], ["gfin_bc"])

        def c_load(g):
            b = g % NCB
            B_ = cb[b]
            DMA(SP, B_["h"], hbuf_d[g * 128:(g + 1) * 128, :], [], [("ch", b)])
            for k, key in ((0, "y1"), (1, "y2")):
                P.add(POOL, lambda e, g=g, k=k, dstt=B_[key]: e.indirect_dma_start(
                    out=dstt, out_offset=None, in_=ys_d,
                    in_offset=bass.IndirectOffsetOnAxis(ap=destall[:, g, k:k + 1], axis=0)),
                    [], [("c" + key, b)], dma=True)

        for g in range(NCB - 1):
            c_load(g)
        for g in range(NTILE):
            b = g % NCB
            B_ = cb[b]
            if g + NCB - 1 < NTILE:
                c_load(g + NCB - 1)
            STT(DVE, B_["h"], B_["y1"], wall[:, g, 0:1], B_["h"], ALU.mult, ALU.add, [("cy1", b), ("ch", b)], [("ch", b)])
            STT(DVE, B_["h"], B_["y2"], wall[:, g, 1:2], B_["h"], ALU.mult, ALU.add, [("cy2", b), ("ch", b)], [("ch", b)])
            ACTV(B_["o"], B_["h"], AF.Square, [("ch", b)], [("co", b), ("ss3", g % 2)], accum=ss[:, 5 + g % 2:6 + g % 2])
            RSTD(rstd[:, 5 + g % 2:6 + g % 2], ss[:, 5 + g % 2:6 + g % 2], 1.0 / D_MODEL, [("ss3", g % 2)], [("rstd3", g % 2)])
            STT(DVE, B_["o"], B_["h"], rstd[:, 5 + g % 2:6 + g % 2], gfin_bc, ALU.mult, ALU.mult,
                [("ch", b), ("rstd3", g % 2), "gfin_bc"], [("co", b)])
            DMA(SP, out_d[g * 128:(g + 1) * 128, :], B_["o"], [("co", b)], [("out", g)])

        P.emit(st)
        build_nc.stats = (P.stats, P.nwaits, A_END, B_END, off[0])
    return nc


_NC_CACHE = {}


def kernel(**inputs):
    x = np.ascontiguousarray(np.asarray(inputs["x"], dtype=np.float32))
    names = ["g_mix", "w_in", "w_dw", "b_dw", "ln_conv_g", "ln_conv_b", "sinks", "w_conv_out", "w_attn_out", "w_out",
             "g_ffn", "w_group", "b_group", "w_expert", "b_expert", "w_gate", "w_up", "w_down", "g_final"]
    shared = {n: np.ascontiguousarray(np.asarray(inputs[n], dtype=np.float32)) for n in names}
    if "nc" not in _NC_CACHE:
        _NC_CACHE["nc"] = build_nc()
    nc = _NC_CACHE["nc"]
    in_maps = []
    for c in range(8):
        m = dict(shared)
        m["x"] = x[c]
        in_maps.append(m)
    res = run_bass_kernel_spmd(nc, in_maps, core_ids=list(range(8)))
    return np.stack([np.asarray(r["out"], dtype=np.float32) for r in res.results], axis=0)
```

```python
import numpy as np
from contextlib import ExitStack
import concourse.bass as bass
import concourse.mybir as mybir
from concourse.bass_utils import run_bass_kernel_spmd

F32 = mybir.dt.float32
BF16 = mybir.dt.bfloat16
I32 = mybir.dt.int32
AF = mybir.ActivationFunctionType
ALU = mybir.AluOpType
AX = mybir.AxisListType

PE, ACT, DVE, POOL, SP = "pe", "act", "dve", "pool", "sp"

D_MODEL = 1024
SEQ = 4096
NTILE = SEQ // 128
NT = 256
TPS = NT // 128
NST = SEQ // NT
CONV_CH = 512
CW = 31
N_EXP = 32
CAP = 448
BLKS = [(0, 128), (128, 128), (256, 128), (384, 64)]
DFF = 512
EPS = 1e-6
BIG = 1.0e30
SEM_EPOCH = 20000


class Prog:
    def __init__(self, nc, n_dma_sems=8):
        self.nc = nc
        self.ops = []
        self.n_dma_sems = n_dma_sems
        self.last_w = {}
        self.readers = {}
        self.last_op = {}
        self.recent_dma = {}

    def add(self, eng, emit, reads=(), writes=(), dma=False):
        ops = self.ops
        i = len(ops)
        op = dict(eng=eng, emit=emit, dma=dma, deps=set(), sig=False)
        deps = set()
        for r in reads:
            w = self.last_w.get(r)
            if w is not None:
                deps.add((w, False))
        for wr in writes:
            w = self.last_w.get(wr)
            if w is not None:
                deps.add((w, False))
            for rd in self.readers.get(wr, ()):
                deps.add((rd, True))
        for r in reads:
            self.readers.setdefault(r, []).append(i)
        for wr in writes:
            self.last_w[wr] = i
            self.readers[wr] = []
        final = set()
        for d, war in deps:
            if d == i:
                continue
            p = ops[d]
            if not p["dma"] and not dma and p["eng"] == eng:
                if eng == PE:
                    continue
                if war:
                    continue
            final.add(d)
        op["deps"] = final
        for d in final:
            ops[d]["sig"] = True
        ops.append(op)
        self.last_op[eng] = i
        if dma:
            self.recent_dma.setdefault(eng, []).append(i)
            self.recent_dma[eng] = self.recent_dma[eng][-self.n_dma_sems:]
        return i

    def barrier(self):
        deps = set(self.last_op.values())
        for lst in self.recent_dma.values():
            deps.update(lst)
        for e in (PE, ACT, DVE, POOL, SP):
            i = len(self.ops)
            op = dict(eng=e, emit=None, dma=False, deps=set(deps), sig=False)
            self.ops.append(op)
            self.last_op[e] = i
        for d in deps:
            self.ops[d]["sig"] = True
        self.last_w = {}
        self.readers = {}

    def emit(self, stack):
        nc = self.nc
        ops = self.ops
        engs = [PE, ACT, DVE, POOL, SP]
        nsig = {e: sum(1 for o in ops if o["eng"] == e and o["sig"] and not o["dma"]) for e in engs}
        csem = {e: [stack.enter_context(nc.semaphore("c_%s%d" % (e, k)))
                    for k in range(nsig[e] // SEM_EPOCH + 1)] for e in engs}
        dsem = {e: [stack.enter_context(nc.semaphore("d_%s%d" % (e, k)))
                    for k in range(self.n_dma_sems)] for e in (ACT, POOL, SP)}
        ccount = {e: 0 for e in engs}
        dcount = {e: 0 for e in dsem}
        dval = {e: [0] * self.n_dma_sems for e in dsem}
        for op in ops:
            e = op["eng"]
            if op["dma"]:
                k = dcount[e] % self.n_dma_sems
                dcount[e] += 1
                op["prev_slot"] = (dsem[e][k], dval[e][k]) if dval[e][k] else None
                dval[e][k] += 16
                op["signal"] = (dsem[e][k], dval[e][k])
            elif op["sig"]:
                ep, v = divmod(ccount[e], SEM_EPOCH)
                ccount[e] += 1
                op["signal"] = (csem[e][ep], v + 1)
            else:
                op["signal"] = None
        per_eng = {e: [op for op in ops if op["eng"] == e] for e in engs}
        self.stats = {e: len(per_eng[e]) for e in engs}
        nwaits = {e: 0 for e in engs}

        def run(e, engine):
            waited = {}

            def wait(sem, val):
                key = id(sem)
                if waited.get(key, 0) >= val:
                    return
                waited[key] = val
                engine.wait_ge(sem, val)
                nwaits[e] += 1

            for op in per_eng[e]:
                need = {}
                for d in op["deps"]:
                    s, v = ops[d]["signal"]
                    k = id(s)
                    if k not in need or need[k][1] < v:
                        need[k] = (s, v)
                if op["dma"] and op["prev_slot"] is not None:
                    s, v = op["prev_slot"]
                    k = id(s)
                    if k not in need or need[k][1] < v:
                        need[k] = (s, v)
                for s, v in need.values():
                    wait(s, v)
                if op["emit"] is None:
                    if op["signal"] is not None:
                        engine.nop().then_inc(op["signal"][0], 1)
                    continue
                ins = op["emit"](engine)
                if op["signal"] is not None:
                    s, v = op["signal"]
                    ins.then_inc(s, 16 if op["dma"] else 1)
            if e in dsem:
                for k, s in enumerate(dsem[e]):
                    if dval[e][k]:
                        wait(s, dval[e][k])

        with nc.Block() as block:
            @block.tensor
            def _(eng):
                run(PE, eng)

            @block.scalar
            def _(eng):
                run(ACT, eng)

            @block.vector
            def _(eng):
                run(DVE, eng)

            @block.gpsimd
            def _(eng):
                run(POOL, eng)

            @block.sync
            def _(eng):
                run(SP, eng)
        self.nwaits = nwaits


def build_nc(debug=False):
    nc = bass.Bass("TRN2", target_bir_lowering=False)
    scr_kind = "ExternalOutput" if debug else "Internal"
    dt_in = lambda n, s: nc.dram_tensor(n, s, F32, kind="ExternalInput")
    x_d = dt_in("x", [SEQ, D_MODEL]).ap()
    g_mix_d = dt_in("g_mix", [1, D_MODEL])
    w_in_d = dt_in("w_in", [1, D_MODEL, 3840]).ap()[0]
    w_dw_d = dt_in("w_dw", [1, CW, 1, CONV_CH]).ap()
    b_dw_d = dt_in("b_dw", [1, CONV_CH]).ap()
    lng_d = dt_in("ln_conv_g", [1, CONV_CH]).ap()
    lnb_d = dt_in("ln_conv_b", [1, CONV_CH]).ap()
    sinks_d = dt_in("sinks", [1, 8]).ap()
    wc_d = dt_in("w_conv_out", [1, CONV_CH, D_MODEL]).ap()[0]
    wa_d = dt_in("w_attn_out", [1, 512, D_MODEL]).ap()[0]
    wo_d = dt_in("w_out", [1, D_MODEL, D_MODEL]).ap()[0]
    g_ffn_d = dt_in("g_ffn", [1, D_MODEL]).ap()
    wgrp_d = dt_in("w_group", [1, D_MODEL, 4]).ap()[0]
    bgrp_d = dt_in("b_group", [1, 4]).ap()
    wexp_d = dt_in("w_expert", [1, D_MODEL, N_EXP]).ap()[0]
    bexp_d = dt_in("b_expert", [1, N_EXP]).ap()
    wgate_d = dt_in("w_gate", [1, N_EXP, D_MODEL, DFF]).ap()[0]
    wup_d = dt_in("w_up", [1, N_EXP, D_MODEL, DFF]).ap()[0]
    wdown_d = dt_in("w_down", [1, N_EXP, DFF, D_MODEL]).ap()[0]
    g_fin_d = dt_in("g_final", [D_MODEL]).ap()
    out_d = nc.dram_tensor("out", [SEQ, D_MODEL], F32, kind="ExternalOutput").ap()
    hbuf_d = nc.dram_tensor("hbuf", [SEQ, D_MODEL], F32, kind=scr_kind).ap()
    xs_d = nc.dram_tensor("xs_scr", [N_EXP * CAP, D_MODEL], BF16, kind=scr_kind).ap()
    ys_d = nc.dram_tensor("ys_scr", [N_EXP * CAP, D_MODEL], BF16, kind=scr_kind).ap()
    wbf_d = nc.dram_tensor("wbf_scr", [N_EXP, 3, 128, 4096], BF16, kind="Internal").ap()

    with ExitStack() as st:
        def sb(name, shape, dtype):
            return st.enter_context(nc.sbuf_tensor(name, shape, dtype))

        P = Prog(nc)
        pbank = [st.enter_context(nc.psum_tensor("pb%d" % k, [128, 512], F32)) for k in range(8)]
        bank_ctr = [0]

        def nb():
            k = bank_ctr[0] % 8
            bank_ctr[0] += 1
            return k, pbank[k], ("pb", k)

        def MM(out, lhsT, rhs, start, stop, r, w):
            P.add(PE, lambda e: e.matmul(out, lhsT=lhsT, rhs=rhs, start=start, stop=stop), r, w)

        def TR32(out, in_, ident, r, w):
            P.add(PE, lambda e: e.transpose(out, in_, ident), r, w)

        def ACTV(out, in_, func, r, w, bias=None, scale=None, accum=None):
            kw = {}
            if bias is not None:
                kw["bias"] = bias
            if scale is not None:
                kw["scale"] = scale
            if accum is not None:
                kw["accum_out"] = accum
            P.add(ACT, lambda e: e.activation(out=out, in_=in_, func=func, **kw), r, w)

        def TT(eng, out, in0, in1, op, r, w):
            P.add(eng, lambda e: e.tensor_tensor(out=out, in0=in0, in1=in1, op=op), r, w)

        def TS(eng, out, in0, s1, s2, op0, op1, r, w, accum=None):
            if op1 is None:
                P.add(eng, lambda e: e.tensor_scalar(out=out, in0=in0, scalar1=s1, scalar2=None, op0=op0), r, w)
            elif accum is None:
                P.add(eng, lambda e: e.tensor_scalar(out=out, in0=in0, scalar1=s1, scalar2=s2, op0=op0, op1=op1), r, w)
            else:
                P.add(eng, lambda e: e.tensor_scalar(out=out, in0=in0, scalar1=s1, scalar2=s2, op0=op0, op1=op1,
                                                     accum_out=accum), r, w)

        def RSTD(dst, src, scale, rs, ws):
            P.add(ACT, lambda e: e.activation(out=dst, in_=src, func=AF.Sqrt, bias=epsc[:, 0:1], scale=scale), rs + ["epsc"], ws)
            P.add(DVE, lambda e: e.reciprocal(out=dst, in_=dst), ws, ws)

        def STT(eng, out, in0, scalar, in1, op0, op1, r, w):
            P.add(eng, lambda e: e.scalar_tensor_tensor(out=out, in0=in0, scalar=scalar, in1=in1, op0=op0, op1=op1), r, w)

        def CP(eng, out, in_, r, w):
            if eng == ACT:
                P.add(ACT, lambda e: e.activation(out=out, in_=in_, func=AF.Copy), r, w)
            else:
                P.add(eng, lambda e: e.tensor_copy(out=out, in_=in_), r, w)

        def RED(eng, out, in_, op, r, w):
            P.add(eng, lambda e: e.tensor_reduce(out=out, in_=in_, axis=AX.X, op=op), r, w)

        def DMA(eng, out, in_, r, w, **kw):
            P.add(eng, lambda e: e.dma_start(out=out, in_=in_, **kw), r, w, dma=True)

        def MEMSET(eng, ap, val, w):
            P.add(eng, lambda e: e.memset(ap, val), (), w)

        def TAP(name, ap, reads):
            if not debug:
                return
            shape = list(ap.shape)
            d = nc.dram_tensor("dbg_" + name, shape, F32, kind="ExternalOutput").ap()
            DMA(POOL, d, ap, reads, [("dbg", name)])

        identb = sb("identb", [128, 128], BF16)
        identf = sb("identf", [128, 128], F32)
        onesf = sb("onesf", [128, 128], F32)
        onesb = sb("onesb", [128, 128], BF16)
        ustrict = sb("ustrict", [128, 128], BF16)
        gffn_bc = sb("gffn_bc", [128, D_MODEL], F32)
        gmixT = sb("gmixT", [128, 8], F32)
        bdw = sb("bdw", [128, 4], F32)
        lng = sb("lng", [128, 4], F32)
        lnb = sb("lnb", [128, 4], F32)
        hlng = sb("hlng", [128, 4], F32)
        hlnb = sb("hlnb", [128, 4], F32)
        wdw_raw = sb("wdw_raw", [CW, CONV_CH], F32)
        wdwT = sb("wdwT", [128, 4, CW], F32)
        esink = sb("esink", [128, 8], F32)
        relt = sb("relt", [128, 2, 128], F32)
        amask = sb("amask", [128, 2, 128], F32)
        EM = sb("EM", [128, 4, 4, 128], BF16)
        wr = sb("wr", [128, 8, 36], F32)
        rbias = sb("rbias", [128, 36], F32)
        ebase = sb("ebase", [128, N_EXP], F32)
        cum = sb("cum", [128, N_EXP], F32)
        cumb2 = sb("cumb2", [128, 2, N_EXP], BF16)
        destall = sb("destall", [128, NTILE, 2], I32)
        wall = sb("wall", [128, NTILE, 2], F32)
        ss = sb("ss", [128, 8], F32)
        rstd = sb("rstd", [128, 8], F32)
        epsc = sb("epsc", [128, 1], F32)
        ztile = sb("ztile", [128, D_MODEL], BF16)
        ht2 = sb("ht2", [128, D_MODEL], F32)
        mtmp2 = sb("mtmp2", [128, 4, NT], F32)
        t1buf = sb("t1buf", [128, 2, NT], F32)

        ARENA_W = 44900
        arena = sb("arena", [128, ARENA_W], F32)
        off = [0]

        def carve(words, dtype, pattern=None, **kw):
            a = arena[:, off[0]:off[0] + words]
            off[0] += words
            assert off[0] <= ARENA_W, off[0]
            if dtype != F32:
                a = a.bitcast(dtype)
            if pattern:
                a = a.rearrange(pattern, **kw)
            return a

        wb_in = carve(15360, BF16, "p (k n) -> p k n", k=8)
        wcb = carve(2048, BF16, "p (k n) -> p k n", k=4)
        wab = carve(2048, BF16, "p (k n) -> p k n", k=4)
        wob = carve(4096, BF16, "p (k n) -> p k n", k=8)
        D64 = carve(3968, BF16, "p (c j m) -> p c j m", c=4, j=CW)
        xt = carve(4096, F32, "p (s n) -> p s n", s=4)
        xsb = carve(1024, BF16, "p (s n) -> p s n", s=2)
        xnT = carve(1024, BF16, "p (k n) -> p k n", k=8)
        vTs = carve(576, BF16, "p (c n) -> p c n", c=4)
        sig = carve(256, F32)
        qTs = carve(512, BF16, "p (c n) -> p c n", c=4)
        kTr = carve(192, BF16)
        vtok_raw = carve(200, BF16)
        vtok = vtok_raw[:, 0:390].rearrange("p (s k d) -> p s k d", s=3, k=2)
        RA = carve(1024, F32)
        RB = carve(1024, F32)
        emf = arena[:, off[0] - 2048:off[0]].rearrange("p (h k q) -> p h k q", h=8, k=2)
        RC = carve(1024, F32)
        RD = carve(1024, F32)
        pexp = carve(512, BF16, "p (b n) -> p b n", b=2)
        attn_tok = carve(256, BF16)
        mtmp = carve(1024, F32, "p (a n) -> p a n", a=4)
        mT = carve(1024, BF16, "p (k n) -> p k n", k=8)
        xn2b2 = [carve(512, BF16) for _ in range(2)]
        attnT = carve(512, BF16, "p (c n) -> p c n", c=4)
        rsm = carve(1024, F32)
        A_END = off[0]

        y32 = RA.rearrange("p (c n) -> p c n", c=4)
        xn2_ = [RA, RB]
        ybf = RB[:, 0:512].bitcast(BF16).rearrange("p (c n) -> p c n", c=4)
        ysq = RB[:, 512:1024].bitcast(BF16).rearrange("p (c n) -> p c n", c=4)
        cT = RB[:, 512:1024].bitcast(BF16).rearrange("p (c n) -> p c n", c=4)
        ln_mean = RC[:, 0:256]
        ln_rstd = RC[:, 256:512]
        ln_tmp = RC[:, 512:768]
        ln_t1 = RC[:, 768:1024]
        ht = RC
        PT = RD.bitcast(BF16).rearrange("p (g c n) -> p g c n", g=4, c=4)
        xn2T_ = [RD.rearrange("p (k n) -> p k n", k=8), mtmp.rearrange("p a n -> p (a n)").rearrange("p (k n) -> p k n", k=8)]

        MEMSET(DVE, onesf[:], 1.0, ["onesf"])
        MEMSET(DVE, onesb[:], 1.0, ["onesb"])
        P.add(POOL, lambda e: e.affine_select(out=identb[:], in_=onesf[:], pattern=[[-1, 128]], compare_op=ALU.is_equal,
                                              fill=0.0, base=0, channel_multiplier=1), ["onesf"], ["identb"])
        P.add(POOL, lambda e: e.affine_select(out=identf[:], in_=onesf[:], pattern=[[-1, 128]], compare_op=ALU.is_equal,
                                              fill=0.0, base=0, channel_multiplier=1), ["onesf"], ["identf"])
        P.add(POOL, lambda e: e.affine_select(out=ustrict[:], in_=onesf[:], pattern=[[1, 128]], compare_op=ALU.is_gt,
                                              fill=0.0, base=0, channel_multiplier=-1), ["onesf"], ["ustrict"])
        P.add(POOL, lambda e: e.iota(ebase[:], [[CAP, N_EXP]], base=0, channel_multiplier=0,
                                     allow_small_or_imprecise_dtypes=True), (), ["ebase"])
        for kb in range(2):
            P.add(POOL, lambda e, kb=kb: e.iota(relt[:, kb, :], [[1, 128]], base=128 * (1 - kb), channel_multiplier=-1,
                                                allow_small_or_imprecise_dtypes=True), (), ["relt"])
        P.add(POOL, lambda e: e.affine_select(out=amask[:, 0, :], in_=onesf[:], pattern=[[-1, 128]], compare_op=ALU.is_gt,
                                              fill=0.0, base=0, channel_multiplier=1), ["onesf"], ["amask"])
        P.add(POOL, lambda e: e.affine_select(out=amask[:, 1, :], in_=onesf[:], pattern=[[1, 128]], compare_op=ALU.is_ge,
                                              fill=0.0, base=0, channel_multiplier=-1), ["onesf"], ["amask"])
        win_v = w_in_d.rearrange("(k p) n -> p k n", p=128)

        def win_load(d0, s0, n):
            for kh in range(2):
                DMA(POOL, wb_in[:, 4 * kh:4 * kh + 4, d0:d0 + n], win_v[:, 4 * kh:4 * kh + 4, s0:s0 + n], [],
                    [("wb_in", d0, kh)])

        def win_res(col):
            if 1024 <= col < 1536:
                c_ = (col - 1024) // 128
                return [("wb_q", c_, 0), ("wb_q", c_, 1)]
            for d0, n in ((0, 512), (512, 512), (1536, 128), (3712, 128), (1664, 1024), (2688, 1024)):
                if d0 <= col < d0 + n:
                    return [("wb_in", d0, 0), ("wb_in", d0, 1)]
            raise ValueError(col)

        DMA(SP, gmixT[:], g_mix_d.ap()[0].rearrange("(c p) -> p c", p=128), [], ["gmixT"], allow_slow_non_contiguous=True)
        win_load(0, 0, 512)
        win_load(512, 512, 512)
        for c in range(4):
            for two in range(2):
                src0 = 1024 + 64 * (4 * two + c)
                DMA(POOL, wb_in[:, :, 1024 + 128 * c + 64 * two:1024 + 128 * c + 64 * two + 64],
                    win_v[:, :, src0:src0 + 64], [], [("wb_q", c, two)])
        win_load(1536, 1536, 128)
        win_load(3712, 1664, 128)
        win_load(1664, 1792, 1024)
        win_load(2688, 2816, 1024)
        DMA(POOL, wcb, wc_d.rearrange("(k p) n -> p k n", p=128), [], ["wcb"])
        DMA(POOL, wab, wa_d.rearrange("(k p) n -> p k n", p=128), [], ["wab"])
        for kh in range(2):
            DMA(POOL, wob[:, 4 * kh:4 * kh + 4, :], wo_d.rearrange("(k p) n -> p k n", p=128)[:, 4 * kh:4 * kh + 4, :],
                [], [("wob", kh)])
        MEMSET(DVE, cum[:], 0.0, ["cum"])
        MEMSET(DVE, epsc[:], EPS, ["epsc"])
        MEMSET(DVE, vtok_raw, 1.0, ["vtok0", "vtok1", "vtok2"])
        MEMSET(DVE, vTs, 0.0, ["vTs"])
        DMA(SP, gffn_bc[:], g_ffn_d[0].partition_broadcast(128), [], ["gffn_bc"])
        DMA(SP, bdw[:], b_dw_d[0].rearrange("(c p) -> p c", p=128), [], ["bdw"], allow_slow_non_contiguous=True)
        DMA(SP, lng[:], lng_d[0].rearrange("(c p) -> p c", p=128), [], ["lng"], allow_slow_non_contiguous=True)
        DMA(SP, lnb[:], lnb_d[0].rearrange("(c p) -> p c", p=128), [], ["lnb"], allow_slow_non_contiguous=True)
        DMA(SP, wdw_raw[:], w_dw_d[0].rearrange("j o c -> j (o c)"), [], ["wdw_raw"])
        DMA(SP, esink[:], sinks_d[0].partition_broadcast(128), [], ["esink"])
        DMA(SP, rbias[:, 0:4], bgrp_d[0].partition_broadcast(128), [], ["rbias"])
        DMA(SP, rbias[:, 4:36], bexp_d[0].partition_broadcast(128), [], ["rbias2"])
        DMA(SP, wr[:, :, 0:4], wgrp_d.rearrange("(k p) n -> p k n", p=128), [], ["wr"])
        DMA(SP, wr[:, :, 4:36], wexp_d.rearrange("(k p) n -> p k n", p=128), [], ["wr2"])
        ACTV(esink[:], esink[:], AF.Exp, ["esink"], ["esink"])
        TS(DVE, esink[:], esink[:], 0.5, None, ALU.mult, None, ["esink"], ["esink"])
        TS(DVE, hlng[:], lng[:], 0.5, None, ALU.mult, None, ["lng"], ["hlng"])
        TS(DVE, hlnb[:], lnb[:], 0.5, None, ALU.mult, None, ["lnb"], ["hlnb"])
        k_, bk, br = nb()
        for c in range(4):
            TR32(bk[:, c * 32:c * 32 + CW], wdw_raw[:, c * 128:(c + 1) * 128], identf[0:CW, 0:CW],
                 ["wdw_raw", "identf"], [br])
        CP(DVE, wdwT[:], bk[:, 0:128].rearrange("p (c j) -> p c j", c=4)[:, :, 0:CW], [br], ["wdwT"])
        for c in range(4):
            for hf in range(2):
                sl = slice(64 * hf, 64 * hf + 64)
                TT(DVE, D64[sl, c, :, :], identb[sl, 64 * hf:64 * hf + 64].unsqueeze(1).broadcast_to([64, CW, 64]),
                   wdwT[sl, c, :].unsqueeze(2).broadcast_to([64, CW, 64]), ALU.mult, ["identb", "wdwT"],
                   [("D64", c, 0), ("D64", c, 1)])
        TS(DVE, relt[:], relt[:], 0.0, 128.0, ALU.max, ALU.min, ["relt"], ["relt"])
        def emf_res(h):
            if h < 4:
                return [("RA", h)]
            hh = h - 4
            return [("RB%d" % (hh // 2), 2 * (hh % 2)), ("RB%d" % (hh // 2), 2 * (hh % 2) + 1)]

        for h in range(8):
            ACTV(emf[:, h, :, :], relt[:], AF.Exp, ["relt"], emf_res(h), scale=-(2.0 ** (-(h + 1))))
        for kv in range(2):
            for kb in range(2):
                TT(DVE, EM[:, 2 * kv + kb, :, :], emf[:, 4 * kv:4 * kv + 4, kb, :],
                   amask[:, kb, :].unsqueeze(1).broadcast_to([128, 4, 128]), ALU.mult,
                   sum([emf_res(h_) for h_ in range(4 * kv, 4 * kv + 4)], []) + ["amask"], ["EM"])

        def prep(s):
            for i in range(TPS):
                g = s * TPS + i
                slot = g % 4
                DMA(SP, xt[:, slot, :], x_d[g * 128:(g + 1) * 128, :], [], [("xt", slot)])
                ACTV(xsb[:, i, :], xt[:, slot, :], AF.Square, [("xt", slot)], [("xsb", i), ("ss", i)],
                     accum=ss[:, i:i + 1])
            RSTD(rstd[:, 0:TPS], ss[:, 0:TPS], 1.0 / D_MODEL, [("ss", i) for i in range(TPS)], [("rstd", i) for i in range(TPS)])
            for i in range(TPS):
                slot = (s * TPS + i) % 4
                TS(DVE, xsb[:, i, :], xt[:, slot, :], rstd[:, i:i + 1], None, ALU.mult, None,
                   [("xt", slot), ("rstd", i)], [("xsb", i)])

        evac_flip = [0]

        def transposes(s):
            for i in range(TPS):
                for hf in range(2):
                    k_, bk, br = nb()
                    for q in range(4):
                        kk = 4 * hf + q
                        MM(bk[:, q * 128:(q + 1) * 128], xsb[:, i, kk * 128:(kk + 1) * 128], identb[:], True, True,
                           [("xsb", i), "identb"], [br])
                    for q in range(4):
                        kk = 4 * hf + q
                        evac_flip[0] ^= 1
                        if evac_flip[0]:
                            ACTV(xnT[:, kk, i * 128:(i + 1) * 128], bk[:, q * 128:(q + 1) * 128], AF.Copy,
                                 [br, "gmixT"], ["xnT"], scale=gmixT[:, kk:kk + 1])
                        else:
                            TS(DVE, xnT[:, kk, i * 128:(i + 1) * 128], bk[:, q * 128:(q + 1) * 128],
                               gmixT[:, kk:kk + 1], None, ALU.mult, None, [br, "gmixT"], ["xnT"])

        def proj_chunk(j):
            k_, bk, br = nb()
            wres = win_res(j * 128)
            for k in range(8):
                MM(bk[:, 0:NT], wb_in[:, k, j * 128:(j + 1) * 128], xnT[:, k, :], k == 0, k == 7, wres + ["xnT"], [br])
            return bk, br

        def inproj_glu(s):
            if s > 0:
                CP(POOL, vTs[:, :, 0:30], vTs[:, :, NT:NT + 30], ["vTs"], ["vTs"])
                CP(POOL, kTr[:, 0:128], kTr[:, NT:NT + 128], ["kTr"], ["kTr"])
                CP(POOL, vtok[:, 0, :, :], vtok[:, TPS, :, :], ["vtok%d" % TPS], ["vtok0"])
            for j in range(4):
                ba, bar = proj_chunk(j)
                bg, bgr = proj_chunk(4 + j)
                ACTV(sig, bg[:, 0:NT], AF.Tanh, [bgr], ["sig"], scale=0.5)
                STT(DVE, vTs[:, j, 30:30 + NT], sig, 1.0, ba[:, 0:NT], ALU.add, ALU.mult, [bar, "sig"], ["vTs"])

        def inproj_qkv(s):
            for c in range(4):
                bq, bqr = proj_chunk(8 + c)
                CP(ACT, qTs[:, c, :], bq[:, 0:NT], [bqr], ["qTs"])
            bk_, bkr = proj_chunk(12)
            CP(DVE, kTr[:, 128:128 + NT], bk_[:, 0:NT], [bkr], ["kTr"])
            wres = win_res(3712)
            for i in range(TPS):
                k_, bv, bvr = nb()
                for k in range(8):
                    MM(bv[:, 0:128], xnT[:, k, i * 128:(i + 1) * 128], wb_in[:, k, 3712:3840], k == 0, k == 7,
                       ["xnT"] + wres, [bvr])
                CP(ACT, vtok[:, 1 + i, :, 0:64], bv[:, 0:128].rearrange("p (k d) -> p k d", k=2), [bvr],
                   ["vtok%d" % (1 + i)])

        def conv_chunk(s, c):
            banks = []
            for hf in range(2):
                k_, bk, br = nb()
                banks.append((bk, br))
            for j in range(CW):
                for hf in range(2):
                    bk, br = banks[hf]
                    MM(bk[64 * hf:64 * hf + 64, 0:NT], D64[64 * hf:64 * hf + 64, c, j, :],
                       vTs[64 * hf:64 * hf + 64, c, j:j + NT], j == 0, j == CW - 1, [("D64", c, 0), ("D64", c, 1), "vTs"], [br])
            for hf in range(2):
                bk, br = banks[hf]
                sl = slice(64 * hf, 64 * hf + 64)
                ACTV(y32[sl, c, :], bk[sl, 0:NT], AF.Identity, [br, "bdw"], [("RA", c)], bias=bdw[sl, c:c + 1], scale=0.5)
                ACTV(ysq[sl, c, :], bk[sl, 0:NT], AF.Square, [br, "bdw"], [("RB1", c)], bias=bdw[sl, c:c + 1], scale=0.5)
            CP(DVE, ybf[:, c, :], y32[:, c, :], [("RA", c)], [("RB0", c)])

        def attn_qk(s, i):
            n = s * TPS + i
            qb = i * 128
            kbs = [1] if n == 0 else [0, 1]
            for kv in range(2):
                sl = slice(64 * kv, 64 * kv + 64)
                for kb in kbs:
                    k_, bk, br = nb()
                    kc = qb + 128 * kb
                    MM(bk[:, :].rearrange("p (c q) -> p c q", c=4), kTr[sl, kc:kc + 128], qTs[sl, :, qb:qb + 128],
                       True, True, ["kTr", "qTs"], [br])
                    pe_ = ("pexp", kb)
                    ACTV(pexp[:, kb, :], bk[:, :], AF.Exp, [br], [pe_], scale=0.125)
                    TT(DVE, PT[:, 2 * kv + kb, :, :], pexp[:, kb, :].rearrange("p (c q) -> p c q", c=4),
                       EM[:, 2 * kv + kb, :, :], ALU.mult, [pe_, "EM"], [("RD", 2 * kv + kb)])

        def attn_pv(s, i):
            n = s * TPS + i
            kbs = [1] if n == 0 else [0, 1]
            for kv in range(2):
                k_, bo, bor = nb()
                ov = bo[:, 0:260].rearrange("p (c d) -> p c d", c=4)
                for c in range(4):
                    for kb in kbs:
                        MM(ov[:, c, :], PT[:, 2 * kv + kb, c, :], vtok[:, i + kb, kv, :], kb == kbs[0], kb == 1,
                           [("RD", 2 * kv + kb), "vtok%d" % (i + kb)], [bor])
                den = rsm[:, 4 * kv:4 * kv + 4]
                dr = ("den", kv)
                STT(DVE, den, ov[:, :, 64], 0.5, esink[:, 4 * kv:4 * kv + 4], ALU.mult, ALU.add, [bor, "esink"], [dr])
                P.add(DVE, lambda e, den=den: e.reciprocal(out=den, in_=den), [dr], [dr])
                TT(DVE, attn_tok[:, 256 * kv:256 * kv + 256].rearrange("p (c d) -> p c d", c=4), ov[:, :, 0:64],
                   den.unsqueeze(2).broadcast_to([128, 4, 64]), ALU.mult, [bor, dr], [("attn_tok", kv)])

        def attn_T(s, i):
            qb = i * 128
            k_, bt, btr = nb()
            for c in range(4):
                MM(bt[:, c * 128:(c + 1) * 128], attn_tok[:, c * 128:(c + 1) * 128], identb[:], True, True,
                   [("attn_tok", c // 2), "identb"], [btr])
            CP(ACT, attnT[:, :, qb:qb + 128], bt[:, :].rearrange("p (c q) -> p c q", c=4), [btr], ["attnT"])

        def ln_merge(s):
            k1, b1, b1r = nb()
            k2, b2, b2r = nb()
            for c in range(4):
                MM(b1[:, 0:NT], onesb[:], ybf[:, c, :], c == 0, c == 3, ["onesb", ("RB0", c)], [b1r])
            for c in range(4):
                MM(b2[:, 0:NT], onesb[:], ysq[:, c, :], c == 0, c == 3, ["onesb", ("RB1", c)], [b2r])
            sg_c = [mtmp[:, 0, :], mtmp[:, 2, :]]
            sg_a = [mtmp[:, 1, :], mtmp[:, 3, :]]
            sg_cr = ["sgc", "t1"]
            sg_ar = ["sga", "t2"]
            t2s = [mtmp2[:, jj, :] for jj in range(4)]
            t2r = [("t2s", jj) for jj in range(4)]
            for i in range(2):
                v_ = xn2b2[i].bitcast(F32).rearrange("p (a n) -> p a n", a=2)
                t2s += [v_[:, 0, :], v_[:, 1, :]]
                t2r += [("xn2b", i), ("xn2b", i)]

            def p1(j):
                k_, bb, bbr = nb()
                for k in range(4):
                    MM(bb[:, 0:NT], wab[:, k, j * 128:(j + 1) * 128], attnT[:, k, :], k == 0, k == 3, ["wab", "attnT"], [bbr])
                bd, bdr = proj_chunk(21 + j)
                ACTV(sg_a[j % 2], bd[:, 0:NT], AF.Tanh, [bdr], [sg_ar[j % 2]], scale=0.5)
                STT(DVE, t2s[j], sg_a[j % 2], 1.0, bb[:, 0:NT], ALU.add, ALU.mult, [bbr, sg_ar[j % 2]], [t2r[j]])

            def ln_chunk(c):
                TT(DVE, ln_t1, y32[:, c, :], ln_mean, ALU.subtract, [("RA", c), "RC0"], ["RC3"])
                TT(DVE, ln_t1, ln_t1, ln_rstd, ALU.mult, ["RC3", "RC1"], ["RC3"])
                ACTV(sig, ln_t1, AF.Tanh, ["RC3", "hlng", "hlnb"], ["sig"], scale=hlng[:, c:c + 1], bias=hlnb[:, c:c + 1])
                TS(DVE, ln_t1, ln_t1, lng[:, c:c + 1], lnb[:, c:c + 1], ALU.mult, ALU.add, ["RC3", "lng", "lnb"], ["RC3"])
                STT(DVE, cT[:, c, :], sig, 1.0, ln_t1, ALU.add, ALU.mult, ["sig", "RC3", ("RB1", c)], [("RB1", c)])

            p1(0)
            TS(DVE, ln_mean, b1[:, 0:NT], 1.0 / CONV_CH, None, ALU.mult, None, [b1r], ["RC0"])
            TT(DVE, ln_tmp, ln_mean, ln_mean, ALU.mult, ["RC0"], ["RC2"])
            STT(DVE, ln_rstd, b2[:, 0:NT], 1.0 / CONV_CH, ln_tmp, ALU.mult, ALU.subtract, [b2r, "RC2"], ["RC1"])
            p1(1)
            RSTD(ln_rstd, ln_rstd, 1.0, ["RC1"], ["RC1"])
            p1(2)
            p1(3)
            ln_chunk(0)
            p1(4)
            ln_chunk(1)
            p1(5)
            ln_chunk(2)
            p1(6)
            ln_chunk(3)
            p1(7)
            for j in range(8):
                k_, ba, bar = nb()
                for k in range(4):
                    MM(ba[:, 0:NT], wcb[:, k, j * 128:(j + 1) * 128], cT[:, k, :], k == 0, k == 3,
                       ["wcb", ("RB1", k)], [bar])
                bc, bcr = proj_chunk(13 + j)
                ACTV(sg_c[j % 2], bc[:, 0:NT], AF.Tanh, [bcr], [sg_cr[j % 2]], scale=0.5)
                STT(DVE, t1buf[:, j % 2, :], sg_c[j % 2], 1.0, ba[:, 0:NT], ALU.add, ALU.mult, [bar, sg_cr[j % 2]],
                    [("t1b", j % 2)])
                TT(POOL, mT[:, j, :], t1buf[:, j % 2, :], t2s[j], ALU.add, [("t1b", j % 2), t2r[j]], [("mT", j)])

        RAr = [("RA", c) for c in range(4)]
        RBr = [("RB0", c) for c in range(4)] + [("RB1", c) for c in range(4)]
        RCr = ["RC0", "RC1", "RC2", "RC3"]
        RDr = [("RD", q) for q in range(4)]
        MTr = ["sgc", "sga", "t1", "t2"]
        xn2_res = [RAr, RBr]
        hts = [ht, ht2[:]]
        htr = [RCr, ["ht2"]]
        xn2T_res = [RDr, MTr]

        def tail_wout(s):
            mres = [("mT", j) for j in range(8)]
            for i in range(TPS):
                g = s * TPS + i
                slot = g % 4
                for hf in range(2):
                    k_, bk, br = nb()
                    for k in range(8):
                        MM(bk[:, :], mT[:, k, i * 128:(i + 1) * 128], wob[:, k, hf * 512:(hf + 1) * 512], k == 0, k == 7,
                           mres + [("wob", 0), ("wob", 1)], [br])
                    STT(DVE, hts[i][:, hf * 512:(hf + 1) * 512], bk[:, :], 0.25, xt[:, slot, hf * 512:(hf + 1) * 512],
                        ALU.mult, ALU.add, [br, ("xt", slot)], htr[i])
                DMA(SP, hbuf_d[g * 128:(g + 1) * 128, :], hts[i], htr[i], [("hbuf", g)])

        def tail_wout_b(s):
            for i in range(TPS):
                ACTV(xn2b2[i], hts[i], AF.Square, htr[i], [("xn2b", i), ("ss2", i)], accum=ss[:, 2 + i:3 + i])
            RSTD(rstd[:, 2:2 + TPS], ss[:, 2:2 + TPS], 1.0 / D_MODEL, [("ss2", i) for i in range(TPS)],
                 [("rstd2", i) for i in range(TPS)])
            for i in range(TPS):
                xb = xn2b2[i]
                STT(DVE, xn2_[i], hts[i], rstd[:, 2 + i:3 + i], gffn_bc[:], ALU.mult, ALU.mult,
                    htr[i] + [("rstd2", i), "gffn_bc"], xn2_res[i])
                CP(ACT, xb, xn2_[i], xn2_res[i], [("xn2b", i)])

        def tail_rtr_T(s):
            for i in range(TPS):
                for hf in range(2):
                    k_, bk, br = nb()
                    for q in range(4):
                        kk = 4 * hf + q
                        TR32(bk[:, q * 128:(q + 1) * 128], xn2_[i][:, kk * 128:(kk + 1) * 128], identf[:],
                             xn2_res[i] + ["identf"], [br])
                    if hf == 0:
                        CP(ACT, xn2T_[i][:, 0:4, :], bk[:, :].rearrange("p (k n) -> p k n", k=4), [br], xn2T_res[i][0:2])
                    else:
                        CP(DVE, xn2T_[i][:, 4:8, :], bk[:, :].rearrange("p (k n) -> p k n", k=4), [br], xn2T_res[i][2:4])

        def rfields(i):
            o = 8 + 500 * i
            f = {}
            names = [("lg", 36), ("gmax", 1), ("ngmax", 1), ("gexp", 4), ("gsum", 1), ("pgrp", 1), ("gone", 4), ("pen", 4),
                     ("em", 32), ("m1", 1), ("one1", 32), ("em2", 32), ("m2", 1), ("one2", 32), ("dlt", 1), ("w1", 1),
                     ("ind", 32), ("indb", 16), ("pos", 32), ("tmp", 32), ("d12", 2)]
            for nm, w_ in names:
                f[nm] = rsm[:, o:o + w_]
                o += w_
            assert o <= 8 + 500 * (i + 1)
            return f

        tail_state = {}

        def tail_logits(s):
            for i in range(TPS):
                k_, bl, blr = nb()
                for k in range(8):
                    MM(bl[:, 0:36], xn2T_[i][:, k, :], wr[:, k, :], k == 0, k == 7, xn2T_res[i] + ["wr", "wr2"], [blr])
                TT(DVE, rfields(i)["lg"], bl[:, 0:36], rbias[:], ALU.add, [blr, "rbias", "rbias2"], [("lg", i)])

        def tail_route(s, i):
            if True:
                g = s * TPS + i
                F = rfields(i)
                R = lambda nm: (nm, i)
                lg, gmax, ngmax, gexp, gsum, pgrp = F["lg"], F["gmax"], F["ngmax"], F["gexp"], F["gsum"], F["pgrp"]
                gone, pen, em, m1, one1, em2, m2, one2 = F["gone"], F["pen"], F["em"], F["m1"], F["one1"], F["em2"], F["m2"], F["one2"]
                dlt, w1, ind = F["dlt"], F["w1"], F["ind"]
                indb = F["indb"].bitcast(BF16)
                RED(DVE, gmax, lg[:, 0:4], ALU.max, [R("lg")], [R("gmax")])
                TS(DVE, ngmax, gmax, -1.0, None, ALU.mult, None, [R("gmax")], [R("ngmax")])
                ACTV(gexp, lg[:, 0:4], AF.Exp, [R("lg"), R("ngmax")], [R("gexp"), R("gsum")], bias=ngmax, accum=gsum)
                P.add(DVE, lambda e, pgrp=pgrp, gsum=gsum: e.reciprocal(out=pgrp, in_=gsum), [R("gsum")], [R("pgrp")])
                TS(DVE, gone, lg[:, 0:4], gmax, None, ALU.is_equal, None, [R("lg"), R("gmax")], [R("gone")])
                TS(DVE, pen, gone, -1.0, BIG, ALU.add, ALU.mult, [R("gone")], [R("pen")])
                TT(DVE, em.rearrange("p (g j) -> p g j", g=4), lg[:, 4:36].rearrange("p (g j) -> p g j", g=4),
                   pen.unsqueeze(2).broadcast_to([128, 4, 8]), ALU.add, [R("lg"), R("pen")], [R("em")])
                RED(DVE, m1, em, ALU.max, [R("em")], [R("m1")])
                TS(DVE, one1, em, m1, None, ALU.is_equal, None, [R("em"), R("m1")], [R("one1")])
                STT(DVE, em2, one1, -BIG, em, ALU.mult, ALU.add, [R("one1"), R("em")], [R("em2")])
                RED(DVE, m2, em2, ALU.max, [R("em2")], [R("m2")])
                TS(DVE, one2, em2, m2, None, ALU.is_equal, None, [R("em2"), R("m2")], [R("one2")])
                TT(DVE, dlt, m2, m1, ALU.subtract, [R("m1"), R("m2")], [R("dlt")])
                ACTV(dlt, dlt, AF.Exp, [R("dlt")], [R("dlt")])
                TS(DVE, dlt, dlt, 1.0, None, ALU.add, None, [R("dlt")], [R("dlt")])
                P.add(DVE, lambda e, w1=w1, dlt=dlt: e.reciprocal(out=w1, in_=dlt), [R("dlt")], [R("w1")])
                TT(DVE, wall[:, g, 0:1], w1, pgrp, ALU.mult, [R("w1"), R("pgrp")], [("wall", g)])
                TT(DVE, wall[:, g, 1:2], pgrp, wall[:, g, 0:1], ALU.subtract, [R("pgrp"), ("wall", g)], [("wall", g)])
                TT(DVE, ind, one1, one2, ALU.add, [R("one1"), R("one2")], [R("ind")])
                CP(DVE, indb, ind, [R("ind")], [R("indb")])

        def tail_pos(s):
            for i in range(TPS):
                g = s * TPS + i
                F = rfields(i)
                R = lambda nm: (nm, i)
                ind, pos, tmp, one1, one2, d12 = F["ind"], F["pos"], F["tmp"], F["one1"], F["one2"], F["d12"]
                indb = F["indb"].bitcast(BF16)
                cb_ = cumb2[:, i, :]
                CP(DVE, cb_, cum[:], ["cum"], [("cumb", i)])
                k_, bp, bpr = nb()
                MM(bp[:, 0:32], ustrict[:], indb, True, False, ["ustrict", R("indb")], [bpr])
                MM(bp[:, 0:32], onesb[:], cb_, False, True, ["onesb", ("cumb", i)], [bpr])
                TT(DVE, cum[:], cum[:], ind, ALU.add, ["cum", R("ind")], ["cum"])
                TS(DVE, pos, bp[:, 0:32], float(CAP - 1), None, ALU.min, None, [bpr], [R("pos")])
                TT(DVE, pos, pos, ebase[:], ALU.add, [R("pos"), "ebase"], [R("pos")])
                TT(DVE, tmp, pos, one1, ALU.mult, [R("pos"), R("one1")], [R("tmp")])
                RED(DVE, d12[:, 0:1], tmp, ALU.add, [R("tmp")], [R("d1f")])
                TT(DVE, tmp, pos, one2, ALU.mult, [R("pos"), R("one2")], [R("tmp")])
                RED(DVE, d12[:, 1:2], tmp, ALU.add, [R("tmp")], [R("d2f")])
                CP(DVE, destall[:, g, :], d12, [R("d1f"), R("d2f")], [("dest", g)])
                zres = [("xs_z", r0) for r0 in range(0, N_EXP * CAP, 1024)]
                for k in range(2):
                    P.add(POOL, lambda e, g=g, k=k, i=i: e.indirect_dma_start(
                        out=xs_d, out_offset=bass.IndirectOffsetOnAxis(ap=destall[:, g, k:k + 1], axis=0),
                        in_=xn2b2[i], in_offset=None), [("xn2b", i), ("dest", g)] + zres, [("xs_scr", g, k)], dma=True)

        def precast(e):
            srcs = (wgate_d[e].rearrange("(k p) n -> p k n", p=128), wup_d[e].rearrange("(k p) n -> p k n", p=128),
                    wdown_d[e].rearrange("(k p) n -> p k n", p=128))
            for m_, src in enumerate(srcs):
                kk = src.shape[1]
                dst = wbf_d[e, m_].rearrange("p (k n) -> p k n", k=kk)
                DMA(POOL, dst, src, [], [("wbf", e, m_)])

        def zero_fill():
            MEMSET(DVE, ztile[:], 0.0, ["ztile"])
            for r0 in range(0, N_EXP * CAP, 1024):
                DMA(POOL, xs_d[r0:r0 + 1024, :].rearrange("(b p) n -> p b n", p=128),
                    ztile[:].unsqueeze(1).broadcast_to([128, 8, D_MODEL]), ["ztile"], [("xs_z", r0)])

        prep(0)
        for s in range(NST):
            transposes(s)
            if s > 0:
                tail_wout(s - 1)
            inproj_glu(s)
            if s > 0:
                tail_wout_b(s - 1)
            inproj_qkv(s)
            if s == 0:
                zero_fill()
            if s + 1 < NST:
                prep(s + 1)
            if s > 0:
                tail_rtr_T(s - 1)
            conv_chunk(s, 0)
            if s > 0:
                tail_logits(s - 1)
                tail_route(s - 1, 0)
            attn_qk(s, 0)
            conv_chunk(s, 1)
            if s > 0:
                tail_route(s - 1, 1)
            attn_pv(s, 0)
            attn_qk(s, 1)
            conv_chunk(s, 2)
            attn_T(s, 0)
            attn_pv(s, 1)
            if s > 0:
                tail_pos(s - 1)
            conv_chunk(s, 3)
            attn_T(s, 1)
            ln_merge(s)
            for e_ in range(s * (N_EXP // NST), (s + 1) * (N_EXP // NST)):
                precast(e_)
        tail_wout(NST - 1)
        tail_wout_b(NST - 1)
        tail_rtr_T(NST - 1)
        tail_logits(NST - 1)
        tail_route(NST - 1, 0)
        tail_route(NST - 1, 1)
        tail_pos(NST - 1)

        P.barrier()
        off[0] = 0
        NWB = 4
        wexp = []
        for b in range(NWB):
            wg = carve(2048, BF16, "p (k n) -> p k n", k=8)
            wu = carve(2048, BF16, "p (k n) -> p k n", k=8)
            wd = carve(2048, BF16, "p (k n) -> p k n", k=4)
            wexp.append((wg, wu, wd))
        NXS = 3
        xsr = [carve(2048, BF16, "p (b n) -> p b n", b=4) for _ in range(NXS)]
        xsT = [carve(4 * CAP, BF16, "p (k n) -> p k n", k=8) for _ in range(2)]
        sgt = [carve(CAP, F32) for _ in range(2)]
        hT = [carve(2 * CAP, BF16, "p (f n) -> p f n", f=4) for _ in range(2)]
        NYB = 4
        ysb = [carve(512, BF16) for _ in range(NYB)]
        B_END = off[0]

        def load_weights(e):
            b = e % NWB
            wg, wu, wd = wexp[b]
            DMA(SP, wg.rearrange("p k n -> p (k n)"), wbf_d[e, 0], [], [("wg", b, 0), ("wg", b, 1)])
            DMA(SP, wu.rearrange("p k n -> p (k n)"), wbf_d[e, 1], [], [("wu", b, 0), ("wu", b, 1)])
            DMA(SP, wd.rearrange("p k n -> p (k n)"), wbf_d[e, 2], [], [("wd", b, 0), ("wd", b, 1)])

        def load_xs(e):
            b = e % NXS
            DMA(SP, xsr[b][:, 0:3, :], xs_d[e * CAP:e * CAP + 384, :].rearrange("(b p) n -> p b n", p=128), [], [("xsr", b)])
            DMA(SP, xsr[b][0:64, 3, :], xs_d[e * CAP + 384:(e + 1) * CAP, :], [], [("xsrt", b)])

        def exp_transposes(e):
            b = e % NXS
            xT = xsT[e % 2]
            for blk, (s0, sz) in enumerate(BLKS):
                for hf in range(2):
                    k_, bk, br = nb()
                    for q in range(4):
                        kk = 4 * hf + q
                        MM(bk[:, q * sz:(q + 1) * sz], xsr[b][0:sz, blk, kk * 128:(kk + 1) * 128], identb[0:sz, 0:sz], True, True,
                           [("xsr", b), ("xsrt", b), "identb"], [br])
                    dst = xT[:, 4 * hf:4 * hf + 4, s0:s0 + sz]
                    src = bk[:, 0:4 * sz].rearrange("p (k n) -> p k n", k=4)
                    if hf == 0:
                        CP(ACT, dst, src, [br], [("xsT", e % 2)])
                    else:
                        CP(DVE, dst, src, [br], [("xsT", e % 2)])

        def exp_gateup(e):
            b = e % NWB
            wg, wu, wd = wexp[b]
            xT = xsT[e % 2]
            h_ = hT[e % 2]
            for f in range(4):
                k_, bg, bgr = nb()
                k_, bu, bur = nb()
                for k in range(8):
                    MM(bg[:, 0:CAP], wg[:, k, f * 128:(f + 1) * 128], xT[:, k, :], k == 0, k == 7,
                       [("wg", b, 0), ("wg", b, 1), ("xsT", e % 2)], [bgr])
                for k in range(8):
                    MM(bu[:, 0:CAP], wu[:, k, f * 128:(f + 1) * 128], xT[:, k, :], k == 0, k == 7,
                       [("wu", b, 0), ("wu", b, 1), ("xsT", e % 2)], [bur])
                sg_ = sgt[f % 2]
                ACTV(sg_, bg[:, 0:CAP], AF.Tanh, [bgr], [("sgt", f % 2)], scale=0.5)
                STT(DVE, sg_, sg_, 1.0, bg[:, 0:CAP], ALU.add, ALU.mult, [bgr, ("sgt", f % 2)], [("sgt", f % 2)])
                TT(DVE, h_[:, f, :], bu[:, 0:CAP], sg_, ALU.mult, [bur, ("sgt", f % 2)], [("hT", e % 2, f)])

        ycount = [0]

        def exp_down(e):
            b = e % NWB
            wg, wu, wd = wexp[b]
            h_ = hT[e % 2]
            for blk, (s0, sz) in enumerate(BLKS):
                yb = ysb[ycount[0] % NYB]
                yr = ("ysb", ycount[0] % NYB)
                ycount[0] += 1
                for hf in range(2):
                    k_, bk, br = nb()
                    for f in range(4):
                        MM(bk[0:sz, :], h_[:, f, s0:s0 + sz], wd[:, f, hf * 512:(hf + 1) * 512], f == 0, f == 3,
                           [("hT", e % 2, f_) for f_ in range(4)] + [("wd", b, 0), ("wd", b, 1)], [br])
                    if hf == 0:
                        ACTV(yb[0:sz, 0:512], bk[0:sz, :], AF.Copy, [br], [yr], scale=0.5)
                    else:
                        TS(DVE, yb[0:sz, 512:1024], bk[0:sz, :], 0.5, None, ALU.mult, None, [br], [yr])
                r0 = e * CAP + s0
                DMA(SP, ys_d[r0:r0 + sz, :], yb[0:sz, :], [yr], [("ys_scr", e, blk)])

        for e in range(3):
            load_weights(e)
            load_xs(e) if e < 3 else None
        exp_transposes(0)
        for e in range(N_EXP):
            if e + 3 < N_EXP:
                load_weights(e + 3)
            exp_gateup(e)
            if e + 1 < N_EXP:
                exp_transposes(e + 1)
            if e + 3 < N_EXP:
                load_xs(e + 3)
            exp_down(e)

        P.barrier()
        off[0] = 0
        gfin_bc = carve(1024, F32)
        NCB = 4
        cb = []
        for b in range(NCB):
            cb.append(dict(h=carve(1024, F32), y1=carve(512, BF16), y2=carve(512, BF16), o=carve(1024, F32)))
        DMA(SP, gfin_bc, g_fin_d.partition_broadcast(128), [], ["gfin_bc"])

        def c_load(g):
            b = g % NCB
            B_ = cb[b]
            DMA(SP, B_["h"], hbuf_d[g * 128:(g + 1) * 128, :], [], [("ch", b)])
            for k, key in ((0, "y1"), (1, "y2")):
                P.add(POOL, lambda e, g=g, k=k, dstt=B_[key]: e.indirect_dma_start(
                    out=dstt, out_offset=None, in_=ys_d,
                    in_offset=bass.IndirectOffsetOnAxis(ap=destall[:, g, k:k + 1], axis=0)),
                    [], [("c" + key, b)], dma=True)

        for g in range(NCB - 1):
            c_load(g)
        for g in range(NTILE):
            b = g % NCB
            B_ = cb[b]
            if g + NCB - 1 < NTILE:
                c_load(g + NCB - 1)
            STT(DVE, B_["h"], B_["y1"], wall[:, g, 0:1], B_["h"], ALU.mult, ALU.add, [("cy1", b), ("ch", b)], [("ch", b)])
            STT(DVE, B_["h"], B_["y2"], wall[:, g, 1:2], B_["h"], ALU.mult, ALU.add, [("cy2", b), ("ch", b)], [("ch", b)])
            ACTV(B_["o"], B_["h"], AF.Square, [("ch", b)], [("co", b), ("ss3", g % 2)], accum=ss[:, 5 + g % 2:6 + g % 2])
            RSTD(rstd[:, 5 + g % 2:6 + g % 2], ss[:, 5 + g % 2:6 + g % 2], 1.0 / D_MODEL, [("ss3", g % 2)], [("rstd3", g % 2)])
            STT(DVE, B_["o"], B_["h"], rstd[:, 5 + g % 2:6 + g % 2], gfin_bc, ALU.mult, ALU.mult,
                [("ch", b), ("rstd3", g % 2), "gfin_bc"], [("co", b)])
            DMA(SP, out_d[g * 128:(g + 1) * 128, :], B_["o"], [("co", b)], [("out", g)])

        P.emit(st)
        build_nc.stats = (P.stats, P.nwaits, A_END, B_END, off[0])
    return nc


_NC_CACHE = {}


def kernel(**inputs):
    x = np.ascontiguousarray(np.asarray(inputs["x"], dtype=np.float32))
    names = ["g_mix", "w_in", "w_dw", "b_dw", "ln_conv_g", "ln_conv_b", "sinks", "w_conv_out", "w_attn_out", "w_out",
             "g_ffn", "w_group", "b_group", "w_expert", "b_expert", "w_gate", "w_up", "w_down", "g_final"]
    shared = {n: np.ascontiguousarray(np.asarray(inputs[n], dtype=np.float32)) for n in names}
    if "nc" not in _NC_CACHE:
        _NC_CACHE["nc"] = build_nc()
    nc = _NC_CACHE["nc"]
    in_maps = []
    for c in range(8):
        m = dict(shared)
        m["x"] = x[c]
        in_maps.append(m)
    res = run_bass_kernel_spmd(nc, in_maps, core_ids=list(range(8)))
    return np.stack([np.asarray(r["out"], dtype=np.float32) for r in res.results], axis=0)
```

```python
import numpy as np
from contextlib import ExitStack
import concourse.bass as bass
import concourse.mybir as mybir
from concourse.bass_utils import run_bass_kernel_spmd

F32 = mybir.dt.float32
BF16 = mybir.dt.bfloat16
I32 = mybir.dt.int32
AF = mybir.ActivationFunctionType
ALU = mybir.AluOpType
AX = mybir.AxisListType

PE, ACT, DVE, POOL, SP = "pe", "act", "dve", "pool", "sp"

D_MODEL = 1024
SEQ = 4096
NTILE = SEQ // 128
NT = 256
TPS = NT // 128
NST = SEQ // NT
CONV_CH = 512
CW = 31
N_EXP = 32
CAP = 384
BLKS = [(b0, min(128, CAP - b0)) for b0 in range(0, CAP, 128)]
NFULL = CAP // 128
DFF = 512
EPS = 1e-6
BIG = 1.0e30
SEM_EPOCH = 20000


class Prog:
    def __init__(self, nc, n_dma_sems=8):
        self.nc = nc
        self.ops = []
        self.n_dma_sems = n_dma_sems
        self.last_w = {}
        self.readers = {}
        self.last_op = {}
        self.recent_dma = {}

    def add(self, eng, emit, reads=(), writes=(), dma=False):
        ops = self.ops
        i = len(ops)
        op = dict(eng=eng, emit=emit, dma=dma, deps=set(), sig=False)
        deps = set()
        for r in reads:
            w = self.last_w.get(r)
            if w is not None:
                deps.add((w, False))
        for wr in writes:
            w = self.last_w.get(wr)
            if w is not None:
                deps.add((w, False))
            for rd in self.readers.get(wr, ()):
                deps.add((rd, True))
        for r in reads:
            self.readers.setdefault(r, []).append(i)
        for wr in writes:
            self.last_w[wr] = i
            self.readers[wr] = []
        final = set()
        for d, war in deps:
            if d == i:
                continue
            p = ops[d]
            if not p["dma"] and not dma and p["eng"] == eng:
                if eng == PE:
                    continue
                if war:
                    continue
            final.add(d)
        op["deps"] = final
        for d in final:
            ops[d]["sig"] = True
        ops.append(op)
        self.last_op[eng] = i
        if dma:
            self.recent_dma.setdefault(eng, []).append(i)
            self.recent_dma[eng] = self.recent_dma[eng][-self.n_dma_sems:]
        return i

    def barrier(self):
        deps = set(self.last_op.values())
        for lst in self.recent_dma.values():
            deps.update(lst)
        for e in (PE, ACT, DVE, POOL, SP):
            i = len(self.ops)
            op = dict(eng=e, emit=None, dma=False, deps=set(deps), sig=False)
            self.ops.append(op)
            self.last_op[e] = i
        for d in deps:
            self.ops[d]["sig"] = True
        self.last_w = {}
        self.readers = {}

    def emit(self, stack):
        nc = self.nc
        ops = self.ops
        engs = [PE, ACT, DVE, POOL, SP]
        nsig = {e: sum(1 for o in ops if o["eng"] == e and o["sig"] and not o["dma"]) for e in engs}
        csem = {e: [stack.enter_context(nc.semaphore("c_%s%d" % (e, k)))
                    for k in range(nsig[e] // SEM_EPOCH + 1)] for e in engs}
        dsem = {e: [stack.enter_context(nc.semaphore("d_%s%d" % (e, k)))
                    for k in range(self.n_dma_sems)] for e in (ACT, POOL, SP)}
        ccount = {e: 0 for e in engs}
        dcount = {e: 0 for e in dsem}
        dval = {e: [0] * self.n_dma_sems for e in dsem}
        for op in ops:
            e = op["eng"]
            if op["dma"]:
                k = dcount[e] % self.n_dma_sems
                dcount[e] += 1
                op["prev_slot"] = (dsem[e][k], dval[e][k]) if dval[e][k] else None
                dval[e][k] += 16
                op["signal"] = (dsem[e][k], dval[e][k])
            elif op["sig"]:
                ep, v = divmod(ccount[e], SEM_EPOCH)
                ccount[e] += 1
                op["signal"] = (csem[e][ep], v + 1)
            else:
                op["signal"] = None
        per_eng = {e: [op for op in ops if op["eng"] == e] for e in engs}
        self.stats = {e: len(per_eng[e]) for e in engs}
        nwaits = {e: 0 for e in engs}

        def run(e, engine):
            waited = {}

            def wait(sem, val):
                key = id(sem)
                if waited.get(key, 0) >= val:
                    return
                waited[key] = val
                engine.wait_ge(sem, val)
                nwaits[e] += 1

            for op in per_eng[e]:
                need = {}
                for d in op["deps"]:
                    s, v = ops[d]["signal"]
                    k = id(s)
                    if k not in need or need[k][1] < v:
                        need[k] = (s, v)
                if op["dma"] and op["prev_slot"] is not None:
                    s, v = op["prev_slot"]
                    k = id(s)
                    if k not in need or need[k][1] < v:
                        need[k] = (s, v)
                for s, v in need.values():
                    wait(s, v)
                if op["emit"] is None:
                    if op["signal"] is not None:
                        engine.nop().then_inc(op["signal"][0], 1)
                    continue
                ins = op["emit"](engine)
                if op["signal"] is not None:
                    s, v = op["signal"]
                    ins.then_inc(s, 16 if op["dma"] else 1)
            if e in dsem:
                for k, s in enumerate(dsem[e]):
                    if dval[e][k]:
                        wait(s, dval[e][k])

        with nc.Block() as block:
            @block.tensor
            def _(eng):
                run(PE, eng)

            @block.scalar
            def _(eng):
                run(ACT, eng)

            @block.vector
            def _(eng):
                run(DVE, eng)

            @block.gpsimd
            def _(eng):
                run(POOL, eng)

            @block.sync
            def _(eng):
                run(SP, eng)
        self.nwaits = nwaits


def build_nc(debug=False):
    nc = bass.Bass("TRN2", target_bir_lowering=False)
    scr_kind = "ExternalOutput" if debug else "Internal"
    dt_in = lambda n, s: nc.dram_tensor(n, s, F32, kind="ExternalInput")
    x_d = dt_in("x", [SEQ, D_MODEL]).ap()
    g_mix_d = dt_in("g_mix", [1, D_MODEL])
    w_in_d = dt_in("w_in", [1, D_MODEL, 3840]).ap()[0]
    w_dw_d = dt_in("w_dw", [1, CW, 1, CONV_CH]).ap()
    b_dw_d = dt_in("b_dw", [1, CONV_CH]).ap()
    lng_d = dt_in("ln_conv_g", [1, CONV_CH]).ap()
    lnb_d = dt_in("ln_conv_b", [1, CONV_CH]).ap()
    sinks_d = dt_in("sinks", [1, 8]).ap()
    wc_d = dt_in("w_conv_out", [1, CONV_CH, D_MODEL]).ap()[0]
    wa_d = dt_in("w_attn_out", [1, 512, D_MODEL]).ap()[0]
    wo_d = dt_in("w_out", [1, D_MODEL, D_MODEL]).ap()[0]
    g_ffn_d = dt_in("g_ffn", [1, D_MODEL]).ap()
    wgrp_d = dt_in("w_group", [1, D_MODEL, 4]).ap()[0]
    bgrp_d = dt_in("b_group", [1, 4]).ap()
    wexp_d = dt_in("w_expert", [1, D_MODEL, N_EXP]).ap()[0]
    bexp_d = dt_in("b_expert", [1, N_EXP]).ap()
    wgate_d = dt_in("w_gate", [1, N_EXP, D_MODEL, DFF]).ap()[0]
    wup_d = dt_in("w_up", [1, N_EXP, D_MODEL, DFF]).ap()[0]
    wdown_d = dt_in("w_down", [1, N_EXP, DFF, D_MODEL]).ap()[0]
    g_fin_d = dt_in("g_final", [D_MODEL]).ap()
    out_d = nc.dram_tensor("out", [SEQ, D_MODEL], F32, kind="ExternalOutput").ap()
    hbuf_d = nc.dram_tensor("hbuf", [SEQ, D_MODEL], F32, kind=scr_kind).ap()
    xs_d = nc.dram_tensor("xs_scr", [N_EXP * CAP, D_MODEL], BF16, kind=scr_kind).ap()
    ys_d = nc.dram_tensor("ys_scr", [N_EXP * CAP, D_MODEL], BF16, kind=scr_kind).ap()
    wbf_d = nc.dram_tensor("wbf_scr", [N_EXP, 3, 128, 4096], BF16, kind="Internal").ap()

    with ExitStack() as st:
        def sb(name, shape, dtype):
            return st.enter_context(nc.sbuf_tensor(name, shape, dtype))

        P = Prog(nc)
        pbank = [st.enter_context(nc.psum_tensor("pb%d" % k, [128, 512], F32)) for k in range(8)]
        bank_ctr = [0]

        def nb():
            k = bank_ctr[0] % 8
            bank_ctr[0] += 1
            return k, pbank[k], ("pb", k)

        def MM(out, lhsT, rhs, start, stop, r, w):
            P.add(PE, lambda e: e.matmul(out, lhsT=lhsT, rhs=rhs, start=start, stop=stop), r, w)

        def TR32(out, in_, ident, r, w):
            P.add(PE, lambda e: e.transpose(out, in_, ident), r, w)

        def ACTV(out, in_, func, r, w, bias=None, scale=None, accum=None):
            kw = {}
            if bias is not None:
                kw["bias"] = bias
            if scale is not None:
                kw["scale"] = scale
            if accum is not None:
                kw["accum_out"] = accum
            P.add(ACT, lambda e: e.activation(out=out, in_=in_, func=func, **kw), r, w)

        def TT(eng, out, in0, in1, op, r, w):
            P.add(eng, lambda e: e.tensor_tensor(out=out, in0=in0, in1=in1, op=op), r, w)

        def TS(eng, out, in0, s1, s2, op0, op1, r, w, accum=None):
            if op1 is None:
                P.add(eng, lambda e: e.tensor_scalar(out=out, in0=in0, scalar1=s1, scalar2=None, op0=op0), r, w)
            elif accum is None:
                P.add(eng, lambda e: e.tensor_scalar(out=out, in0=in0, scalar1=s1, scalar2=s2, op0=op0, op1=op1), r, w)
            else:
                P.add(eng, lambda e: e.tensor_scalar(out=out, in0=in0, scalar1=s1, scalar2=s2, op0=op0, op1=op1,
                                                     accum_out=accum), r, w)

        def RSTD(dst, src, scale, rs, ws):
            P.add(ACT, lambda e: e.activation(out=dst, in_=src, func=AF.Sqrt, bias=epsc[:, 0:1], scale=scale), rs + ["epsc"], ws)
            P.add(DVE, lambda e: e.reciprocal(out=dst, in_=dst), ws, ws)

        def STT(eng, out, in0, scalar, in1, op0, op1, r, w):
            P.add(eng, lambda e: e.scalar_tensor_tensor(out=out, in0=in0, scalar=scalar, in1=in1, op0=op0, op1=op1), r, w)

        def CP(eng, out, in_, r, w):
            if eng == ACT:
                P.add(ACT, lambda e: e.activation(out=out, in_=in_, func=AF.Copy), r, w)
            else:
                P.add(eng, lambda e: e.tensor_copy(out=out, in_=in_), r, w)

        def RED(eng, out, in_, op, r, w):
            P.add(eng, lambda e: e.tensor_reduce(out=out, in_=in_, axis=AX.X, op=op), r, w)

        def DMA(eng, out, in_, r, w, **kw):
            P.add(eng, lambda e: e.dma_start(out=out, in_=in_, **kw), r, w, dma=True)

        def MEMSET(eng, ap, val, w):
            P.add(eng, lambda e: e.memset(ap, val), (), w)

        def TAP(name, ap, reads):
            if not debug:
                return
            shape = list(ap.shape)
            d = nc.dram_tensor("dbg_" + name, shape, F32, kind="ExternalOutput").ap()
            DMA(POOL, d, ap, reads, [("dbg", name)])

        identb = sb("identb", [128, 128], BF16)
        identf = sb("identf", [128, 128], F32)
        onesf = sb("onesf", [128, 128], F32)
        onesb = sb("onesb", [128, 128], BF16)
        ustrict = sb("ustrict", [128, 128], BF16)
        gffn_bc = sb("gffn_bc", [128, D_MODEL], F32)
        gmixT = sb("gmixT", [128, 8], F32)
        bdw = sb("bdw", [128, 4], F32)
        lng = sb("lng", [128, 4], F32)
        lnb = sb("lnb", [128, 4], F32)
        wdw_raw = sb("wdw_raw", [CW, CONV_CH], F32)
        wdwT = sb("wdwT", [128, 4, CW], F32)
        esink = sb("esink", [128, 8], F32)
        relt = sb("relt", [128, 2, 128], F32)
        amask = sb("amask", [128, 2, 128], F32)
        EM = sb("EM", [128, 4, 4, 128], BF16)
        wr = sb("wr", [128, 8, 36], F32)
        rbias = sb("rbias", [128, 36], F32)
        ebase = sb("ebase", [128, N_EXP], F32)
        cum = sb("cum", [128, N_EXP], F32)
        cumb2 = sb("cumb2", [128, 2, N_EXP], BF16)
        destall = sb("destall", [128, NTILE, 2], I32)
        wall = sb("wall", [128, NTILE, 2], F32)
        ss = sb("ss", [128, 8], F32)
        rstd = sb("rstd", [128, 8], F32)
        epsc = sb("epsc", [128, 1], F32)
        ztile = sb("ztile", [128, D_MODEL], BF16)
        ht2 = sb("ht2", [128, D_MODEL], F32)
        mtmp2 = sb("mtmp2", [128, 4, NT], F32)
        t1buf = sb("t1buf", [128, 2, NT], F32)

        ARENA_W = 44900
        arena = sb("arena", [128, ARENA_W], F32)
        off = [0]

        def carve(words, dtype, pattern=None, **kw):
            a = arena[:, off[0]:off[0] + words]
            off[0] += words
            assert off[0] <= ARENA_W, off[0]
            if dtype != F32:
                a = a.bitcast(dtype)
            if pattern:
                a = a.rearrange(pattern, **kw)
            return a

        wb_in = carve(15360, BF16, "p (k n) -> p k n", k=8)
        wcb = carve(2048, BF16, "p (k n) -> p k n", k=4)
        wab = carve(2048, BF16, "p (k n) -> p k n", k=4)
        wob = carve(4096, BF16, "p (k n) -> p k n", k=8)
        D64 = carve(3968, BF16, "p (c j m) -> p c j m", c=4, j=CW)
        xt = carve(4096, F32, "p (s n) -> p s n", s=4)
        xsb = carve(1024, BF16, "p (s n) -> p s n", s=2)
        xnT = carve(1024, BF16, "p (k n) -> p k n", k=8)
        vTs = carve(576, BF16, "p (c n) -> p c n", c=4)
        sig = carve(256, F32)
        qTs = carve(512, BF16, "p (c n) -> p c n", c=4)
        kTr = carve(192, BF16)
        vtok_raw = carve(200, BF16)
        vtok = vtok_raw[:, 0:390].rearrange("p (s k d) -> p s k d", s=3, k=2)
        RA = carve(1024, F32)
        RB = carve(1024, F32)
        emf = arena[:, off[0] - 2048:off[0]].rearrange("p (h k q) -> p h k q", h=8, k=2)
        RC = carve(1024, F32)
        RD = carve(1024, F32)
        pexp = carve(512, BF16, "p (b n) -> p b n", b=2)
        attn_tok = carve(256, BF16)
        mtmp = carve(1024, F32, "p (a n) -> p a n", a=4)
        mT = carve(1024, BF16, "p (k n) -> p k n", k=8)
        xn2b2 = [carve(512, BF16) for _ in range(2)]
        attnT = carve(512, BF16, "p (c n) -> p c n", c=4)
        rsm = carve(1024, F32)
        A_END = off[0]

        y32 = RA.rearrange("p (c n) -> p c n", c=4)
        xn2_ = [RA, RB]
        ybf = RB[:, 0:512].bitcast(BF16).rearrange("p (c n) -> p c n", c=4)
        ysq = RB[:, 512:1024].bitcast(BF16).rearrange("p (c n) -> p c n", c=4)
        cT = RB[:, 512:1024].bitcast(BF16).rearrange("p (c n) -> p c n", c=4)
        ln_mean = RC[:, 0:256]
        ln_rstd = RC[:, 256:512]
        ln_tmp = RC[:, 512:768]
        ln_t1 = RC[:, 768:1024]
        ht = RC
        PT = RD.bitcast(BF16).rearrange("p (g c n) -> p g c n", g=4, c=4)
        xn2T_ = [RD.rearrange("p (k n) -> p k n", k=8), mtmp.rearrange("p a n -> p (a n)").rearrange("p (k n) -> p k n", k=8)]

        MEMSET(DVE, onesf[:], 1.0, ["onesf"])
        MEMSET(DVE, onesb[:], 1.0, ["onesb"])
        P.add(POOL, lambda e: e.affine_select(out=identb[:], in_=onesf[:], pattern=[[-1, 128]], compare_op=ALU.is_equal,
                                              fill=0.0, base=0, channel_multiplier=1), ["onesf"], ["identb"])
        P.add(POOL, lambda e: e.affine_select(out=identf[:], in_=onesf[:], pattern=[[-1, 128]], compare_op=ALU.is_equal,
                                              fill=0.0, base=0, channel_multiplier=1), ["onesf"], ["identf"])
        P.add(POOL, lambda e: e.affine_select(out=ustrict[:], in_=onesf[:], pattern=[[1, 128]], compare_op=ALU.is_gt,
                                              fill=0.0, base=0, channel_multiplier=-1), ["onesf"], ["ustrict"])
        P.add(POOL, lambda e: e.iota(ebase[:], [[CAP, N_EXP]], base=0, channel_multiplier=0,
                                     allow_small_or_imprecise_dtypes=True), (), ["ebase"])
        for kb in range(2):
            P.add(POOL, lambda e, kb=kb: e.iota(relt[:, kb, :], [[1, 128]], base=128 * (1 - kb), channel_multiplier=-1,
                                                allow_small_or_imprecise_dtypes=True), (), ["relt"])
        P.add(POOL, lambda e: e.affine_select(out=amask[:, 0, :], in_=onesf[:], pattern=[[-1, 128]], compare_op=ALU.is_gt,
                                              fill=0.0, base=0, channel_multiplier=1), ["onesf"], ["amask"])
        P.add(POOL, lambda e: e.affine_select(out=amask[:, 1, :], in_=onesf[:], pattern=[[1, 128]], compare_op=ALU.is_ge,
                                              fill=0.0, base=0, channel_multiplier=-1), ["onesf"], ["amask"])
        win_v = w_in_d.rearrange("(k p) n -> p k n", p=128)

        def win_load(d0, s0, n):
            for kh in range(2):
                DMA(POOL, wb_in[:, 4 * kh:4 * kh + 4, d0:d0 + n], win_v[:, 4 * kh:4 * kh + 4, s0:s0 + n], [],
                    [("wb_in", d0, kh)])

        def win_res(col):
            if 1024 <= col < 1536:
                c_ = (col - 1024) // 128
                return [("wb_q", c_, 0), ("wb_q", c_, 1)]
            for d0, n in ((0, 512), (512, 512), (1536, 128), (3712, 128), (1664, 1024), (2688, 1024)):
                if d0 <= col < d0 + n:
                    return [("wb_in", d0, 0), ("wb_in", d0, 1)]
            raise ValueError(col)

        DMA(SP, gmixT[:], g_mix_d.ap()[0].rearrange("(c p) -> p c", p=128), [], ["gmixT"], allow_slow_non_contiguous=True)
        win_load(0, 0, 512)
        win_load(512, 512, 512)
        for c in range(4):
            for two in range(2):
                src0 = 1024 + 64 * (4 * two + c)
                DMA(POOL, wb_in[:, :, 1024 + 128 * c + 64 * two:1024 + 128 * c + 64 * two + 64],
                    win_v[:, :, src0:src0 + 64], [], [("wb_q", c, two)])
        win_load(1536, 1536, 128)
        win_load(3712, 1664, 128)
        win_load(1664, 1792, 1024)
        win_load(2688, 2816, 1024)
        DMA(POOL, wcb, wc_d.rearrange("(k p) n -> p k n", p=128), [], ["wcb"])
        DMA(POOL, wab, wa_d.rearrange("(k p) n -> p k n", p=128), [], ["wab"])
        for kh in range(2):
            DMA(POOL, wob[:, 4 * kh:4 * kh + 4, :], wo_d.rearrange("(k p) n -> p k n", p=128)[:, 4 * kh:4 * kh + 4, :],
                [], [("wob", kh)])
        MEMSET(DVE, cum[:], 0.0, ["cum"])
        MEMSET(DVE, epsc[:], EPS, ["epsc"])
        MEMSET(DVE, vtok_raw, 1.0, ["vtok0", "vtok1", "vtok2"])
        MEMSET(DVE, vTs, 0.0, ["vTs"])
        DMA(SP, gffn_bc[:], g_ffn_d[0].partition_broadcast(128), [], ["gffn_bc"])
        DMA(SP, bdw[:], b_dw_d[0].rearrange("(c p) -> p c", p=128), [], ["bdw"], allow_slow_non_contiguous=True)
        DMA(SP, lng[:], lng_d[0].rearrange("(c p) -> p c", p=128), [], ["lng"], allow_slow_non_contiguous=True)
        DMA(SP, lnb[:], lnb_d[0].rearrange("(c p) -> p c", p=128), [], ["lnb"], allow_slow_non_contiguous=True)
        DMA(SP, wdw_raw[:], w_dw_d[0].rearrange("j o c -> j (o c)"), [], ["wdw_raw"])
        DMA(SP, esink[:], sinks_d[0].partition_broadcast(128), [], ["esink"])
        DMA(SP, rbias[:, 0:4], bgrp_d[0].partition_broadcast(128), [], ["rbias"])
        DMA(SP, rbias[:, 4:36], bexp_d[0].partition_broadcast(128), [], ["rbias2"])
        DMA(SP, wr[:, :, 0:4], wgrp_d.rearrange("(k p) n -> p k n", p=128), [], ["wr"])
        DMA(SP, wr[:, :, 4:36], wexp_d.rearrange("(k p) n -> p k n", p=128), [], ["wr2"])
        ACTV(esink[:], esink[:], AF.Exp, ["esink"], ["esink"])
        k_, bk, br = nb()
        for c in range(4):
            TR32(bk[:, c * 32:c * 32 + CW], wdw_raw[:, c * 128:(c + 1) * 128], identf[0:CW, 0:CW],
                 ["wdw_raw", "identf"], [br])
        CP(DVE, wdwT[:], bk[:, 0:128].rearrange("p (c j) -> p c j", c=4)[:, :, 0:CW], [br], ["wdwT"])
        for c in range(4):
            for hf in range(2):
                sl = slice(64 * hf, 64 * hf + 64)
                TT(DVE, D64[sl, c, :, :], identb[sl, 64 * hf:64 * hf + 64].unsqueeze(1).broadcast_to([64, CW, 64]),
                   wdwT[sl, c, :].unsqueeze(2).broadcast_to([64, CW, 64]), ALU.mult, ["identb", "wdwT"],
                   [("D64", c, 0), ("D64", c, 1)])
        TS(DVE, relt[:], relt[:], 0.0, 128.0, ALU.max, ALU.min, ["relt"], ["relt"])
        def emf_res(h):
            if h < 4:
                return [("RA", h)]
            hh = h - 4
            return [("RB%d" % (hh // 2), 2 * (hh % 2)), ("RB%d" % (hh // 2), 2 * (hh % 2) + 1)]

        for h in range(8):
            ACTV(emf[:, h, :, :], relt[:], AF.Exp, ["relt"], emf_res(h), scale=-(2.0 ** (-(h + 1))))
        for kv in range(2):
            for kb in range(2):
                TT(DVE, EM[:, 2 * kv + kb, :, :], emf[:, 4 * kv:4 * kv + 4, kb, :],
                   amask[:, kb, :].unsqueeze(1).broadcast_to([128, 4, 128]), ALU.mult,
                   sum([emf_res(h_) for h_ in range(4 * kv, 4 * kv + 4)], []) + ["amask"], ["EM"])

        def prep(s):
            for i in range(TPS):
                g = s * TPS + i
                slot = g % 4
                DMA(SP, xt[:, slot, :], x_d[g * 128:(g + 1) * 128, :], [], [("xt", slot)])
                ACTV(xsb[:, i, :], xt[:, slot, :], AF.Square, [("xt", slot)], [("xsb", i), ("ss", i)],
                     accum=ss[:, i:i + 1])
                RSTD(rstd[:, i:i + 1], ss[:, i:i + 1], 1.0 / D_MODEL, [("ss", i)], [("rstd", i)])
                TS(DVE, xsb[:, i, :], xt[:, slot, :], rstd[:, i:i + 1], None, ALU.mult, None,
                   [("xt", slot), ("rstd", i)], [("xsb", i)])

        evac_flip = [0]

        def transposes(s):
            for i in range(TPS):
                for hf in range(2):
                    k_, bk, br = nb()
                    for q in range(4):
                        kk = 4 * hf + q
                        MM(bk[:, q * 128:(q + 1) * 128], xsb[:, i, kk * 128:(kk + 1) * 128], identb[:], True, True,
                           [("xsb", i), "identb"], [br])
                    for q in range(4):
                        kk = 4 * hf + q
                        evac_flip[0] ^= 1
                        if evac_flip[0]:
                            ACTV(xnT[:, kk, i * 128:(i + 1) * 128], bk[:, q * 128:(q + 1) * 128], AF.Copy,
                                 [br, "gmixT"], ["xnT"], scale=gmixT[:, kk:kk + 1])
                        else:
                            TS(DVE, xnT[:, kk, i * 128:(i + 1) * 128], bk[:, q * 128:(q + 1) * 128],
                               gmixT[:, kk:kk + 1], None, ALU.mult, None, [br, "gmixT"], ["xnT"])

        def proj_chunk(j):
            k_, bk, br = nb()
            wres = win_res(j * 128)
            for k in range(8):
                MM(bk[:, 0:NT], wb_in[:, k, j * 128:(j + 1) * 128], xnT[:, k, :], k == 0, k == 7, wres + ["xnT"], [br])
            return bk, br

        def inproj_glu(s):
            if s > 0:
                CP(POOL, vTs[:, :, 0:30], vTs[:, :, NT:NT + 30], ["vTs"], ["vTs"])
                CP(POOL, kTr[:, 0:128], kTr[:, NT:NT + 128], ["kTr"], ["kTr"])
                CP(POOL, vtok[:, 0, :, :], vtok[:, TPS, :, :], ["vtok%d" % TPS], ["vtok0"])
            for j in range(4):
                ba, bar = proj_chunk(j)
                bg, bgr = proj_chunk(4 + j)
                ACTV(sig, bg[:, 0:NT], AF.Sigmoid, [bgr], ["sig"])
                TT(DVE, vTs[:, j, 30:30 + NT], ba[:, 0:NT], sig, ALU.mult, [bar, "sig"], ["vTs"])

        def inproj_qkv(s):
            for c in range(4):
                bq, bqr = proj_chunk(8 + c)
                CP(ACT, qTs[:, c, :], bq[:, 0:NT], [bqr], ["qTs"])
            bk_, bkr = proj_chunk(12)
            CP(DVE, kTr[:, 128:128 + NT], bk_[:, 0:NT], [bkr], ["kTr"])
            wres = win_res(3712)
            for i in range(TPS):
                k_, bv, bvr = nb()
                for k in range(8):
                    MM(bv[:, 0:128], xnT[:, k, i * 128:(i + 1) * 128], wb_in[:, k, 3712:3840], k == 0, k == 7,
                       ["xnT"] + wres, [bvr])
                CP(ACT, vtok[:, 1 + i, :, 0:64], bv[:, 0:128].rearrange("p (k d) -> p k d", k=2), [bvr],
                   ["vtok%d" % (1 + i)])

        def conv_chunk(s, c):
            banks = []
            for hf in range(2):
                k_, bk, br = nb()
                banks.append((bk, br))
            for j in range(CW):
                for hf in range(2):
                    bk, br = banks[hf]
                    MM(bk[64 * hf:64 * hf + 64, 0:NT], D64[64 * hf:64 * hf + 64, c, j, :],
                       vTs[64 * hf:64 * hf + 64, c, j:j + NT], j == 0, j == CW - 1, [("D64", c, 0), ("D64", c, 1), "vTs"], [br])
            for hf in range(2):
                bk, br = banks[hf]
                sl = slice(64 * hf, 64 * hf + 64)
                ACTV(y32[sl, c, :], bk[sl, 0:NT], AF.Identity, [br, "bdw"], [("RA", c)], bias=bdw[sl, c:c + 1])
                ACTV(ysq[sl, c, :], bk[sl, 0:NT], AF.Square, [br, "bdw"], [("RB1", c)], bias=bdw[sl, c:c + 1])
            CP(DVE, ybf[:, c, :], y32[:, c, :], [("RA", c)], [("RB0", c)])

        def ln(s):
            k1, b1, b1r = nb()
            k2, b2, b2r = nb()
            for c in range(4):
                MM(b1[:, 0:NT], onesb[:], ybf[:, c, :], c == 0, c == 3, ["onesb", ("RB0", c)], [b1r])
            for c in range(4):
                MM(b2[:, 0:NT], onesb[:], ysq[:, c, :], c == 0, c == 3, ["onesb", ("RB1", c)], [b2r])
            TS(DVE, ln_mean, b1[:, 0:NT], 1.0 / CONV_CH, None, ALU.mult, None, [b1r], ["RC0"])
            TT(DVE, ln_tmp, ln_mean, ln_mean, ALU.mult, ["RC0"], ["RC2"])
            STT(DVE, ln_rstd, b2[:, 0:NT], 1.0 / CONV_CH, ln_tmp, ALU.mult, ALU.subtract, [b2r, "RC2"], ["RC1"])
            RSTD(ln_rstd, ln_rstd, 1.0, ["RC1"], ["RC1"])
            for c in range(4):
                TT(DVE, ln_t1, y32[:, c, :], ln_mean, ALU.subtract, [("RA", c), "RC0"], ["RC3"])
                TT(DVE, ln_t1, ln_t1, ln_rstd, ALU.mult, ["RC3", "RC1"], ["RC3"])
                ACTV(cT[:, c, :], ln_t1, AF.Silu, ["RC3", "lng", "lnb", ("RB1", c)], [("RB1", c)],
                     scale=lng[:, c:c + 1], bias=lnb[:, c:c + 1])

        def attn_qk(s, i):
            n = s * TPS + i
            qb = i * 128
            kbs = [1] if n == 0 else [0, 1]
            for kv in range(2):
                sl = slice(64 * kv, 64 * kv + 64)
                for kb in kbs:
                    k_, bk, br = nb()
                    kc = qb + 128 * kb
                    MM(bk[:, :].rearrange("p (c q) -> p c q", c=4), kTr[sl, kc:kc + 128], qTs[sl, :, qb:qb + 128],
                       True, True, ["kTr", "qTs"], [br])
                    pe_ = ("pexp", kb)
                    ACTV(pexp[:, kb, :], bk[:, :], AF.Exp, [br], [pe_], scale=0.125)
                    TT(DVE, PT[:, 2 * kv + kb, :, :], pexp[:, kb, :].rearrange("p (c q) -> p c q", c=4),
                       EM[:, 2 * kv + kb, :, :], ALU.mult, [pe_, "EM"], [("RD", 2 * kv + kb)])

        def attn_pv(s, i):
            n = s * TPS + i
            kbs = [1] if n == 0 else [0, 1]
            for kv in range(2):
                k_, bo, bor = nb()
                ov = bo[:, 0:260].rearrange("p (c d) -> p c d", c=4)
                for c in range(4):
                    for kb in kbs:
                        MM(ov[:, c, :], PT[:, 2 * kv + kb, c, :], vtok[:, i + kb, kv, :], kb == kbs[0], kb == 1,
                           [("RD", 2 * kv + kb), "vtok%d" % (i + kb)], [bor])
                den = rsm[:, 4 * kv:4 * kv + 4]
                dr = ("den", kv)
                TT(DVE, den, ov[:, :, 64], esink[:, 4 * kv:4 * kv + 4], ALU.add, [bor, "esink"], [dr])
                P.add(DVE, lambda e, den=den: e.reciprocal(out=den, in_=den), [dr], [dr])
                TT(DVE, attn_tok[:, 256 * kv:256 * kv + 256].rearrange("p (c d) -> p c d", c=4), ov[:, :, 0:64],
                   den.unsqueeze(2).broadcast_to([128, 4, 64]), ALU.mult, [bor, dr], [("attn_tok", kv)])

        def attn_T(s, i):
            qb = i * 128
            k_, bt, btr = nb()
            for c in range(4):
                MM(bt[:, c * 128:(c + 1) * 128], attn_tok[:, c * 128:(c + 1) * 128], identb[:], True, True,
                   [("attn_tok", c // 2), "identb"], [btr])
            CP(ACT, attnT[:, :, qb:qb + 128], bt[:, :].rearrange("p (c q) -> p c q", c=4), [btr], ["attnT"])

        def merge(s):
            sg_c = [mtmp[:, 0, :], mtmp[:, 2, :]]
            sg_a = [mtmp[:, 1, :], mtmp[:, 3, :]]
            sg_cr = ["sgc", "t1"]
            sg_ar = ["sga", "t2"]
            t2s = [mtmp2[:, jj, :] for jj in range(4)]
            t2r = [("t2s", jj) for jj in range(4)]
            for i in range(2):
                v_ = xn2b2[i].bitcast(F32).rearrange("p (a n) -> p a n", a=2)
                t2s += [v_[:, 0, :], v_[:, 1, :]]
                t2r += [("xn2b", i), ("xn2b", i)]
            for j in range(8):
                k_, bb, bbr = nb()
                for k in range(4):
                    MM(bb[:, 0:NT], wab[:, k, j * 128:(j + 1) * 128], attnT[:, k, :], k == 0, k == 3, ["wab", "attnT"], [bbr])
                bd, bdr = proj_chunk(21 + j)
                ACTV(sg_a[j % 2], bd[:, 0:NT], AF.Sigmoid, [bdr], [sg_ar[j % 2]])
                TT(DVE, t2s[j], bb[:, 0:NT], sg_a[j % 2], ALU.mult, [bbr, sg_ar[j % 2]], [t2r[j]])
            for j in range(8):
                k_, ba, bar = nb()
                for k in range(4):
                    MM(ba[:, 0:NT], wcb[:, k, j * 128:(j + 1) * 128], cT[:, k, :], k == 0, k == 3,
                       ["wcb", ("RB1", k)], [bar])
                bc, bcr = proj_chunk(13 + j)
                ACTV(sg_c[j % 2], bc[:, 0:NT], AF.Sigmoid, [bcr], [sg_cr[j % 2]])
                TT(DVE, t1buf[:, j % 2, :], ba[:, 0:NT], sg_c[j % 2], ALU.mult, [bar, sg_cr[j % 2]], [("t1b", j % 2)])
                TT(POOL, mT[:, j, :], t1buf[:, j % 2, :], t2s[j], ALU.add, [("t1b", j % 2), t2r[j]], [("mT", j)])

        RAr = [("RA", c) for c in range(4)]
        RBr = [("RB0", c) for c in range(4)] + [("RB1", c) for c in range(4)]
        RCr = ["RC0", "RC1", "RC2", "RC3"]
        RDr = [("RD", q) for q in range(4)]
        MTr = ["sgc", "sga", "t1", "t2"]
        xn2_res = [RAr, RBr]
        hts = [ht, ht2[:]]
        htr = [RCr, ["ht2"]]
        xn2T_res = [RDr, MTr]

        def tail_wout(s):
            mres = [("mT", j) for j in range(8)]
            for i in range(TPS):
                g = s * TPS + i
                slot = g % 4
                for hf in range(2):
                    k_, bk, br = nb()
                    for k in range(8):
                        MM(bk[:, :], mT[:, k, i * 128:(i + 1) * 128], wob[:, k, hf * 512:(hf + 1) * 512], k == 0, k == 7,
                           mres + [("wob", 0), ("wob", 1)], [br])
                    TT(DVE, hts[i][:, hf * 512:(hf + 1) * 512], bk[:, :], xt[:, slot, hf * 512:(hf + 1) * 512], ALU.add,
                       [br, ("xt", slot)], htr[i])
                DMA(SP, hbuf_d[g * 128:(g + 1) * 128, :], hts[i], htr[i], [("hbuf", g)])

        def tail_wout_b(s):
            for i in range(TPS):
                xb = xn2b2[i]
                ACTV(xb, hts[i], AF.Square, htr[i], [("xn2b", i), ("ss2", i)], accum=ss[:, 2 + i:3 + i])
                RSTD(rstd[:, 2 + i:3 + i], ss[:, 2 + i:3 + i], 1.0 / D_MODEL, [("ss2", i)], [("rstd2", i)])
                STT(DVE, xn2_[i], hts[i], rstd[:, 2 + i:3 + i], gffn_bc[:], ALU.mult, ALU.mult,
                    htr[i] + [("rstd2", i), "gffn_bc"], xn2_res[i])
                CP(ACT, xb, xn2_[i], xn2_res[i], [("xn2b", i)])

        def tail_rtr_T(s):
            for i in range(TPS):
                for hf in range(2):
                    k_, bk, br = nb()
                    for q in range(4):
                        kk = 4 * hf + q
                        TR32(bk[:, q * 128:(q + 1) * 128], xn2_[i][:, kk * 128:(kk + 1) * 128], identf[:],
                             xn2_res[i] + ["identf"], [br])
                    if hf == 0:
                        CP(ACT, xn2T_[i][:, 0:4, :], bk[:, :].rearrange("p (k n) -> p k n", k=4), [br], xn2T_res[i][0:2])
                    else:
                        CP(DVE, xn2T_[i][:, 4:8, :], bk[:, :].rearrange("p (k n) -> p k n", k=4), [br], xn2T_res[i][2:4])

        def rfields(i):
            o = 8 + 500 * i
            f = {}
            names = [("lg", 36), ("gmax", 1), ("ngmax", 1), ("gexp", 4), ("gsum", 1), ("pgrp", 1), ("gone", 4), ("pen", 4),
                     ("em", 32), ("m1", 1), ("one1", 32), ("em2", 32), ("m2", 1), ("one2", 32), ("dlt", 1), ("w1", 1),
                     ("ind", 32), ("indb", 16), ("pos", 32), ("tmp", 32), ("d12", 2)]
            for nm, w_ in names:
                f[nm] = rsm[:, o:o + w_]
                o += w_
            assert o <= 8 + 500 * (i + 1)
            return f

        tail_state = {}

        def tail_logits(s):
            for i in range(TPS):
                k_, bl, blr = nb()
                for k in range(8):
                    MM(bl[:, 0:36], xn2T_[i][:, k, :], wr[:, k, :], k == 0, k == 7, xn2T_res[i] + ["wr", "wr2"], [blr])
                TT(DVE, rfields(i)["lg"], bl[:, 0:36], rbias[:], ALU.add, [blr, "rbias", "rbias2"], [("lg", i)])

        def tail_route(s, i):
            if True:
                g = s * TPS + i
                F = rfields(i)
                R = lambda nm: (nm, i)
                lg, gmax, ngmax, gexp, gsum, pgrp = F["lg"], F["gmax"], F["ngmax"], F["gexp"], F["gsum"], F["pgrp"]
                gone, pen, em, m1, one1, em2, m2, one2 = F["gone"], F["pen"], F["em"], F["m1"], F["one1"], F["em2"], F["m2"], F["one2"]
                dlt, w1, ind = F["dlt"], F["w1"], F["ind"]
                indb = F["indb"].bitcast(BF16)
                RED(DVE, gmax, lg[:, 0:4], ALU.max, [R("lg")], [R("gmax")])
                TS(DVE, ngmax, gmax, -1.0, None, ALU.mult, None, [R("gmax")], [R("ngmax")])
                ACTV(gexp, lg[:, 0:4], AF.Exp, [R("lg"), R("ngmax")], [R("gexp"), R("gsum")], bias=ngmax, accum=gsum)
                P.add(DVE, lambda e, pgrp=pgrp, gsum=gsum: e.reciprocal(out=pgrp, in_=gsum), [R("gsum")], [R("pgrp")])
                TS(DVE, gone, lg[:, 0:4], gmax, None, ALU.is_equal, None, [R("lg"), R("gmax")], [R("gone")])
                TS(DVE, pen, gone, -1.0, BIG, ALU.add, ALU.mult, [R("gone")], [R("pen")])
                TT(DVE, em.rearrange("p (g j) -> p g j", g=4), lg[:, 4:36].rearrange("p (g j) -> p g j", g=4),
                   pen.unsqueeze(2).broadcast_to([128, 4, 8]), ALU.add, [R("lg"), R("pen")], [R("em")])
                RED(DVE, m1, em, ALU.max, [R("em")], [R("m1")])
                TS(DVE, one1, em, m1, None, ALU.is_equal, None, [R("em"), R("m1")], [R("one1")])
                STT(DVE, em2, one1, -BIG, em, ALU.mult, ALU.add, [R("one1"), R("em")], [R("em2")])
                RED(DVE, m2, em2, ALU.max, [R("em2")], [R("m2")])
                TS(DVE, one2, em2, m2, None, ALU.is_equal, None, [R("em2"), R("m2")], [R("one2")])
                TT(DVE, dlt, m2, m1, ALU.subtract, [R("m1"), R("m2")], [R("dlt")])
                ACTV(dlt, dlt, AF.Exp, [R("dlt")], [R("dlt")])
                TS(DVE, dlt, dlt, 1.0, None, ALU.add, None, [R("dlt")], [R("dlt")])
                P.add(DVE, lambda e, w1=w1, dlt=dlt: e.reciprocal(out=w1, in_=dlt), [R("dlt")], [R("w1")])
                TT(DVE, wall[:, g, 0:1], w1, pgrp, ALU.mult, [R("w1"), R("pgrp")], [("wall", g)])
                TT(DVE, wall[:, g, 1:2], pgrp, wall[:, g, 0:1], ALU.subtract, [R("pgrp"), ("wall", g)], [("wall", g)])
                TT(DVE, ind, one1, one2, ALU.add, [R("one1"), R("one2")], [R("ind")])
                CP(DVE, indb, ind, [R("ind")], [R("indb")])

        def tail_pos(s):
            for i in range(TPS):
                g = s * TPS + i
                F = rfields(i)
                R = lambda nm: (nm, i)
                ind, pos, tmp, one1, one2, d12 = F["ind"], F["pos"], F["tmp"], F["one1"], F["one2"], F["d12"]
                indb = F["indb"].bitcast(BF16)
                cb_ = cumb2[:, i, :]
                CP(DVE, cb_, cum[:], ["cum"], [("cumb", i)])
                k_, bp, bpr = nb()
                MM(bp[:, 0:32], ustrict[:], indb, True, False, ["ustrict", R("indb")], [bpr])
                MM(bp[:, 0:32], onesb[:], cb_, False, True, ["onesb", ("cumb", i)], [bpr])
                TT(DVE, cum[:], cum[:], ind, ALU.add, ["cum", R("ind")], ["cum"])
                TS(DVE, pos, bp[:, 0:32], float(CAP - 1), None, ALU.min, None, [bpr], [R("pos")])
                TT(DVE, pos, pos, ebase[:], ALU.add, [R("pos"), "ebase"], [R("pos")])
                TT(DVE, tmp, pos, one1, ALU.mult, [R("pos"), R("one1")], [R("tmp")])
                RED(DVE, d12[:, 0:1], tmp, ALU.add, [R("tmp")], [R("d1f")])
                TT(DVE, tmp, pos, one2, ALU.mult, [R("pos"), R("one2")], [R("tmp")])
                RED(DVE, d12[:, 1:2], tmp, ALU.add, [R("tmp")], [R("d2f")])
                CP(DVE, destall[:, g, :], d12, [R("d1f"), R("d2f")], [("dest", g)])
                zres = [("xs_z", r0) for r0 in range(0, N_EXP * CAP, 1024)]
                for k in range(2):
                    P.add(POOL, lambda e, g=g, k=k, i=i: e.indirect_dma_start(
                        out=xs_d, out_offset=bass.IndirectOffsetOnAxis(ap=destall[:, g, k:k + 1], axis=0),
                        in_=xn2b2[i], in_offset=None), [("xn2b", i), ("dest", g)] + zres, [("xs_scr", g, k)], dma=True)

        def precast(e):
            srcs = (wgate_d[e].rearrange("(k p) n -> p k n", p=128), wup_d[e].rearrange("(k p) n -> p k n", p=128),
                    wdown_d[e].rearrange("(k p) n -> p k n", p=128))
            for m_, src in enumerate(srcs):
                kk = src.shape[1]
                dst = wbf_d[e, m_].rearrange("p (k n) -> p k n", k=kk)
                DMA(POOL, dst, src, [], [("wbf", e, m_)])

        def zero_fill():
            MEMSET(DVE, ztile[:], 0.0, ["ztile"])
            for r0 in range(0, N_EXP * CAP, 1024):
                DMA(POOL, xs_d[r0:r0 + 1024, :].rearrange("(b p) n -> p b n", p=128),
                    ztile[:].unsqueeze(1).broadcast_to([128, 8, D_MODEL]), ["ztile"], [("xs_z", r0)])

        prep(0)
        for s in range(NST):
            transposes(s)
            if s > 0:
                tail_wout(s - 1)
            inproj_glu(s)
            if s > 0:
                tail_wout_b(s - 1)
            inproj_qkv(s)
            if s == 0:
                zero_fill()
            if s + 1 < NST:
                prep(s + 1)
            if s > 0:
                tail_rtr_T(s - 1)
            conv_chunk(s, 0)
            if s > 0:
                tail_logits(s - 1)
                tail_route(s - 1, 0)
            attn_qk(s, 0)
            conv_chunk(s, 1)
            if s > 0:
                tail_route(s - 1, 1)
            attn_pv(s, 0)
            attn_qk(s, 1)
            conv_chunk(s, 2)
            attn_T(s, 0)
            attn_pv(s, 1)
            if s > 0:
                tail_pos(s - 1)
            conv_chunk(s, 3)
            attn_T(s, 1)
            ln(s)
            merge(s)
            for e_ in range(s * (N_EXP // NST), (s + 1) * (N_EXP // NST)):
                precast(e_)
        tail_wout(NST - 1)
        tail_wout_b(NST - 1)
        tail_rtr_T(NST - 1)
        tail_logits(NST - 1)
        tail_route(NST - 1, 0)
        tail_route(NST - 1, 1)
        tail_pos(NST - 1)

        P.barrier()
        off[0] = 0
        NWB = 4
        wexp = []
        for b in range(NWB):
            wg = carve(2048, BF16, "p (k n) -> p k n", k=8)
            wu = carve(2048, BF16, "p (k n) -> p k n", k=8)
            wd = carve(2048, BF16, "p (k n) -> p k n", k=4)
            wexp.append((wg, wu, wd))
        NXS = 3
        xsr = [carve(2048, BF16, "p (b n) -> p b n", b=4) for _ in range(NXS)]
        xsT = [carve(4 * CAP, BF16, "p (k n) -> p k n", k=8) for _ in range(2)]
        sgt = [carve(CAP, F32) for _ in range(2)]
        hT = [carve(2 * CAP, BF16, "p (f n) -> p f n", f=4) for _ in range(2)]
        NYB = 4
        ysb = [carve(512, BF16) for _ in range(NYB)]
        B_END = off[0]

        def load_weights(e):
            b = e % NWB
            wg, wu, wd = wexp[b]
            DMA(SP, wg.rearrange("p k n -> p (k n)"), wbf_d[e, 0], [], [("wg", b, 0), ("wg", b, 1)])
            DMA(SP, wu.rearrange("p k n -> p (k n)"), wbf_d[e, 1], [], [("wu", b, 0), ("wu", b, 1)])
            DMA(SP, wd.rearrange("p k n -> p (k n)"), wbf_d[e, 2], [], [("wd", b, 0), ("wd", b, 1)])

        def load_xs(e):
            b = e % NXS
            DMA(SP, xsr[b][:, 0:NFULL, :], xs_d[e * CAP:e * CAP + 128 * NFULL, :].rearrange("(b p) n -> p b n", p=128), [],
                [("xsr", b)])
            if CAP > 128 * NFULL:
                DMA(SP, xsr[b][0:CAP - 128 * NFULL, NFULL, :], xs_d[e * CAP + 128 * NFULL:(e + 1) * CAP, :], [], [("xsrt", b)])

        def exp_transposes(e):
            b = e % NXS
            xT = xsT[e % 2]
            for blk, (s0, sz) in enumerate(BLKS):
                for hf in range(2):
                    k_, bk, br = nb()
                    for q in range(4):
                        kk = 4 * hf + q
                        MM(bk[:, q * sz:(q + 1) * sz], xsr[b][0:sz, blk, kk * 128:(kk + 1) * 128], identb[0:sz, 0:sz], True, True,
                           [("xsr", b), ("xsrt", b), "identb"], [br])
                    dst = xT[:, 4 * hf:4 * hf + 4, s0:s0 + sz]
                    src = bk[:, 0:4 * sz].rearrange("p (k n) -> p k n", k=4)
                    if hf == 0:
                        CP(ACT, dst, src, [br], [("xsT", e % 2)])
                    else:
                        CP(DVE, dst, src, [br], [("xsT", e % 2)])

        def exp_gateup(e):
            b = e % NWB
            wg, wu, wd = wexp[b]
            xT = xsT[e % 2]
            h_ = hT[e % 2]
            for f in range(4):
                k_, bg, bgr = nb()
                k_, bu, bur = nb()
                for k in range(8):
                    MM(bg[:, 0:CAP], wg[:, k, f * 128:(f + 1) * 128], xT[:, k, :], k == 0, k == 7,
                       [("wg", b, 0), ("wg", b, 1), ("xsT", e % 2)], [bgr])
                for k in range(8):
                    MM(bu[:, 0:CAP], wu[:, k, f * 128:(f + 1) * 128], xT[:, k, :], k == 0, k == 7,
                       [("wu", b, 0), ("wu", b, 1), ("xsT", e % 2)], [bur])
                sg_ = sgt[f % 2]
                ACTV(sg_, bg[:, 0:CAP], AF.Silu, [bgr], [("sgt", f % 2)])
                TT(DVE, h_[:, f, :], bu[:, 0:CAP], sg_, ALU.mult, [bur, ("sgt", f % 2)], [("hT", e % 2, f)])

        ycount = [0]

        def exp_down(e):
            b = e % NWB
            wg, wu, wd = wexp[b]
            h_ = hT[e % 2]
            for blk, (s0, sz) in enumerate(BLKS):
                yb = ysb[ycount[0] % NYB]
                yr = ("ysb", ycount[0] % NYB)
                ycount[0] += 1
                for hf in range(2):
                    k_, bk, br = nb()
                    for f in range(4):
                        MM(bk[0:sz, :], h_[:, f, s0:s0 + sz], wd[:, f, hf * 512:(hf + 1) * 512], f == 0, f == 3,
                           [("hT", e % 2, f_) for f_ in range(4)] + [("wd", b, 0), ("wd", b, 1)], [br])
                    if hf == 0:
                        CP(ACT, yb[0:sz, 0:512], bk[0:sz, :], [br], [yr])
                    else:
                        CP(DVE, yb[0:sz, 512:1024], bk[0:sz, :], [br], [yr])
                r0 = e * CAP + s0
                DMA(SP, ys_d[r0:r0 + sz, :], yb[0:sz, :], [yr], [("ys_scr", e, blk)])

        for e in range(3):
            load_weights(e)
            load_xs(e) if e < 3 else None
        exp_transposes(0)
        for e in range(N_EXP):
            if e + 3 < N_EXP:
                load_weights(e + 3)
            exp_gateup(e)
            if e + 1 < N_EXP:
                exp_transposes(e + 1)
            if e + 3 < N_EXP:
                load_xs(e + 3)
            exp_down(e)

        P.barrier()
        off[0] = 0
        gfin_bc = carve(1024, F32)
        NCB = 8
        cb = []
        for b in range(NCB):
            cb.append(dict(h=carve(1024, F32), y1=carve(512, BF16), y2=carve(512, BF16), o=carve(1024, F32)))
        DMA(SP, gfin_bc, g_fin_d.partition_broadcast(128), [], ["gfin_bc"])

        def c_load(g):
            b = g % NCB
            B_ = cb[b]
            DMA(SP, B_["h"], hbuf_d[g * 128:(g + 1) * 128, :], [], [("ch", b)])
            for k, key in ((0, "y1"), (1, "y2")):
                P.add(POOL, lambda e, g=g, k=k, dstt=B_[key]: e.indirect_dma_start(
                    out=dstt, out_offset=None, in_=ys_d,
                    in_offset=bass.IndirectOffsetOnAxis(ap=destall[:, g, k:k + 1], axis=0)),
                    [], [("c" + key, b)], dma=True)

        for g in range(NCB - 1):
            c_load(g)
        for g in range(NTILE):
            b = g % NCB
            B_ = cb[b]
            if g + NCB - 1 < NTILE:
                c_load(g + NCB - 1)
            STT(DVE, B_["h"], B_["y1"], wall[:, g, 0:1], B_["h"], ALU.mult, ALU.add, [("cy1", b), ("ch", b)], [("ch", b)])
            STT(DVE, B_["h"], B_["y2"], wall[:, g, 1:2], B_["h"], ALU.mult, ALU.add, [("cy2", b), ("ch", b)], [("ch", b)])
            ACTV(B_["o"], B_["h"], AF.Square, [("ch", b)], [("co", b), ("ss3", g % 2)], accum=ss[:, 5 + g % 2:6 + g % 2])
            RSTD(rstd[:, 5 + g % 2:6 + g % 2], ss[:, 5 + g % 2:6 + g % 2], 1.0 / D_MODEL, [("ss3", g % 2)], [("rstd3", g % 2)])
            STT(DVE, B_["o"], B_["h"], rstd[:, 5 + g % 2:6 + g % 2], gfin_bc, ALU.mult, ALU.mult,
                [("ch", b), ("rstd3", g % 2), "gfin_bc"], [("co", b)])
            DMA(SP, out_d[g * 128:(g + 1) * 128, :], B_["o"], [("co", b)], [("out", g)])

        P.emit(st)
        build_nc.stats = (P.stats, P.nwaits, A_END, B_END, off[0])
    return nc


_NC_CACHE = {}


def kernel(**inputs):
    x = np.ascontiguousarray(np.asarray(inputs["x"], dtype=np.float32))
    names = ["g_mix", "w_in", "w_dw", "b_dw", "ln_conv_g", "ln_conv_b", "sinks", "w_conv_out", "w_attn_out", "w_out",
             "g_ffn", "w_group", "b_group", "w_expert", "b_expert", "w_gate", "w_up", "w_down", "g_final"]
    shared = {n: np.ascontiguousarray(np.asarray(inputs[n], dtype=np.float32)) for n in names}
    if "nc" not in _NC_CACHE:
        _NC_CACHE["nc"] = build_nc()
    nc = _NC_CACHE["nc"]
    in_maps = []
    for c in range(8):
        m = dict(shared)
        m["x"] = x[c]
        in_maps.append(m)
    res = run_bass_kernel_spmd(nc, in_maps, core_ids=list(range(8)))
    return np.stack([np.asarray(r["out"], dtype=np.float32) for r in res.results], axis=0)
```

```python
import numpy as np
from contextlib import ExitStack
import concourse.bass as bass
import concourse.mybir as mybir
from concourse.bass_utils import run_bass_kernel_spmd

F32 = mybir.dt.float32
BF16 = mybir.dt.bfloat16
I32 = mybir.dt.int32
AF = mybir.ActivationFunctionType
ALU = mybir.AluOpType
AX = mybir.AxisListType

PE, ACT, DVE, POOL, SP = "pe", "act", "dve", "pool", "sp"

D_MODEL = 1024
SEQ = 4096
NTILE = SEQ // 128
NT = 256
TPS = NT // 128
NST = SEQ // NT
CONV_CH = 512
CW = 31
N_EXP = 32
CAP = 384
BLKS = [(b0, min(128, CAP - b0)) for b0 in range(0, CAP, 128)]
NFULL = CAP // 128
DFF = 512
EPS = 1e-6
BIG = 1.0e30
SEM_EPOCH = 20000


class Prog:
    def __init__(self, nc, n_dma_sems=8):
        self.nc = nc
        self.ops = []
        self.n_dma_sems = n_dma_sems
        self.last_w = {}
        self.readers = {}
        self.last_op = {}
        self.recent_dma = {}

    def add(self, eng, emit, reads=(), writes=(), dma=False):
        ops = self.ops
        i = len(ops)
        op = dict(eng=eng, emit=emit, dma=dma, deps=set(), sig=False)
        deps = set()
        for r in reads:
            w = self.last_w.get(r)
            if w is not None:
                deps.add((w, False))
        for wr in writes:
            w = self.last_w.get(wr)
            if w is not None:
                deps.add((w, False))
            for rd in self.readers.get(wr, ()):
                deps.add((rd, True))
        for r in reads:
            self.readers.setdefault(r, []).append(i)
        for wr in writes:
            self.last_w[wr] = i
            self.readers[wr] = []
        final = set()
        for d, war in deps:
            if d == i:
                continue
            p = ops[d]
            if not p["dma"] and not dma and p["eng"] == eng:
                if eng == PE:
                    continue
                if war:
                    continue
            final.add(d)
        op["deps"] = final
        for d in final:
            ops[d]["sig"] = True
        ops.append(op)
        self.last_op[eng] = i
        if dma:
            self.recent_dma.setdefault(eng, []).append(i)
            self.recent_dma[eng] = self.recent_dma[eng][-self.n_dma_sems:]
        return i

    def barrier(self):
        deps = set(self.last_op.values())
        for lst in self.recent_dma.values():
            deps.update(lst)
        for e in (PE, ACT, DVE, POOL, SP):
            i = len(self.ops)
            op = dict(eng=e, emit=None, dma=False, deps=set(deps), sig=False)
            self.ops.append(op)
            self.last_op[e] = i
        for d in deps:
            self.ops[d]["sig"] = True
        self.last_w = {}
        self.readers = {}

    def emit(self, stack):
        nc = self.nc
        ops = self.ops
        engs = [PE, ACT, DVE, POOL, SP]
        nsig = {e: sum(1 for o in ops if o["eng"] == e and o["sig"] and not o["dma"]) for e in engs}
        csem = {e: [stack.enter_context(nc.semaphore("c_%s%d" % (e, k)))
                    for k in range(nsig[e] // SEM_EPOCH + 1)] for e in engs}
        dsem = {e: [stack.enter_context(nc.semaphore("d_%s%d" % (e, k)))
                    for k in range(self.n_dma_sems)] for e in (ACT, POOL, SP)}
        ccount = {e: 0 for e in engs}
        dcount = {e: 0 for e in dsem}
        dval = {e: [0] * self.n_dma_sems for e in dsem}
        for op in ops:
            e = op["eng"]
            if op["dma"]:
                k = dcount[e] % self.n_dma_sems
                dcount[e] += 1
                op["prev_slot"] = (dsem[e][k], dval[e][k]) if dval[e][k] else None
                dval[e][k] += 16
                op["signal"] = (dsem[e][k], dval[e][k])
            elif op["sig"]:
                ep, v = divmod(ccount[e], SEM_EPOCH)
                ccount[e] += 1
                op["signal"] = (csem[e][ep], v + 1)
            else:
                op["signal"] = None
        per_eng = {e: [op for op in ops if op["eng"] == e] for e in engs}
        self.stats = {e: len(per_eng[e]) for e in engs}
        nwaits = {e: 0 for e in engs}

        def run(e, engine):
            waited = {}

            def wait(sem, val):
                key = id(sem)
                if waited.get(key, 0) >= val:
                    return
                waited[key] = val
                engine.wait_ge(sem, val)
                nwaits[e] += 1

            for op in per_eng[e]:
                need = {}
                for d in op["deps"]:
                    s, v = ops[d]["signal"]
                    k = id(s)
                    if k not in need or need[k][1] < v:
                        need[k] = (s, v)
                if op["dma"] and op["prev_slot"] is not None:
                    s, v = op["prev_slot"]
                    k = id(s)
                    if k not in need or need[k][1] < v:
                        need[k] = (s, v)
                for s, v in need.values():
                    wait(s, v)
                if op["emit"] is None:
                    if op["signal"] is not None:
                        engine.nop().then_inc(op["signal"][0], 1)
                    continue
                ins = op["emit"](engine)
                if op["signal"] is not None:
                    s, v = op["signal"]
                    ins.then_inc(s, 16 if op["dma"] else 1)
            if e in dsem:
                for k, s in enumerate(dsem[e]):
                    if dval[e][k]:
                        wait(s, dval[e][k])

        with nc.Block() as block:
            @block.tensor
            def _(eng):
                run(PE, eng)

            @block.scalar
            def _(eng):
                run(ACT, eng)

            @block.vector
            def _(eng):
                run(DVE, eng)

            @block.gpsimd
            def _(eng):
                run(POOL, eng)

            @block.sync
            def _(eng):
                run(SP, eng)
        self.nwaits = nwaits


def build_nc(debug=False):
    nc = bass.Bass("TRN2", target_bir_lowering=False)
    scr_kind = "ExternalOutput" if debug else "Internal"
    dt_in = lambda n, s: nc.dram_tensor(n, s, F32, kind="ExternalInput")
    x_d = dt_in("x", [SEQ, D_MODEL]).ap()
    g_mix_d = dt_in("g_mix", [1, D_MODEL])
    w_in_d = dt_in("w_in", [1, D_MODEL, 3840]).ap()[0]
    w_dw_d = dt_in("w_dw", [1, CW, 1, CONV_CH]).ap()
    b_dw_d = dt_in("b_dw", [1, CONV_CH]).ap()
    lng_d = dt_in("ln_conv_g", [1, CONV_CH]).ap()
    lnb_d = dt_in("ln_conv_b", [1, CONV_CH]).ap()
    sinks_d = dt_in("sinks", [1, 8]).ap()
    wc_d = dt_in("w_conv_out", [1, CONV_CH, D_MODEL]).ap()[0]
    wa_d = dt_in("w_attn_out", [1, 512, D_MODEL]).ap()[0]
    wo_d = dt_in("w_out", [1, D_MODEL, D_MODEL]).ap()[0]
    g_ffn_d = dt_in("g_ffn", [1, D_MODEL]).ap()
    wgrp_d = dt_in("w_group", [1, D_MODEL, 4]).ap()[0]
    bgrp_d = dt_in("b_group", [1, 4]).ap()
    wexp_d = dt_in("w_expert", [1, D_MODEL, N_EXP]).ap()[0]
    bexp_d = dt_in("b_expert", [1, N_EXP]).ap()
    wgate_d = dt_in("w_gate", [1, N_EXP, D_MODEL, DFF]).ap()[0]
    wup_d = dt_in("w_up", [1, N_EXP, D_MODEL, DFF]).ap()[0]
    wdown_d = dt_in("w_down", [1, N_EXP, DFF, D_MODEL]).ap()[0]
    g_fin_d = dt_in("g_final", [D_MODEL]).ap()
    out_d = nc.dram_tensor("out", [SEQ, D_MODEL], F32, kind="ExternalOutput").ap()
    hbuf_d = nc.dram_tensor("hbuf", [SEQ, D_MODEL], F32, kind=scr_kind).ap()
    xs_d = nc.dram_tensor("xs_scr", [N_EXP * CAP, D_MODEL], BF16, kind=scr_kind).ap()
    ys_d = nc.dram_tensor("ys_scr", [N_EXP * CAP, D_MODEL], BF16, kind=scr_kind).ap()
    wbf_d = nc.dram_tensor("wbf_scr", [N_EXP, 3, 128, 4096], BF16, kind="Internal").ap()

    with ExitStack() as st:
        def sb(name, shape, dtype):
            return st.enter_context(nc.sbuf_tensor(name, shape, dtype))

        P = Prog(nc)
        pbank = [st.enter_context(nc.psum_tensor("pb%d" % k, [128, 512], F32)) for k in range(8)]
        bank_ctr = [0]

        def nb():
            k = bank_ctr[0] % 8
            bank_ctr[0] += 1
            return k, pbank[k], ("pb", k)

        def MM(out, lhsT, rhs, start, stop, r, w):
            P.add(PE, lambda e: e.matmul(out, lhsT=lhsT, rhs=rhs, start=start, stop=stop), r, w)

        def TR32(out, in_, ident, r, w):
            P.add(PE, lambda e: e.transpose(out, in_, ident), r, w)

        def ACTV(out, in_, func, r, w, bias=None, scale=None, accum=None):
            kw = {}
            if bias is not None:
                kw["bias"] = bias
            if scale is not None:
                kw["scale"] = scale
            if accum is not None:
                kw["accum_out"] = accum
            P.add(ACT, lambda e: e.activation(out=out, in_=in_, func=func, **kw), r, w)

        def TT(eng, out, in0, in1, op, r, w):
            P.add(eng, lambda e: e.tensor_tensor(out=out, in0=in0, in1=in1, op=op), r, w)

        def TS(eng, out, in0, s1, s2, op0, op1, r, w, accum=None):
            if op1 is None:
                P.add(eng, lambda e: e.tensor_scalar(out=out, in0=in0, scalar1=s1, scalar2=None, op0=op0), r, w)
            elif accum is None:
                P.add(eng, lambda e: e.tensor_scalar(out=out, in0=in0, scalar1=s1, scalar2=s2, op0=op0, op1=op1), r, w)
            else:
                P.add(eng, lambda e: e.tensor_scalar(out=out, in0=in0, scalar1=s1, scalar2=s2, op0=op0, op1=op1,
                                                     accum_out=accum), r, w)

        def RSTD(dst, src, scale, rs, ws):
            P.add(ACT, lambda e: e.activation(out=dst, in_=src, func=AF.Sqrt, bias=epsc[:, 0:1], scale=scale), rs + ["epsc"], ws)
            P.add(DVE, lambda e: e.reciprocal(out=dst, in_=dst), ws, ws)

        def STT(eng, out, in0, scalar, in1, op0, op1, r, w):
            P.add(eng, lambda e: e.scalar_tensor_tensor(out=out, in0=in0, scalar=scalar, in1=in1, op0=op0, op1=op1), r, w)

        def CP(eng, out, in_, r, w):
            if eng == ACT:
                P.add(ACT, lambda e: e.activation(out=out, in_=in_, func=AF.Copy), r, w)
            else:
                P.add(eng, lambda e: e.tensor_copy(out=out, in_=in_), r, w)

        def RED(eng, out, in_, op, r, w):
            P.add(eng, lambda e: e.tensor_reduce(out=out, in_=in_, axis=AX.X, op=op), r, w)

        def DMA(eng, out, in_, r, w, **kw):
            P.add(eng, lambda e: e.dma_start(out=out, in_=in_, **kw), r, w, dma=True)

        def MEMSET(eng, ap, val, w):
            P.add(eng, lambda e: e.memset(ap, val), (), w)

        def TAP(name, ap, reads):
            if not debug:
                return
            shape = list(ap.shape)
            d = nc.dram_tensor("dbg_" + name, shape, F32, kind="ExternalOutput").ap()
            DMA(POOL, d, ap, reads, [("dbg", name)])

        identb = sb("identb", [128, 128], BF16)
        identf = sb("identf", [128, 128], F32)
        onesf = sb("onesf", [128, 128], F32)
        onesb = sb("onesb", [128, 128], BF16)
        ustrict = sb("ustrict", [128, 128], BF16)
        gffn_bc = sb("gffn_bc", [128, D_MODEL], F32)
        gmixT = sb("gmixT", [128, 8], F32)
        bdw = sb("bdw", [128, 4], F32)
        lng = sb("lng", [128, 4], F32)
        lnb = sb("lnb", [128, 4], F32)
        wdw_raw = sb("wdw_raw", [CW, CONV_CH], F32)
        wdwT = sb("wdwT", [128, 4, CW], F32)
        esink = sb("esink", [128, 8], F32)
        relt = sb("relt", [128, 2, 128], F32)
        amask = sb("amask", [128, 2, 128], F32)
        EM = sb("EM", [128, 4, 4, 128], BF16)
        wr = sb("wr", [128, 8, 36], F32)
        rbias = sb("rbias", [128, 36], F32)
        ebase = sb("ebase", [128, N_EXP], F32)
        cum = sb("cum", [128, N_EXP], F32)
        cumb2 = sb("cumb2", [128, 2, N_EXP], BF16)
        destall = sb("destall", [128, NTILE, 2], I32)
        wall = sb("wall", [128, NTILE, 2], F32)
        ss = sb("ss", [128, 8], F32)
        rstd = sb("rstd", [128, 8], F32)
        epsc = sb("epsc", [128, 1], F32)
        ztile = sb("ztile", [128, D_MODEL], BF16)
        ht2 = sb("ht2", [128, D_MODEL], F32)
        mtmp2 = sb("mtmp2", [128, 4, NT], F32)
        t1buf = sb("t1buf", [128, 2, NT], F32)

        ARENA_W = 44900
        arena = sb("arena", [128, ARENA_W], F32)
        off = [0]

        def carve(words, dtype, pattern=None, **kw):
            a = arena[:, off[0]:off[0] + words]
            off[0] += words
            assert off[0] <= ARENA_W, off[0]
            if dtype != F32:
                a = a.bitcast(dtype)
            if pattern:
                a = a.rearrange(pattern, **kw)
            return a

        wb_in = carve(15360, BF16, "p (k n) -> p k n", k=8)
        wcb = carve(2048, BF16, "p (k n) -> p k n", k=4)
        wab = carve(2048, BF16, "p (k n) -> p k n", k=4)
        wob = carve(4096, BF16, "p (k n) -> p k n", k=8)
        D64 = carve(3968, BF16, "p (c j m) -> p c j m", c=4, j=CW)
        xt = carve(4096, F32, "p (s n) -> p s n", s=4)
        xsb = carve(1024, BF16, "p (s n) -> p s n", s=2)
        xnT = carve(1024, BF16, "p (k n) -> p k n", k=8)
        vTs = carve(576, BF16, "p (c n) -> p c n", c=4)
        sig = carve(256, F32)
        qTs = carve(512, BF16, "p (c n) -> p c n", c=4)
        kTr = carve(192, BF16)
        vtok_raw = carve(200, BF16)
        vtok = vtok_raw[:, 0:390].rearrange("p (s k d) -> p s k d", s=3, k=2)
        RA = carve(1024, F32)
        RB = carve(1024, F32)
        emf = arena[:, off[0] - 2048:off[0]].rearrange("p (h k q) -> p h k q", h=8, k=2)
        RC = carve(1024, F32)
        RD = carve(1024, F32)
        pexp = carve(512, BF16, "p (b n) -> p b n", b=2)
        attn_tok = carve(256, BF16)
        mtmp = carve(1024, F32, "p (a n) -> p a n", a=4)
        mT = carve(1024, BF16, "p (k n) -> p k n", k=8)
        xn2b2 = [carve(512, BF16) for _ in range(2)]
        attnT = carve(512, BF16, "p (c n) -> p c n", c=4)
        rsm = carve(1024, F32)
        A_END = off[0]

        y32 = RA.rearrange("p (c n) -> p c n", c=4)
        xn2_ = [RA, RB]
        ybf = RB[:, 0:512].bitcast(BF16).rearrange("p (c n) -> p c n", c=4)
        ysq = RB[:, 512:1024].bitcast(BF16).rearrange("p (c n) -> p c n", c=4)
        cT = RB[:, 512:1024].bitcast(BF16).rearrange("p (c n) -> p c n", c=4)
        ln_mean = RC[:, 0:256]
        ln_rstd = RC[:, 256:512]
        ln_tmp = RC[:, 512:768]
        ln_t1 = RC[:, 768:1024]
        ht = RC
        PT = RD.bitcast(BF16).rearrange("p (g c n) -> p g c n", g=4, c=4)
        xn2T_ = [RD.rearrange("p (k n) -> p k n", k=8), mtmp.rearrange("p a n -> p (a n)").rearrange("p (k n) -> p k n", k=8)]

        MEMSET(DVE, onesf[:], 1.0, ["onesf"])
        MEMSET(DVE, onesb[:], 1.0, ["onesb"])
        P.add(POOL, lambda e: e.affine_select(out=identb[:], in_=onesf[:], pattern=[[-1, 128]], compare_op=ALU.is_equal,
                                              fill=0.0, base=0, channel_multiplier=1), ["onesf"], ["identb"])
        P.add(POOL, lambda e: e.affine_select(out=identf[:], in_=onesf[:], pattern=[[-1, 128]], compare_op=ALU.is_equal,
                                              fill=0.0, base=0, channel_multiplier=1), ["onesf"], ["identf"])
        P.add(POOL, lambda e: e.affine_select(out=ustrict[:], in_=onesf[:], pattern=[[1, 128]], compare_op=ALU.is_gt,
                                              fill=0.0, base=0, channel_multiplier=-1), ["onesf"], ["ustrict"])
        P.add(POOL, lambda e: e.iota(ebase[:], [[CAP, N_EXP]], base=0, channel_multiplier=0,
                                     allow_small_or_imprecise_dtypes=True), (), ["ebase"])
        for kb in range(2):
            P.add(POOL, lambda e, kb=kb: e.iota(relt[:, kb, :], [[1, 128]], base=128 * (1 - kb), channel_multiplier=-1,
                                                allow_small_or_imprecise_dtypes=True), (), ["relt"])
        P.add(POOL, lambda e: e.affine_select(out=amask[:, 0, :], in_=onesf[:], pattern=[[-1, 128]], compare_op=ALU.is_gt,
                                              fill=0.0, base=0, channel_multiplier=1), ["onesf"], ["amask"])
        P.add(POOL, lambda e: e.affine_select(out=amask[:, 1, :], in_=onesf[:], pattern=[[1, 128]], compare_op=ALU.is_ge,
                                              fill=0.0, base=0, channel_multiplier=-1), ["onesf"], ["amask"])
        win_v = w_in_d.rearrange("(k p) n -> p k n", p=128)

        def win_load(d0, s0, n):
            for kh in range(2):
                DMA(POOL, wb_in[:, 4 * kh:4 * kh + 4, d0:d0 + n], win_v[:, 4 * kh:4 * kh + 4, s0:s0 + n], [],
                    [("wb_in", d0, kh)])

        def win_res(col):
            if 1024 <= col < 1536:
                c_ = (col - 1024) // 128
                return [("wb_q", c_, 0), ("wb_q", c_, 1)]
            for d0, n in ((0, 512), (512, 512), (1536, 128), (3712, 128), (1664, 1024), (2688, 1024)):
                if d0 <= col < d0 + n:
                    return [("wb_in", d0, 0), ("wb_in", d0, 1)]
            raise ValueError(col)

        DMA(SP, gmixT[:], g_mix_d.ap()[0].rearrange("(c p) -> p c", p=128), [], ["gmixT"], allow_slow_non_contiguous=True)
        win_load(0, 0, 512)
        win_load(512, 512, 512)
        for c in range(4):
            for two in range(2):
                src0 = 1024 + 64 * (4 * two + c)
                DMA(POOL, wb_in[:, :, 1024 + 128 * c + 64 * two:1024 + 128 * c + 64 * two + 64],
                    win_v[:, :, src0:src0 + 64], [], [("wb_q", c, two)])
        win_load(1536, 1536, 128)
        win_load(3712, 1664, 128)
        win_load(1664, 1792, 1024)
        win_load(2688, 2816, 1024)
        DMA(POOL, wcb, wc_d.rearrange("(k p) n -> p k n", p=128), [], ["wcb"])
        DMA(POOL, wab, wa_d.rearrange("(k p) n -> p k n", p=128), [], ["wab"])
        for kh in range(2):
            DMA(POOL, wob[:, 4 * kh:4 * kh + 4, :], wo_d.rearrange("(k p) n -> p k n", p=128)[:, 4 * kh:4 * kh + 4, :],
                [], [("wob", kh)])
        MEMSET(DVE, cum[:], 0.0, ["cum"])
        MEMSET(DVE, epsc[:], EPS, ["epsc"])
        MEMSET(DVE, vtok_raw, 1.0, ["vtok0", "vtok1", "vtok2"])
        MEMSET(DVE, vTs, 0.0, ["vTs"])
        DMA(SP, gffn_bc[:], g_ffn_d[0].partition_broadcast(128), [], ["gffn_bc"])
        DMA(SP, bdw[:], b_dw_d[0].rearrange("(c p) -> p c", p=128), [], ["bdw"], allow_slow_non_contiguous=True)
        DMA(SP, lng[:], lng_d[0].rearrange("(c p) -> p c", p=128), [], ["lng"], allow_slow_non_contiguous=True)
        DMA(SP, lnb[:], lnb_d[0].rearrange("(c p) -> p c", p=128), [], ["lnb"], allow_slow_non_contiguous=True)
        DMA(SP, wdw_raw[:], w_dw_d[0].rearrange("j o c -> j (o c)"), [], ["wdw_raw"])
        DMA(SP, esink[:], sinks_d[0].partition_broadcast(128), [], ["esink"])
        DMA(SP, rbias[:, 0:4], bgrp_d[0].partition_broadcast(128), [], ["rbias"])
        DMA(SP, rbias[:, 4:36], bexp_d[0].partition_broadcast(128), [], ["rbias2"])
        DMA(SP, wr[:, :, 0:4], wgrp_d.rearrange("(k p) n -> p k n", p=128), [], ["wr"])
        DMA(SP, wr[:, :, 4:36], wexp_d.rearrange("(k p) n -> p k n", p=128), [], ["wr2"])
        ACTV(esink[:], esink[:], AF.Exp, ["esink"], ["esink"])
        k_, bk, br = nb()
        for c in range(4):
            TR32(bk[:, c * 32:c * 32 + CW], wdw_raw[:, c * 128:(c + 1) * 128], identf[0:CW, 0:CW],
                 ["wdw_raw", "identf"], [br])
        CP(DVE, wdwT[:], bk[:, 0:128].rearrange("p (c j) -> p c j", c=4)[:, :, 0:CW], [br], ["wdwT"])
        for c in range(4):
            for hf in range(2):
                sl = slice(64 * hf, 64 * hf + 64)
                TT(DVE, D64[sl, c, :, :], identb[sl, 64 * hf:64 * hf + 64].unsqueeze(1).broadcast_to([64, CW, 64]),
                   wdwT[sl, c, :].unsqueeze(2).broadcast_to([64, CW, 64]), ALU.mult, ["identb", "wdwT"],
                   [("D64", c, 0), ("D64", c, 1)])
        TS(DVE, relt[:], relt[:], 0.0, 128.0, ALU.max, ALU.min, ["relt"], ["relt"])
        def emf_res(h):
            if h < 4:
                return [("RA", h)]
            hh = h - 4
            return [("RB%d" % (hh // 2), 2 * (hh % 2)), ("RB%d" % (hh // 2), 2 * (hh % 2) + 1)]

        for h in range(8):
            ACTV(emf[:, h, :, :], relt[:], AF.Exp, ["relt"], emf_res(h), scale=-(2.0 ** (-(h + 1))))
        for kv in range(2):
            for kb in range(2):
                TT(DVE, EM[:, 2 * kv + kb, :, :], emf[:, 4 * kv:4 * kv + 4, kb, :],
                   amask[:, kb, :].unsqueeze(1).broadcast_to([128, 4, 128]), ALU.mult,
                   sum([emf_res(h_) for h_ in range(4 * kv, 4 * kv + 4)], []) + ["amask"], ["EM"])

        def prep(s):
            for i in range(TPS):
                g = s * TPS + i
                slot = g % 4
                DMA(SP, xt[:, slot, :], x_d[g * 128:(g + 1) * 128, :], [], [("xt", slot)])
                ACTV(xsb[:, i, :], xt[:, slot, :], AF.Square, [("xt", slot)], [("xsb", i), ("ss", i)],
                     accum=ss[:, i:i + 1])
                RSTD(rstd[:, i:i + 1], ss[:, i:i + 1], 1.0 / D_MODEL, [("ss", i)], [("rstd", i)])
                TS(DVE, xsb[:, i, :], xt[:, slot, :], rstd[:, i:i + 1], None, ALU.mult, None,
                   [("xt", slot), ("rstd", i)], [("xsb", i)])

        evac_flip = [0]

        def transposes(s):
            for i in range(TPS):
                for hf in range(2):
                    k_, bk, br = nb()
                    for q in range(4):
                        kk = 4 * hf + q
                        MM(bk[:, q * 128:(q + 1) * 128], xsb[:, i, kk * 128:(kk + 1) * 128], identb[:], True, True,
                           [("xsb", i), "identb"], [br])
                    for q in range(4):
                        kk = 4 * hf + q
                        evac_flip[0] ^= 1
                        if evac_flip[0]:
                            ACTV(xnT[:, kk, i * 128:(i + 1) * 128], bk[:, q * 128:(q + 1) * 128], AF.Copy,
                                 [br, "gmixT"], ["xnT"], scale=gmixT[:, kk:kk + 1])
                        else:
                            TS(DVE, xnT[:, kk, i * 128:(i + 1) * 128], bk[:, q * 128:(q + 1) * 128],
                               gmixT[:, kk:kk + 1], None, ALU.mult, None, [br, "gmixT"], ["xnT"])

        def proj_chunk(j):
            k_, bk, br = nb()
            wres = win_res(j * 128)
            for k in range(8):
                MM(bk[:, 0:NT], wb_in[:, k, j * 128:(j + 1) * 128], xnT[:, k, :], k == 0, k == 7, wres + ["xnT"], [br])
            return bk, br

        def inproj_glu(s):
            if s > 0:
                CP(POOL, vTs[:, :, 0:30], vTs[:, :, NT:NT + 30], ["vTs"], ["vTs"])
                CP(POOL, kTr[:, 0:128], kTr[:, NT:NT + 128], ["kTr"], ["kTr"])
                CP(POOL, vtok[:, 0, :, :], vtok[:, TPS, :, :], ["vtok%d" % TPS], ["vtok0"])
            for j in range(4):
                ba, bar = proj_chunk(j)
                bg, bgr = proj_chunk(4 + j)
                ACTV(sig, bg[:, 0:NT], AF.Sigmoid, [bgr], ["sig"])
                TT(DVE, vTs[:, j, 30:30 + NT], ba[:, 0:NT], sig, ALU.mult, [bar, "sig"], ["vTs"])

        def inproj_qkv(s):
            for c in range(4):
                bq, bqr = proj_chunk(8 + c)
                CP(ACT, qTs[:, c, :], bq[:, 0:NT], [bqr], ["qTs"])
            bk_, bkr = proj_chunk(12)
            CP(DVE, kTr[:, 128:128 + NT], bk_[:, 0:NT], [bkr], ["kTr"])
            wres = win_res(3712)
            for i in range(TPS):
                k_, bv, bvr = nb()
                for k in range(8):
                    MM(bv[:, 0:128], xnT[:, k, i * 128:(i + 1) * 128], wb_in[:, k, 3712:3840], k == 0, k == 7,
                       ["xnT"] + wres, [bvr])
                CP(ACT, vtok[:, 1 + i, :, 0:64], bv[:, 0:128].rearrange("p (k d) -> p k d", k=2), [bvr],
                   ["vtok%d" % (1 + i)])

        def conv_chunk(s, c):
            banks = []
            for hf in range(2):
                k_, bk, br = nb()
                banks.append((bk, br))
            for j in range(CW):
                for hf in range(2):
                    bk, br = banks[hf]
                    MM(bk[64 * hf:64 * hf + 64, 0:NT], D64[64 * hf:64 * hf + 64, c, j, :],
                       vTs[64 * hf:64 * hf + 64, c, j:j + NT], j == 0, j == CW - 1, [("D64", c, 0), ("D64", c, 1), "vTs"], [br])
            for hf in range(2):
                bk, br = banks[hf]
                sl = slice(64 * hf, 64 * hf + 64)
                ACTV(y32[sl, c, :], bk[sl, 0:NT], AF.Identity, [br, "bdw"], [("RA", c)], bias=bdw[sl, c:c + 1])
                ACTV(ysq[sl, c, :], bk[sl, 0:NT], AF.Square, [br, "bdw"], [("RB1", c)], bias=bdw[sl, c:c + 1])
            CP(DVE, ybf[:, c, :], y32[:, c, :], [("RA", c)], [("RB0", c)])

        def ln(s):
            k1, b1, b1r = nb()
            k2, b2, b2r = nb()
            for c in range(4):
                MM(b1[:, 0:NT], onesb[:], ybf[:, c, :], c == 0, c == 3, ["onesb", ("RB0", c)], [b1r])
            for c in range(4):
                MM(b2[:, 0:NT], onesb[:], ysq[:, c, :], c == 0, c == 3, ["onesb", ("RB1", c)], [b2r])
            TS(DVE, ln_mean, b1[:, 0:NT], 1.0 / CONV_CH, None, ALU.mult, None, [b1r], ["RC0"])
            TT(DVE, ln_tmp, ln_mean, ln_mean, ALU.mult, ["RC0"], ["RC2"])
            STT(DVE, ln_rstd, b2[:, 0:NT], 1.0 / CONV_CH, ln_tmp, ALU.mult, ALU.subtract, [b2r, "RC2"], ["RC1"])
            RSTD(ln_rstd, ln_rstd, 1.0, ["RC1"], ["RC1"])
            for c in range(4):
                TT(DVE, ln_t1, y32[:, c, :], ln_mean, ALU.subtract, [("RA", c), "RC0"], ["RC3"])
                TT(DVE, ln_t1, ln_t1, ln_rstd, ALU.mult, ["RC3", "RC1"], ["RC3"])
                ACTV(cT[:, c, :], ln_t1, AF.Silu, ["RC3", "lng", "lnb", ("RB1", c)], [("RB1", c)],
                     scale=lng[:, c:c + 1], bias=lnb[:, c:c + 1])

        def attn_qk(s, i):
            n = s * TPS + i
            qb = i * 128
            kbs = [1] if n == 0 else [0, 1]
            for kv in range(2):
                sl = slice(64 * kv, 64 * kv + 64)
                for kb in kbs:
                    k_, bk, br = nb()
                    kc = qb + 128 * kb
                    MM(bk[:, :].rearrange("p (c q) -> p c q", c=4), kTr[sl, kc:kc + 128], qTs[sl, :, qb:qb + 128],
                       True, True, ["kTr", "qTs"], [br])
                    pe_ = ("pexp", kb)
                    ACTV(pexp[:, kb, :], bk[:, :], AF.Exp, [br], [pe_], scale=0.125)
                    TT(DVE, PT[:, 2 * kv + kb, :, :], pexp[:, kb, :].rearrange("p (c q) -> p c q", c=4),
                       EM[:, 2 * kv + kb, :, :], ALU.mult, [pe_, "EM"], [("RD", 2 * kv + kb)])

        def attn_pv(s, i):
            n = s * TPS + i
            kbs = [1] if n == 0 else [0, 1]
            for kv in range(2):
                k_, bo, bor = nb()
                ov = bo[:, 0:260].rearrange("p (c d) -> p c d", c=4)
                for c in range(4):
                    for kb in kbs:
                        MM(ov[:, c, :], PT[:, 2 * kv + kb, c, :], vtok[:, i + kb, kv, :], kb == kbs[0], kb == 1,
                           [("RD", 2 * kv + kb), "vtok%d" % (i + kb)], [bor])
                den = rsm[:, 4 * kv:4 * kv + 4]
                dr = ("den", kv)
                TT(DVE, den, ov[:, :, 64], esink[:, 4 * kv:4 * kv + 4], ALU.add, [bor, "esink"], [dr])
                P.add(DVE, lambda e, den=den: e.reciprocal(out=den, in_=den), [dr], [dr])
                TT(DVE, attn_tok[:, 256 * kv:256 * kv + 256].rearrange("p (c d) -> p c d", c=4), ov[:, :, 0:64],
                   den.unsqueeze(2).broadcast_to([128, 4, 64]), ALU.mult, [bor, dr], [("attn_tok", kv)])

        def attn_T(s, i):
            qb = i * 128
            k_, bt, btr = nb()
            for c in range(4):
                MM(bt[:, c * 128:(c + 1) * 128], attn_tok[:, c * 128:(c + 1) * 128], identb[:], True, True,
                   [("attn_tok", c // 2), "identb"], [btr])
            CP(ACT, attnT[:, :, qb:qb + 128], bt[:, :].rearrange("p (c q) -> p c q", c=4), [btr], ["attnT"])

        def merge(s):
            sg_c = [mtmp[:, 0, :], mtmp[:, 2, :]]
            sg_a = [mtmp[:, 1, :], mtmp[:, 3, :]]
            sg_cr = ["sgc", "t1"]
            sg_ar = ["sga", "t2"]
            t2s = [mtmp2[:, jj, :] for jj in range(4)]
            t2r = [("t2s", jj) for jj in range(4)]
            for i in range(2):
                v_ = xn2b2[i].bitcast(F32).rearrange("p (a n) -> p a n", a=2)
                t2s += [v_[:, 0, :], v_[:, 1, :]]
                t2r += [("xn2b", i), ("xn2b", i)]
            for j in range(8):
                k_, bb, bbr = nb()
                for k in range(4):
                    MM(bb[:, 0:NT], wab[:, k, j * 128:(j + 1) * 128], attnT[:, k, :], k == 0, k == 3, ["wab", "attnT"], [bbr])
                bd, bdr = proj_chunk(21 + j)
                ACTV(sg_a[j % 2], bd[:, 0:NT], AF.Sigmoid, [bdr], [sg_ar[j % 2]])
                TT(DVE, t2s[j], bb[:, 0:NT], sg_a[j % 2], ALU.mult, [bbr, sg_ar[j % 2]], [t2r[j]])
            for j in range(8):
                k_, ba, bar = nb()
                for k in range(4):
                    MM(ba[:, 0:NT], wcb[:, k, j * 128:(j + 1) * 128], cT[:, k, :], k == 0, k == 3,
                       ["wcb", ("RB1", k)], [bar])
                bc, bcr = proj_chunk(13 + j)
                ACTV(sg_c[j % 2], bc[:, 0:NT], AF.Sigmoid, [bcr], [sg_cr[j % 2]])
                TT(DVE, t1buf[:, j % 2, :], ba[:, 0:NT], sg_c[j % 2], ALU.mult, [bar, sg_cr[j % 2]], [("t1b", j % 2)])
                TT(POOL, mT[:, j, :], t1buf[:, j % 2, :], t2s[j], ALU.add, [("t1b", j % 2), t2r[j]], [("mT", j)])

        RAr = [("RA", c) for c in range(4)]
        RBr = [("RB0", c) for c in range(4)] + [("RB1", c) for c in range(4)]
        RCr = ["RC0", "RC1", "RC2", "RC3"]
        RDr = [("RD", q) for q in range(4)]
        MTr = ["sgc", "sga", "t1", "t2"]
        xn2_res = [RAr, RBr]
        hts = [ht, ht2[:]]
        htr = [RCr, ["ht2"]]
        xn2T_res = [RDr, MTr]

        def tail_wout(s):
            mres = [("mT", j) for j in range(8)]
            for i in range(TPS):
                g = s * TPS + i
                slot = g % 4
                for hf in range(2):
                    k_, bk, br = nb()
                    for k in range(8):
                        MM(bk[:, :], mT[:, k, i * 128:(i + 1) * 128], wob[:, k, hf * 512:(hf + 1) * 512], k == 0, k == 7,
                           mres + [("wob", 0), ("wob", 1)], [br])
                    TT(DVE, hts[i][:, hf * 512:(hf + 1) * 512], bk[:, :], xt[:, slot, hf * 512:(hf + 1) * 512], ALU.add,
                       [br, ("xt", slot)], htr[i])
                DMA(SP, hbuf_d[g * 128:(g + 1) * 128, :], hts[i], htr[i], [("hbuf", g)])

        def tail_wout_b(s):
            for i in range(TPS):
                xb = xn2b2[i]
                ACTV(xb, hts[i], AF.Square, htr[i], [("xn2b", i), ("ss2", i)], accum=ss[:, 2 + i:3 + i])
                RSTD(rstd[:, 2 + i:3 + i], ss[:, 2 + i:3 + i], 1.0 / D_MODEL, [("ss2", i)], [("rstd2", i)])
                STT(DVE, xn2_[i], hts[i], rstd[:, 2 + i:3 + i], gffn_bc[:], ALU.mult, ALU.mult,
                    htr[i] + [("rstd2", i), "gffn_bc"], xn2_res[i])
                CP(ACT, xb, xn2_[i], xn2_res[i], [("xn2b", i)])

        def tail_rtr_T(s):
            for i in range(TPS):
                for hf in range(2):
                    k_, bk, br = nb()
                    for q in range(4):
                        kk = 4 * hf + q
                        TR32(bk[:, q * 128:(q + 1) * 128], xn2_[i][:, kk * 128:(kk + 1) * 128], identf[:],
                             xn2_res[i] + ["identf"], [br])
                    if hf == 0:
                        CP(ACT, xn2T_[i][:, 0:4, :], bk[:, :].rearrange("p (k n) -> p k n", k=4), [br], xn2T_res[i][0:2])
                    else:
                        CP(DVE, xn2T_[i][:, 4:8, :], bk[:, :].rearrange("p (k n) -> p k n", k=4), [br], xn2T_res[i][2:4])

        def rfields(i):
            o = 8 + 500 * i
            f = {}
            names = [("lg", 36), ("gmax", 1), ("ngmax", 1), ("gexp", 4), ("gsum", 1), ("pgrp", 1), ("gone", 4), ("pen", 4),
                     ("em", 32), ("m1", 1), ("one1", 32), ("em2", 32), ("m2", 1), ("one2", 32), ("dlt", 1), ("w1", 1),
                     ("ind", 32), ("indb", 16), ("pos", 32), ("tmp", 32), ("d12", 2)]
            for nm, w_ in names:
                f[nm] = rsm[:, o:o + w_]
                o += w_
            assert o <= 8 + 500 * (i + 1)
            return f

        tail_state = {}

        def tail_logits(s):
            for i in range(TPS):
                k_, bl, blr = nb()
                for k in range(8):
                    MM(bl[:, 0:36], xn2T_[i][:, k, :], wr[:, k, :], k == 0, k == 7, xn2T_res[i] + ["wr", "wr2"], [blr])
                TT(DVE, rfields(i)["lg"], bl[:, 0:36], rbias[:], ALU.add, [blr, "rbias", "rbias2"], [("lg", i)])

        def tail_route(s, i):
            if True:
                g = s * TPS + i
                F = rfields(i)
                R = lambda nm: (nm, i)
                lg, gmax, ngmax, gexp, gsum, pgrp = F["lg"], F["gmax"], F["ngmax"], F["gexp"], F["gsum"], F["pgrp"]
                gone, pen, em, m1, one1, em2, m2, one2 = F["gone"], F["pen"], F["em"], F["m1"], F["one1"], F["em2"], F["m2"], F["one2"]
                dlt, w1, ind = F["dlt"], F["w1"], F["ind"]
                indb = F["indb"].bitcast(BF16)
                RED(DVE, gmax, lg[:, 0:4], ALU.max, [R("lg")], [R("gmax")])
                TS(DVE, ngmax, gmax, -1.0, None, ALU.mult, None, [R("gmax")], [R("ngmax")])
                ACTV(gexp, lg[:, 0:4], AF.Exp, [R("lg"), R("ngmax")], [R("gexp"), R("gsum")], bias=ngmax, accum=gsum)
                P.add(DVE, lambda e, pgrp=pgrp, gsum=gsum: e.reciprocal(out=pgrp, in_=gsum), [R("gsum")], [R("pgrp")])
                TS(DVE, gone, lg[:, 0:4], gmax, None, ALU.is_equal, None, [R("lg"), R("gmax")], [R("gone")])
                TS(DVE, pen, gone, -1.0, BIG, ALU.add, ALU.mult, [R("gone")], [R("pen")])
                TT(DVE, em.rearrange("p (g j) -> p g j", g=4), lg[:, 4:36].rearrange("p (g j) -> p g j", g=4),
                   pen.unsqueeze(2).broadcast_to([128, 4, 8]), ALU.add, [R("lg"), R("pen")], [R("em")])
                RED(DVE, m1, em, ALU.max, [R("em")], [R("m1")])
                TS(DVE, one1, em, m1, None, ALU.is_equal, None, [R("em"), R("m1")], [R("one1")])
                STT(DVE, em2, one1, -BIG, em, ALU.mult, ALU.add, [R("one1"), R("em")], [R("em2")])
                RED(DVE, m2, em2, ALU.max, [R("em2")], [R("m2")])
                TS(DVE, one2, em2, m2, None, ALU.is_equal, None, [R("em2"), R("m2")], [R("one2")])
                TT(DVE, dlt, m2, m1, ALU.subtract, [R("m1"), R("m2")], [R("dlt")])
                ACTV(dlt, dlt, AF.Exp, [R("dlt")], [R("dlt")])
                TS(DVE, dlt, dlt, 1.0, None, ALU.add, None, [R("dlt")], [R("dlt")])
                P.add(DVE, lambda e, w1=w1, dlt=dlt: e.reciprocal(out=w1, in_=dlt), [R("dlt")], [R("w1")])
                TT(DVE, wall[:, g, 0:1], w1, pgrp, ALU.mult, [R("w1"), R("pgrp")], [("wall", g)])
                TT(DVE, wall[:, g, 1:2], pgrp, wall[:, g, 0:1], ALU.subtract, [R("pgrp"), ("wall", g)], [("wall", g)])
                TT(DVE, ind, one1, one2, ALU.add, [R("one1"), R("one2")], [R("ind")])
                CP(DVE, indb, ind, [R("ind")], [R("indb")])

        def tail_pos(s):
            for i in range(TPS):
                g = s * TPS + i
                F = rfields(i)
                R = lambda nm: (nm, i)
                ind, pos, tmp, one1, one2, d12 = F["ind"], F["pos"], F["tmp"], F["one1"], F["one2"], F["d12"]
                indb = F["indb"].bitcast(BF16)
                cb_ = cumb2[:, i, :]
                CP(DVE, cb_, cum[:], ["cum"], [("cumb", i)])
                k_, bp, bpr = nb()
                MM(bp[:, 0:32], ustrict[:], indb, True, False, ["ustrict", R("indb")], [bpr])
                MM(bp[:, 0:32], onesb[:], cb_, False, True, ["onesb", ("cumb", i)], [bpr])
                TT(DVE, cum[:], cum[:], ind, ALU.add, ["cum", R("ind")], ["cum"])
                TS(DVE, pos, bp[:, 0:32], float(CAP - 1), None, ALU.min, None, [bpr], [R("pos")])
                TT(DVE, pos, pos, ebase[:], ALU.add, [R("pos"), "ebase"], [R("pos")])
                TT(DVE, tmp, pos, one1, ALU.mult, [R("pos"), R("one1")], [R("tmp")])
                RED(DVE, d12[:, 0:1], tmp, ALU.add, [R("tmp")], [R("d1f")])
                TT(DVE, tmp, pos, one2, ALU.mult, [R("pos"), R("one2")], [R("tmp")])
                RED(DVE, d12[:, 1:2], tmp, ALU.add, [R("tmp")], [R("d2f")])
                CP(DVE, destall[:, g, :], d12, [R("d1f"), R("d2f")], [("dest", g)])
                zres = [("xs_z", r0) for r0 in range(0, N_EXP * CAP, 1024)]
                for k in range(2):
                    P.add(POOL, lambda e, g=g, k=k, i=i: e.indirect_dma_start(
                        out=xs_d, out_offset=bass.IndirectOffsetOnAxis(ap=destall[:, g, k:k + 1], axis=0),
                        in_=xn2b2[i], in_offset=None), [("xn2b", i), ("dest", g)] + zres, [("xs_scr", g, k)], dma=True)

        def precast(e):
            srcs = (wgate_d[e].rearrange("(k p) n -> p k n", p=128), wup_d[e].rearrange("(k p) n -> p k n", p=128),
                    wdown_d[e].rearrange("(k p) n -> p k n", p=128))
            for m_, src in enumerate(srcs):
                kk = src.shape[1]
                dst = wbf_d[e, m_].rearrange("p (k n) -> p k n", k=kk)
                DMA(POOL, dst, src, [], [("wbf", e, m_)])

        def zero_fill():
            MEMSET(DVE, ztile[:], 0.0, ["ztile"])
            for r0 in range(0, N_EXP * CAP, 1024):
                DMA(POOL, xs_d[r0:r0 + 1024, :].rearrange("(b p) n -> p b n", p=128),
                    ztile[:].unsqueeze(1).broadcast_to([128, 8, D_MODEL]), ["ztile"], [("xs_z", r0)])

        prep(0)
        for s in range(NST):
            transposes(s)
            if s > 0:
                tail_wout(s - 1)
            inproj_glu(s)
            if s > 0:
                tail_wout_b(s - 1)
            inproj_qkv(s)
            if s == 0:
                zero_fill()
            if s + 1 < NST:
                prep(s + 1)
            if s > 0:
                tail_rtr_T(s - 1)
            conv_chunk(s, 0)
            if s > 0:
                tail_logits(s - 1)
                tail_route(s - 1, 0)
            attn_qk(s, 0)
            conv_chunk(s, 1)
            if s > 0:
                tail_route(s - 1, 1)
            attn_pv(s, 0)
            attn_qk(s, 1)
            conv_chunk(s, 2)
            attn_T(s, 0)
            attn_pv(s, 1)
            if s > 0:
                tail_pos(s - 1)
            conv_chunk(s, 3)
            attn_T(s, 1)
            ln(s)
            merge(s)
            for e_ in range(s * (N_EXP // NST), (s + 1) * (N_EXP // NST)):
                precast(e_)
        tail_wout(NST - 1)
        tail_wout_b(NST - 1)
        tail_rtr_T(NST - 1)
        tail_logits(NST - 1)
        tail_route(NST - 1, 0)
        tail_route(NST - 1, 1)
        tail_pos(NST - 1)

        P.barrier()
        off[0] = 0
        NWB = 4
        wexp = []
        for b in range(NWB):
            wg = carve(2048, BF16, "p (k n) -> p k n", k=8)
            wu = carve(2048, BF16, "p (k n) -> p k n", k=8)
            wd = carve(2048, BF16, "p (k n) -> p k n", k=4)
            wexp.append((wg, wu, wd))
        NXS = 3
        xsr = [carve(2048, BF16, "p (b n) -> p b n", b=4) for _ in range(NXS)]
        xsT = [carve(4 * CAP, BF16, "p (k n) -> p k n", k=8) for _ in range(2)]
        sgt = [carve(CAP, F32) for _ in range(2)]
        hT = [carve(2 * CAP, BF16, "p (f n) -> p f n", f=4) for _ in range(2)]
        NYB = 4
        ysb = [carve(512, BF16) for _ in range(NYB)]
        B_END = off[0]

        def load_weights(e):
            b = e % NWB
            wg, wu, wd = wexp[b]
            DMA(SP, wg.rearrange("p k n -> p (k n)"), wbf_d[e, 0], [], [("wg", b, 0), ("wg", b, 1)])
            DMA(SP, wu.rearrange("p k n -> p (k n)"), wbf_d[e, 1], [], [("wu", b, 0), ("wu", b, 1)])
            DMA(SP, wd.rearrange("p k n -> p (k n)"), wbf_d[e, 2], [], [("wd", b, 0), ("wd", b, 1)])

        def load_xs(e):
            b = e % NXS
            DMA(SP, xsr[b][:, 0:NFULL, :], xs_d[e * CAP:e * CAP + 128 * NFULL, :].rearrange("(b p) n -> p b n", p=128), [],
                [("xsr", b)])
            if CAP > 128 * NFULL:
                DMA(SP, xsr[b][0:CAP - 128 * NFULL, NFULL, :], xs_d[e * CAP + 128 * NFULL:(e + 1) * CAP, :], [], [("xsrt", b)])

        def exp_transposes(e):
            b = e % NXS
            xT = xsT[e % 2]
            for blk, (s0, sz) in enumerate(BLKS):
                for hf in range(2):
                    k_, bk, br = nb()
                    for q in range(4):
                        kk = 4 * hf + q
                        MM(bk[:, q * sz:(q + 1) * sz], xsr[b][0:sz, blk, kk * 128:(kk + 1) * 128], identb[0:sz, 0:sz], True, True,
                           [("xsr", b), ("xsrt", b), "identb"], [br])
                    dst = xT[:, 4 * hf:4 * hf + 4, s0:s0 + sz]
                    src = bk[:, 0:4 * sz].rearrange("p (k n) -> p k n", k=4)
                    if hf == 0:
                        CP(ACT, dst, src, [br], [("xsT", e % 2)])
                    else:
                        CP(DVE, dst, src, [br], [("xsT", e % 2)])

        def exp_gateup(e):
            b = e % NWB
            wg, wu, wd = wexp[b]
            xT = xsT[e % 2]
            h_ = hT[e % 2]
            for f in range(4):
                k_, bg, bgr = nb()
                k_, bu, bur = nb()
                for k in range(8):
                    MM(bg[:, 0:CAP], wg[:, k, f * 128:(f + 1) * 128], xT[:, k, :], k == 0, k == 7,
                       [("wg", b, 0), ("wg", b, 1), ("xsT", e % 2)], [bgr])
                for k in range(8):
                    MM(bu[:, 0:CAP], wu[:, k, f * 128:(f + 1) * 128], xT[:, k, :], k == 0, k == 7,
                       [("wu", b, 0), ("wu", b, 1), ("xsT", e % 2)], [bur])
                sg_ = sgt[f % 2]
                ACTV(sg_, bg[:, 0:CAP], AF.Silu, [bgr], [("sgt", f % 2)])
                TT(DVE, h_[:, f, :], bu[:, 0:CAP], sg_, ALU.mult, [bur, ("sgt", f % 2)], [("hT", e % 2, f)])

        ycount = [0]

        def exp_down(e):
            b = e % NWB
            wg, wu, wd = wexp[b]
            h_ = hT[e % 2]
            for blk, (s0, sz) in enumerate(BLKS):
                yb = ysb[ycount[0] % NYB]
                yr = ("ysb", ycount[0] % NYB)
                ycount[0] += 1
                for hf in range(2):
                    k_, bk, br = nb()
                    for f in range(4):
                        MM(bk[0:sz, :], h_[:, f, s0:s0 + sz], wd[:, f, hf * 512:(hf + 1) * 512], f == 0, f == 3,
                           [("hT", e % 2, f_) for f_ in range(4)] + [("wd", b, 0), ("wd", b, 1)], [br])
                    if hf == 0:
                        CP(ACT, yb[0:sz, 0:512], bk[0:sz, :], [br], [yr])
                    else:
                        CP(DVE, yb[0:sz, 512:1024], bk[0:sz, :], [br], [yr])
                r0 = e * CAP + s0
                DMA(SP, ys_d[r0:r0 + sz, :], yb[0:sz, :], [yr], [("ys_scr", e, blk)])

        for e in range(3):
            load_weights(e)
            load_xs(e) if e < 3 else None
        exp_transposes(0)
        for e in range(N_EXP):
            if e + 3 < N_EXP:
                load_weights(e + 3)
            exp_gateup(e)
            if e + 1 < N_EXP:
                exp_transposes(e + 1)
            if e + 3 < N_EXP:
                load_xs(e + 3)
            exp_down(e)

        P.barrier()
        off[0] = 0
        gfin_bc = carve(1024, F32)
        NCB = 8
        cb = []
        for b in range(NCB):
            cb.append(dict(h=carve(1024, F32), y1=carve(512, BF16), y2=carve(512, BF16), o=carve(1024, F32)))
        DMA(SP, gfin_bc, g_fin_d.partition_broadcast(128), [], ["gfin_bc"])

        def c_load(g):
            b = g % NCB
            B_ = cb[b]
            DMA(SP, B_["h"], hbuf_d[g * 128:(g + 1) * 128, :], [], [("ch", b)])
            for k, key in ((0, "y1"), (1, "y2")):
                P.add(POOL, lambda e, g=g, k=k, dstt=B_[key]: e.indirect_dma_start(
                    out=dstt, out_offset=None, in_=ys_d,
                    in_offset=bass.IndirectOffsetOnAxis(ap=destall[:, g, k:k + 1], axis=0)),
                    [], [("c" + key, b)], dma=True)

        for g in range(NCB - 2):
            c_load(g)

        def c_stage1(g):
            b = g % NCB
            B_ = cb[b]
            col = 5 + g % 2
            STT(DVE, B_["h"], B_["y1"], wall[:, g, 0:1], B_["h"], ALU.mult, ALU.add, [("cy1", b), ("ch", b)], [("ch", b)])
            STT(DVE, B_["h"], B_["y2"], wall[:, g, 1:2], B_["h"], ALU.mult, ALU.add, [("cy2", b), ("ch", b)], [("ch", b)])
            ACTV(B_["o"], B_["h"], AF.Square, [("ch", b)], [("co", b), ("ss3", g % 2)], accum=ss[:, col:col + 1])
            P.add(ACT, lambda e: e.activation(out=rstd[:, col:col + 1], in_=ss[:, col:col + 1], func=AF.Sqrt,
                                              bias=epsc[:, 0:1], scale=1.0 / D_MODEL),
                  [("ss3", g % 2), "epsc"], [("rstd3", g % 2)])

        def c_stage2(g):
            b = g % NCB
            B_ = cb[b]
            col = 5 + g % 2
            P.add(DVE, lambda e: e.reciprocal(out=rstd[:, col:col + 1], in_=rstd[:, col:col + 1]),
                  [("rstd3", g % 2)], [("rstd3", g % 2)])
            STT(DVE, B_["o"], B_["h"], rstd[:, col:col + 1], gfin_bc, ALU.mult, ALU.mult,
                [("ch", b), ("rstd3", g % 2), "gfin_bc"], [("co", b)])
            DMA(SP, out_d[g * 128:(g + 1) * 128, :], B_["o"], [("co", b)], [("out", g)])

        for g in range(NTILE + 1):
            if g < NTILE:
                if g + NCB - 2 < NTILE:
                    c_load(g + NCB - 2)
                c_stage1(g)
            if g >= 1:
                c_stage2(g - 1)

        P.emit(st)
        build_nc.stats = (P.stats, P.nwaits, A_END, B_END, off[0])
    return nc


_NC_CACHE = {}


def kernel(**inputs):
    x = np.ascontiguousarray(np.asarray(inputs["x"], dtype=np.float32))
    names = ["g_mix", "w_in", "w_dw", "b_dw", "ln_conv_g", "ln_conv_b", "sinks", "w_conv_out", "w_attn_out", "w_out",
             "g_ffn", "w_group", "b_group", "w_expert", "b_expert", "w_gate", "w_up", "w_down", "g_final"]
    shared = {n: np.ascontiguousarray(np.asarray(inputs[n], dtype=np.float32)) for n in names}
    if "nc" not in _NC_CACHE:
        _NC_CACHE["nc"] = build_nc()
    nc = _NC_CACHE["nc"]
    in_maps = []
    for c in range(8):
        m = dict(shared)
        m["x"] = x[c]
        in_maps.append(m)
    res = run_bass_kernel_spmd(nc, in_maps, core_ids=list(range(8)))
    return np.stack([np.asarray(r["out"], dtype=np.float32) for r in res.results], axis=0)
```
